# Optimizing a Trainium2 kernel written in Bass

```python
import math
import jax, jax.numpy as jnp
from jax import lax
import numpy as np

D_MODEL = 1024
BATCH = 8
SEQ = 2048
DEPTH = 2

CTX_LEN = 256
GRID_W = 64
EPS = 1e-6
ROPE_BASE = 10000.0

D_MIX = D_MODEL
N_MIXERS = 4
GROUP_W = D_MIX // N_MIXERS
HEAD_DIM = 64
CHUNK = 64

RET_HEADS = GROUP_W // HEAD_DIM
RET_DK = HEAD_DIM
GLA_HEADS = 4
GLA_DV = GROUP_W // GLA_HEADS
GLA_DK = GLA_DV // 2
GLA_QK = GLA_HEADS * GLA_DK
GLA_RANK = 16
GLA_TAU = 16.0
GLA_CHUNK = 16
SSD_HEADS = GROUP_W // HEAD_DIM
SSD_GROUPS = 2
SSD_STATE = 64
SSD_CONV = 5
SSD_BC = SSD_GROUPS * SSD_STATE
SSD_CONV_CH = GROUP_W + 2 * SSD_BC
MLSTM_HEADS = 4
MLSTM_DH = GROUP_W // MLSTM_HEADS
N_EXPERTS = 16
EC_CAPACITY = 2
EXPERT_FF = 1536

RET_COLS = 4 * GROUP_W
GLA_COLS = 2 * GLA_QK + 2 * GROUP_W + GLA_RANK
SSD_COLS = GROUP_W + SSD_CONV_CH + 2 * SSD_HEADS
MLSTM_COLS = 4 * GROUP_W + 4 * MLSTM_HEADS
IN_COLS = RET_COLS + GLA_COLS + SSD_COLS + MLSTM_COLS

kernel_name = 'hybrid_bidir_ret_gla_ssd_mlstm_ec'

F32 = jnp.float32


def rms_norm(x, w):
    xf = x.astype(F32)
    y = xf * lax.rsqrt(jnp.mean(jnp.square(xf), axis=-1, keepdims=True) + EPS)
    return (y * w).astype(x.dtype)


def modulate(x, w, shift, scale):
    return rms_norm(x, w) * (1 + scale) + shift


def to_heads(t, n_heads):
    b, n, _ = t.shape
    return t.reshape(b, n, n_heads, -1).transpose(0, 2, 1, 3)


def from_heads(t):
    b, h, n, d = t.shape
    return t.transpose(0, 2, 1, 3).reshape(b, n, h * d)


def head_norm(y, w, center):
    if center:
        y = y - jnp.mean(y, axis=-1, keepdims=True)
    y = y * lax.rsqrt(jnp.mean(jnp.square(y), axis=-1, keepdims=True) + EPS)
    return from_heads(y) * w


def _rotate(t, pos):
    nf = t.shape[-1] // 2
    inv = ROPE_BASE ** (-jnp.arange(nf, dtype=F32) / nf)
    ang = pos[:, None] * inv[None, :]
    cos, sin = jnp.cos(ang).astype(t.dtype), jnp.sin(ang).astype(t.dtype)
    t1, t2 = t[..., :nf], t[..., nf:]
    return jnp.concatenate([t1 * cos - t2 * sin, t1 * sin + t2 * cos], axis=-1)


def axial_rope(t, rows, cols):
    half = t.shape[-1] // 2
    return jnp.concatenate([_rotate(t[..., :half], rows), _rotate(t[..., half:], cols)], axis=-1)


def depthwise_conv(x, w, b):
    pad = (w.shape[0] - 1) // 2
    y = lax.conv_general_dilated(x, w[:, None, :], window_strides=(1,), padding=[(pad, pad)],
                                 dimension_numbers=('NWC', 'WIO', 'NWC'),
                                 feature_group_count=x.shape[-1])
    return y + b


def _causal_mask(n):
    return jnp.tril(jnp.ones((n, n), dtype=bool))


def scalar_decay_scan(q, k, v, log_a, state, with_output):
    b, h, n_tok, dk = q.shape
    dv = v.shape[-1]
    n = n_tok // CHUNK
    qc = q.astype(F32).reshape(b, h, n, CHUNK, dk)
    kc = k.astype(F32).reshape(b, h, n, CHUNK, dk)
    vc = v.astype(F32).reshape(b, h, n, CHUNK, dv)
    g = jnp.cumsum(log_a.astype(F32).reshape(b, h, n, CHUNK), axis=-1)
    g_last = g[..., -1]
    kv = jnp.einsum('bhncd,bhnce->bhnde', kc * jnp.exp(g_last[..., None] - g)[..., None], vc)

    def step(s, inp):
        a, kv_c = inp
        return a[..., None, None] * s + kv_c, s

    s_fin, s_in = lax.scan(step, state, (jnp.moveaxis(jnp.exp(g_last), 2, 0), jnp.moveaxis(kv, 2, 0)))
    if not with_output:
        return None, s_fin
    s_in = jnp.moveaxis(s_in, 0, 2)
    decay = jnp.exp(jnp.where(_causal_mask(CHUNK), g[..., :, None] - g[..., None, :], -jnp.inf))
    scores = jnp.einsum('bhnid,bhnjd->bhnij', qc, kc) * decay
    o = (jnp.einsum('bhnij,bhnje->bhnie', scores, vc)
         + jnp.einsum('bhnid,bhnde->bhnie', qc * jnp.exp(g)[..., None], s_in))
    return o.reshape(b, h, n_tok, dv), s_fin


def gla_scan(q, k, v, log_a, state, with_output):
    b, h, n_tok, dk = q.shape
    dv = v.shape[-1]
    n = n_tok // GLA_CHUNK
    qc = q.astype(F32).reshape(b, h, n, GLA_CHUNK, dk)
    kc = k.astype(F32).reshape(b, h, n, GLA_CHUNK, dk)
    vc = v.astype(F32).reshape(b, h, n, GLA_CHUNK, dv)
    g = jnp.cumsum(log_a.astype(F32).reshape(b, h, n, GLA_CHUNK, dk), axis=3)
    g_last = g[:, :, :, -1]
    kv = jnp.einsum('bhncd,bhnce->bhnde', kc * jnp.exp(g_last[:, :, :, None] - g), vc)

    def step(s, inp):
        a, kv_c = inp
        return a[..., None] * s + kv_c, s

    s_fin, s_in = lax.scan(step, state, (jnp.moveaxis(jnp.exp(g_last), 2, 0), jnp.moveaxis(kv, 2, 0)))
    if not with_output:
        return None, s_fin
    s_in = jnp.moveaxis(s_in, 0, 2)
    diff = g[:, :, :, :, None, :] - g[:, :, :, None, :, :]
    decay = jnp.exp(jnp.where(_causal_mask(GLA_CHUNK)[:, :, None], diff, -jnp.inf))
    scores = jnp.einsum('bhnid,bhnjd,bhnijd->bhnij', qc, kc, decay)
    o = (jnp.einsum('bhnij,bhnje->bhnie', scores, vc)
         + jnp.einsum('bhnid,bhnde->bhnie', qc * jnp.exp(g), s_in))
    return o.reshape(b, h, n_tok, dv), s_fin


def mlstm_scan(q, k, v, log_f, i_pre, state, with_output):
    b, h, n_tok, dk = q.shape
    dv = v.shape[-1]
    n = n_tok // CHUNK
    qc = q.astype(F32).reshape(b, h, n, CHUNK, dk)
    kc = k.astype(F32).reshape(b, h, n, CHUNK, dk)
    vc = v.astype(F32).reshape(b, h, n, CHUNK, dv)
    g = jnp.cumsum(log_f.astype(F32).reshape(b, h, n, CHUNK), axis=-1)
    ig = i_pre.astype(F32).reshape(b, h, n, CHUNK)
    g_last = g[..., -1]
    w_end = g_last[..., None] - g + ig
    b_max = jnp.max(w_end, axis=-1)
    e = jnp.exp(w_end - b_max[..., None])
    kv = jnp.einsum('bhnc,bhncd,bhnce->bhnde', e, kc, vc)
    ksum = jnp.einsum('bhnc,bhncd->bhnd', e, kc)

    def step(carry, inp):
        c_s, n_s, m_s = carry
        a, b_c, kv_c, k_c = inp
        m_new = jnp.maximum(a + m_s, b_c)
        old = jnp.exp(a + m_s - m_new)
        new = jnp.exp(b_c - m_new)
        return (old[..., None, None] * c_s + new[..., None, None] * kv_c,
                old[..., None] * n_s + new[..., None] * k_c, m_new), carry

    mv = lambda t: jnp.moveaxis(t, 2, 0)
    final, (c_in, n_in, m_in) = lax.scan(step, state, (mv(g_last), mv(b_max), mv(kv), mv(ksum)))
    if not with_output:
        return None, final
    c_in, n_in, m_in = [jnp.moveaxis(t, 0, 2) for t in (c_in, n_in, m_in)]
    d_log = jnp.where(_causal_mask(CHUNK), g[..., :, None] - g[..., None, :] + ig[..., None, :], -jnp.inf)
    m_inter = g + m_in[..., None]
    m_tot = jnp.maximum(m_inter, jnp.max(d_log, axis=-1))
    qk = jnp.einsum('bhnid,bhnjd->bhnij', qc, kc) * jnp.exp(d_log - m_tot[..., None])
    s_inter = jnp.exp(m_inter - m_tot)
    num = (jnp.einsum('bhnij,bhnje->bhnie', qk, vc)
           + s_inter[..., None] * jnp.einsum('bhnid,bhnde->bhnie', qc, c_in))
    den = jnp.sum(qk, axis=-1) + s_inter * jnp.einsum('bhnid,bhnd->bhni', qc, n_in)
    hout = num / jnp.maximum(jnp.abs(den), jnp.exp(-m_tot))[..., None]
    return hout.reshape(b, h, n_tok, dv), final


def _seq_flip(t, reverse):
    return jnp.flip(t, axis=2) if reverse else t


def bidirectional(scan_fn, ctx_dirs, lat_dirs, init_state, ctx_out):
    y_ctx, y_lat = None, None
    for d in range(2):
        rev = d == 1
        o_c, s_c = scan_fn(*[_seq_flip(t, rev) for t in ctx_dirs[d]], init_state, ctx_out)
        o_l, _ = scan_fn(*[_seq_flip(t, rev) for t in lat_dirs[d]], s_c, True)
        o_l = _seq_flip(o_l, rev)
        y_lat = o_l if y_lat is None else y_lat + o_l
        if ctx_out:
            o_c = _seq_flip(o_c, rev)
            y_ctx = o_c if y_ctx is None else y_ctx + o_c
    return y_ctx, y_lat


def retention_mixer(u_ctx, u_lat, decay_logit, norm_w, rows, cols, ctx_out):
    log_gamma = jax.nn.log_sigmoid(decay_logit.astype(F32))

    def prep(u, rope):
        q, k, v, g = jnp.split(u, 4, axis=-1)
        q = to_heads(q, RET_HEADS) * RET_DK ** -0.5
        k = to_heads(k, RET_HEADS)
        if rope:
            q, k = axial_rope(q, rows, cols), axial_rope(k, rows, cols)
        v = to_heads(v, RET_HEADS)
        b, h, n, _ = q.shape
        dirs = [(q, k, v, jnp.broadcast_to(log_gamma[d][None, :, None], (b, h, n))) for d in range(2)]
        return dirs, g

    ctx_dirs, g_ctx = prep(u_ctx, False)
    lat_dirs, g_lat = prep(u_lat, True)
    s0 = jnp.zeros((u_lat.shape[0], RET_HEADS, RET_DK, RET_DK), F32)
    o_ctx, o_lat = bidirectional(scalar_decay_scan, ctx_dirs, lat_dirs, s0, ctx_out)
    out = lambda o, g: (head_norm(o, norm_w, True) * jax.nn.silu(g.astype(F32))).astype(g.dtype)
    return (out(o_ctx, g_ctx) if ctx_out else None), out(o_lat, g_lat)


def gla_mixer(u_ctx, u_lat, gate_w, gate_b, norm_w, ctx_out):
    cuts = [GLA_QK, 2 * GLA_QK, 2 * GLA_QK + GROUP_W, 2 * GLA_QK + 2 * GROUP_W]

    def prep(u):
        q, k, v, r, lr = jnp.split(u, cuts, axis=-1)
        q = to_heads(q, GLA_HEADS) * GLA_DK ** -0.5
        k = to_heads(k, GLA_HEADS)
        v = to_heads(v, GLA_HEADS)
        dirs = []
        for d in range(2):
            log_a = jax.nn.log_sigmoid((lr @ gate_w[d] + gate_b[d]).astype(F32)) / GLA_TAU
            dirs.append((q, k, v, to_heads(log_a, GLA_HEADS)))
        return dirs, r

    ctx_dirs, r_ctx = prep(u_ctx)
    lat_dirs, r_lat = prep(u_lat)
    s0 = jnp.zeros((u_lat.shape[0], GLA_HEADS, GLA_DK, GLA_DV), F32)
    o_ctx, o_lat = bidirectional(gla_scan, ctx_dirs, lat_dirs, s0, ctx_out)
    out = lambda o, r: (head_norm(o, norm_w, False) * jax.nn.silu(r.astype(F32))).astype(r.dtype)
    return (out(o_ctx, r_ctx) if ctx_out else None), out(o_lat, r_lat)


def ssd_mixer(u_ctx, u_lat, conv_w, conv_b, dt_bias, a_log, d_skip, norm_w, ctx_out):
    a_neg = -jnp.exp(a_log.astype(F32))
    rep = SSD_HEADS // SSD_GROUPS

    def prep(u):
        z, xbc, dt = jnp.split(u, [GROUP_W, GROUP_W + SSD_CONV_CH], axis=-1)
        xbc = jax.nn.silu(depthwise_conv(xbc, conv_w, conv_b))
        xs, bm, cm = jnp.split(xbc, [GROUP_W, GROUP_W + SSD_BC], axis=-1)
        xs = to_heads(xs, SSD_HEADS)
        bm = jnp.repeat(to_heads(bm, SSD_GROUPS), rep, axis=1)
        cm = jnp.repeat(to_heads(cm, SSD_GROUPS), rep, axis=1)
        dirs = []
        for d in range(2):
            dt_d = jax.nn.softplus(dt[..., d * SSD_HEADS:(d + 1) * SSD_HEADS].astype(F32) + dt_bias[d])
            dt_d = dt_d.transpose(0, 2, 1)
            dirs.append((cm, bm, xs * dt_d[..., None], dt_d * a_neg[d][:, None]))
        return dirs, xs, z

    ctx_dirs, x_ctx, z_ctx = prep(u_ctx)
    lat_dirs, x_lat, z_lat = prep(u_lat)
    s0 = jnp.zeros((u_lat.shape[0], SSD_HEADS, SSD_STATE, HEAD_DIM), F32)
    o_ctx, o_lat = bidirectional(scalar_decay_scan, ctx_dirs, lat_dirs, s0, ctx_out)

    def out(o, xs, z):
        y = from_heads(o + d_skip[:, None, None] * xs)
        return rms_norm(y * jax.nn.silu(z.astype(F32)), norm_w).astype(z.dtype)

    return (out(o_ctx, x_ctx, z_ctx) if ctx_out else None), out(o_lat, x_lat, z_lat)


def mlstm_mixer(u_ctx, u_lat, gate_b, norm_w, ctx_out):
    def prep(u):
        q, k, v, o, gates = jnp.split(u, [GROUP_W, 2 * GROUP_W, 3 * GROUP_W, 4 * GROUP_W], axis=-1)
        q = to_heads(q, MLSTM_HEADS)
        k = to_heads(k, MLSTM_HEADS) * MLSTM_DH ** -0.5
        v = to_heads(v, MLSTM_HEADS)
        b, n, _ = gates.shape
        gates = gates.astype(F32).reshape(b, n, 2, 2, MLSTM_HEADS) + gate_b
        dirs = [(q, k, v, jax.nn.log_sigmoid(gates[:, :, d, 1]).transpose(0, 2, 1),
                 gates[:, :, d, 0].transpose(0, 2, 1)) for d in range(2)]
        return dirs, o

    ctx_dirs, o_ctx_gate = prep(u_ctx)
    lat_dirs, o_lat_gate = prep(u_lat)
    b = u_lat.shape[0]
    s0 = (jnp.zeros((b, MLSTM_HEADS, MLSTM_DH, MLSTM_DH), F32),
          jnp.zeros((b, MLSTM_HEADS, MLSTM_DH), F32),
          jnp.zeros((b, MLSTM_HEADS), F32))
    h_ctx, h_lat = bidirectional(mlstm_scan, ctx_dirs, lat_dirs, s0, ctx_out)
    out = lambda h, og: (jax.nn.sigmoid(og.astype(F32)) * head_norm(h, norm_w, False)).astype(og.dtype)
    return (out(h_ctx, o_ctx_gate) if ctx_out else None), out(h_lat, o_lat_gate)


def parallel_mixers(u_ctx, u_lat, ret_decay_logit, ret_norm_w, gla_gate_w, gla_gate_b, gla_norm_w,
                    ssd_conv_w, ssd_conv_b, ssd_dt_bias, ssd_a_log, ssd_d, ssd_norm_w,
                    mlstm_gate_b, mlstm_norm_w, rows, cols, ctx_out):
    cuts = [RET_COLS, RET_COLS + GLA_COLS, RET_COLS + GLA_COLS + SSD_COLS]
    a_c, b_c, s_c, m_c = jnp.split(u_ctx, cuts, axis=-1)
    a_l, b_l, s_l, m_l = jnp.split(u_lat, cuts, axis=-1)
    outs = [retention_mixer(a_c, a_l, ret_decay_logit, ret_norm_w, rows, cols, ctx_out),
            gla_mixer(b_c, b_l, gla_gate_w, gla_gate_b, gla_norm_w, ctx_out),
            ssd_mixer(s_c, s_l, ssd_conv_w, ssd_conv_b, ssd_dt_bias, ssd_a_log, ssd_d, ssd_norm_w, ctx_out),
            mlstm_mixer(m_c, m_l, mlstm_gate_b, mlstm_norm_w, ctx_out)]
    y_lat = jnp.concatenate([o[1] for o in outs], axis=-1)
    y_ctx = jnp.concatenate([o[0] for o in outs], axis=-1) if ctx_out else None
    return y_ctx, y_lat


def expert_choice_ffn(h, router_w, w_gate, w_up, w_down):
    n_tok = h.shape[1]
    cap = EC_CAPACITY * n_tok // N_EXPERTS
    aff = jax.nn.softmax(jnp.einsum('btd,de->bte', h, router_w).astype(F32), axis=-1)
    top_aff, top_idx = lax.top_k(jnp.swapaxes(aff, 1, 2), cap)
    xs = jax.vmap(lambda hb, ib: hb[ib])(h, top_idx)
    hid = (jax.nn.silu(jnp.einsum('becd,edf->becf', xs, w_gate))
           * jnp.einsum('becd,edf->becf', xs, w_up))
    out = jnp.einsum('becf,efd->becd', hid, w_down) * top_aff[..., None].astype(h.dtype)
    b, e, cp, d = out.shape
    return jax.vmap(lambda o, i: jax.ops.segment_sum(o, i, num_segments=n_tok))(
        out.reshape(b, e * cp, d), top_idx.reshape(b, e * cp))


def setup_inputs(seed: int = 0) -> dict:
    key = jax.random.key(seed)
    kit = iter(jax.random.split(key, 40))
    nrm = lambda shape, scale: jax.random.normal(next(kit), shape, F32) * scale
    gam = 1.0 - 2.0 ** (-5.0 - jnp.arange(RET_HEADS, dtype=F32))
    ret_logit = jnp.log(gam / (1.0 - gam))
    dt0 = jnp.exp(jax.random.uniform(next(kit), (DEPTH, 2, SSD_HEADS), F32,
                                     minval=math.log(1e-3), maxval=math.log(1e-1)))
    i_bias = nrm((DEPTH, 2, MLSTM_HEADS), 0.1)
    f_bias = jnp.linspace(3.0, 6.0, MLSTM_HEADS, dtype=F32) + nrm((DEPTH, 2, MLSTM_HEADS), 0.1)
    return {
        'x': nrm((BATCH, SEQ, D_MODEL), 1.0),
        'c': nrm((BATCH, D_MODEL), 1.0),
        'ctx': nrm((BATCH, CTX_LEN, D_MODEL), 1.0),
        'c_ctx': nrm((D_MODEL,), 1.0),
        'w_mod': nrm((DEPTH, D_MODEL, 6 * D_MODEL), 0.5 * D_MODEL ** -0.5),
        'b_mod': nrm((DEPTH, 6 * D_MODEL), 0.02),
        'norm1_w': 1.0 + nrm((DEPTH, D_MODEL), 0.05),
        'norm2_w': 1.0 + nrm((DEPTH, D_MODEL), 0.05),
        'w_in': nrm((DEPTH, D_MODEL, IN_COLS), D_MODEL ** -0.5),
        'w_out': nrm((DEPTH, D_MIX, D_MODEL), D_MIX ** -0.5),
        'ret_decay_logit': ret_logit + nrm((DEPTH, 2, RET_HEADS), 0.1),
        'ret_norm_w': 1.0 + nrm((DEPTH, GROUP_W), 0.05),
        'gla_gate_w': nrm((DEPTH, 2, GLA_RANK, GLA_QK), GLA_RANK ** -0.5),
        'gla_gate_b': nrm((DEPTH, 2, GLA_QK), 0.5),
        'gla_norm_w': 1.0 + nrm((DEPTH, GROUP_W), 0.05),
        'ssd_conv_w': nrm((DEPTH, SSD_CONV, SSD_CONV_CH), SSD_CONV ** -0.5),
        'ssd_conv_b': nrm((DEPTH, SSD_CONV_CH), 0.02),
        'ssd_dt_bias': dt0 + jnp.log(-jnp.expm1(-dt0)),
        'ssd_a_log': jnp.log(jax.random.uniform(next(kit), (DEPTH, 2, SSD_HEADS), F32, minval=1.0, maxval=16.0)),
        'ssd_d': 1.0 + nrm((DEPTH, SSD_HEADS), 0.1),
        'ssd_norm_w': 1.0 + nrm((DEPTH, GROUP_W), 0.05),
        'mlstm_gate_b': jnp.stack([i_bias, f_bias], axis=2),
        'mlstm_norm_w': 1.0 + nrm((DEPTH, GROUP_W), 0.05),
        'router_w': nrm((DEPTH, D_MODEL, N_EXPERTS), D_MODEL ** -0.5),
        'expert_w_gate': nrm((DEPTH, N_EXPERTS, D_MODEL, EXPERT_FF), D_MODEL ** -0.5),
        'expert_w_up': nrm((DEPTH, N_EXPERTS, D_MODEL, EXPERT_FF), D_MODEL ** -0.5),
        'expert_w_down': nrm((DEPTH, N_EXPERTS, EXPERT_FF, D_MODEL), EXPERT_FF ** -0.5),
        'final_norm_w': 1.0 + nrm((D_MODEL,), 0.05),
    }


def reference(x, c, ctx, c_ctx, w_mod, b_mod, norm1_w, norm2_w, w_in, w_out,
              ret_decay_logit, ret_norm_w, gla_gate_w, gla_gate_b, gla_norm_w,
              ssd_conv_w, ssd_conv_b, ssd_dt_bias, ssd_a_log, ssd_d, ssd_norm_w,
              mlstm_gate_b, mlstm_norm_w, router_w, expert_w_gate, expert_w_up, expert_w_down,
              final_norm_w):
    n_lat = x.shape[1]
    ROWS = n_lat // GRID_W
    rows = jnp.repeat(jnp.arange(ROWS), GRID_W).astype(F32)
    cols = jnp.tile(jnp.arange(GRID_W), ROWS).astype(F32)
    h_ctx, h_lat = ctx, x
    for l in range(DEPTH):
        ctx_out = l < DEPTH - 1
        sh1, sc1, g1, sh2, sc2, g2 = jnp.split((jax.nn.silu(c) @ w_mod[l] + b_mod[l])[:, None, :], 6, axis=-1)
        csh1, csc1, cg1, csh2, csc2, cg2 = jnp.split(jax.nn.silu(c_ctx) @ w_mod[l] + b_mod[l], 6, axis=-1)
        u_lat = modulate(h_lat, norm1_w[l], sh1, sc1) @ w_in[l]
        u_ctx = modulate(h_ctx, norm1_w[l], csh1, csc1) @ w_in[l]
        y_ctx, y_lat = parallel_mixers(u_ctx, u_lat, ret_decay_logit[l], ret_norm_w[l],
                                       gla_gate_w[l], gla_gate_b[l], gla_norm_w[l],
                                       ssd_conv_w[l], ssd_conv_b[l], ssd_dt_bias[l], ssd_a_log[l],
                                       ssd_d[l], ssd_norm_w[l], mlstm_gate_b[l], mlstm_norm_w[l],
                                       rows, cols, ctx_out)
        h_lat = h_lat + g1 * (y_lat @ w_out[l])
        h_lat = h_lat + g2 * expert_choice_ffn(modulate(h_lat, norm2_w[l], sh2, sc2), router_w[l],
                                               expert_w_gate[l], expert_w_up[l], expert_w_down[l])
        if ctx_out:
            h_ctx = h_ctx + cg1 * (y_ctx @ w_out[l])
            h_ctx = h_ctx + cg2 * expert_choice_ffn(modulate(h_ctx, norm2_w[l], csh2, csc2), router_w[l],
                                                   expert_w_gate[l], expert_w_up[l], expert_w_down[l])
    return rms_norm(h_lat, final_norm_w)
```

```python
import math
from contextlib import ExitStack
import numpy as np
import concourse.bass as bass
import concourse.mybir as mybir
from concourse.bass_utils import run_bass_kernel_spmd

F32 = mybir.dt.float32
BF16 = mybir.dt.bfloat16
AF = mybir.ActivationFunctionType
ALU = mybir.AluOpType
AX = mybir.AxisListType

NT = 18
NTOK = 2304
EPS = 1e-6
NEGV = -1.0e5
C_TRI = (0, 128)
C_STR = (256, 384)
C_ID = 512
C_ONE = 640
C_NEG = (768, 1024)
C_IOC = 1280
C_IOP = 1536
NCST = 1540


class Dep:
    __slots__ = ("w", "r")

    def __init__(self):
        self.w = None
        self.r = {}


class KB:
    SEMCAP = 20000
    ND = 8

    def __init__(self):
        self.nc = nc = bass.Bass("TRN2", target_bir_lowering=False)
        self.E = {"pe": nc.tensor, "act": nc.scalar, "dve": nc.vector, "pool": nc.gpsimd, "sp": nc.sync}
        self.cnt = {e: 0 for e in self.E}
        self.sems = {e: [] for e in self.E}
        self.waited = {e: {} for e in self.E}
        self.dmak = {"sp": 0, "pool": 0}
        self.dsems = {q: [nc.alloc_semaphore(f"d_{q}_{i}") for i in range(self.ND)] for q in self.dmak}
        self.latest = {}
        self.uid = 0

    def name(self, s):
        self.uid += 1
        return f"{s}_{self.uid}"

    def _tok(self, e):
        kk = self.cnt[e]
        i = kk // self.SEMCAP
        if i >= len(self.sems[e]):
            self.sems[e].append(self.nc.alloc_semaphore(f"s_{e}_{i}"))
        self.cnt[e] = kk + 1
        t = (self.sems[e][i], kk % self.SEMCAP + 1, f"{e}{i}", e)
        self.latest[t[2]] = t
        return t

    def _wait(self, e, toks):
        best = {}
        for t in toks:
            if t is None:
                continue
            if e == "pe" and t[3] == "pe":
                continue
            if best.get(t[2], (None, 0))[1] < t[1]:
                best[t[2]] = t
        for key, t in best.items():
            if self.waited[e].get(key, 0) >= t[1]:
                continue
            self.E[e].wait_ge(t[0], t[1])
            self.waited[e][key] = t[1]

    @staticmethod
    def _deps(reads, writes):
        toks = []
        for d in reads:
            toks.append(d.w)
        for d in writes:
            toks.append(d.w)
            toks.extend(d.r.values())
        return toks

    @staticmethod
    def _mark(t, reads, writes):
        for d in reads:
            d.r[t[2]] = t
        for d in writes:
            d.w = t
            d.r = {}

    def op(self, e, fn, reads=(), writes=(), pe_serial=False):
        self._wait(e, self._deps(reads, writes))
        if pe_serial and e == "pe" and self.cnt["pe"] > 0:
            kk = self.cnt["pe"] - 1
            i = kk // self.SEMCAP
            key = f"pe{i}"
            val = kk % self.SEMCAP + 1
            if self.waited["pe"].get(key, 0) < val:
                self.E["pe"].wait_ge(self.sems["pe"][i], val)
                self.waited["pe"][key] = val
        ins = fn(self.E[e])
        t = self._tok(e)
        ins.then_inc(t[0], 1)
        self._mark(t, reads, writes)
        return t

    def dma(self, q, out, in_, reads=(), writes=(), **kw):
        kk = self.dmak[q]
        slot = kk % self.ND
        val = 16 * (kk // self.ND + 1)
        sem = self.dsems[q][slot]
        key = f"d{q}{slot}"
        toks = self._deps(reads, writes)
        if kk >= self.ND:
            toks.append((sem, val - 16, key, "dma"))
        self._wait(q, toks)
        self.E[q].dma_start(out=out, in_=in_, **kw).then_inc(sem, 16)
        self.dmak[q] = kk + 1
        t = (sem, val, key, "dma")
        self.latest[key] = t
        self._mark(t, reads, writes)
        return t

    def barrier(self):
        toks = list(self.latest.values())
        for e in self.E:
            self._wait(e, toks)


LIVE = set()


class Rot:
    def __init__(self, items):
        self.items = items
        self.i = 0
        self.owner = [None] * len(items)

    def next(self, owner=None):
        j = self.i % len(self.items)
        if self.owner[j] is not None and self.owner[j] in LIVE and self.owner[j] != owner:
            raise AssertionError("rotating buffer reused while its previous owner generator is still live")
        self.owner[j] = owner
        it = self.items[j]
        self.i += 1
        return it


_GID = [0]


def run_window(facts, W, limits=None):
    limits = limits or {}
    active = []
    nxt = 0
    while True:
        while len(active) < W and nxt < len(facts):
            f = facts[nxt]
            rdy = getattr(f, "ready", None)
            kind = getattr(f, "kind", None)
            nk = sum(1 for a in active if a[2] == kind)
            if (rdy is not None and not rdy()) or (kind in limits and nk >= limits[kind]):
                assert active, "window deadlock"
                break
            _GID[0] += 1
            gid = _GID[0]
            LIVE.add(gid)
            active.append((gid, f(gid), kind))
            nxt += 1
        if not active:
            break
        still = []
        for gid, g, kind in active:
            try:
                next(g)
                still.append((gid, g, kind))
            except StopIteration:
                LIVE.discard(gid)
        active = still


class _Stop(Exception):
    pass


def build(nlayers=2, stop=None, dbg=()):
    k = KB()
    try:
        _build(k, nlayers, stop, dbg)
    except _Stop:
        pass
    k.barrier()
    return k.nc


def _build(k, nlayers, stop, dbg):
    nc = k.nc

    def chk(tag):
        if stop == tag:
            raise _Stop()

    def dump(name, src, shape, reads, q="pool"):
        if name not in dbg:
            return
        o = nc.dram_tensor(name, list(shape), F32, kind="ExternalOutput").ap()
        k.dma(q, o, src, reads=reads, writes=[Dep()])

    def din(name, shape):
        return nc.dram_tensor(name, list(shape), F32, kind="ExternalInput").ap()

    x_d = din("x", [2048, 1024])
    ctx_d = din("ctx", [256, 1024])
    cin_d = din("cin", [128, 8, 2])
    w_mod = din("w_mod", [2, 1024, 6144])
    bmT = din("bmT", [2, 128, 48])
    nw1T = din("nw1T", [2, 128, 8])
    nw2T = din("nw2T", [2, 128, 8])
    w_in = din("w_in", [2, 1024, 3624])
    w_out = din("w_out", [2, 1024, 1024])
    ret_logit = din("ret_decay_logit", [2, 2, 4])
    ret_nw = din("ret_norm_w", [2, 256])
    gla_gw = din("gla_gate_w", [2, 2, 16, 128])
    gla_gb = din("gla_gate_b", [2, 2, 128])
    gla_nw = din("gla_norm_w", [2, 256])
    ssd_cw = din("ssd_conv_w", [2, 5, 512])
    ssd_cb = din("ssd_conv_b", [2, 512])
    ssd_dtb = din("ssd_dt_bias", [2, 2, 4])
    ssd_alog = din("ssd_a_log", [2, 2, 4])
    ssd_dsk = din("ssd_d", [2, 4])
    ssd_nw = din("ssd_norm_w", [2, 256])
    ml_gb = din("mlstm_gate_b", [2, 2, 2, 4])
    ml_nw = din("mlstm_norm_w", [2, 256])
    router_w = din("router_w", [2, 1024, 16])
    ewg = din("ewg", [2, 16, 1024, 1536])
    ewu = din("ewu", [2, 16, 1024, 1536])
    ewd = din("ewd", [2, 16, 1536, 1024])
    fnw = din("fnw", [1, 1024])
    rope_d = din("rope", [2, 18, 128, 2, 256])
    cst_d = din("cst", [128, NCST])
    msk_d = din("msk", [128, 512])
    esel_d = din("esel", [16, 2048])
    y_d = nc.dram_tensor("y", [2048, 1024], F32, kind="ExternalOutput").ap()
    hd = nc.dram_tensor("hd", [NTOK, 1024], F32, kind="Internal").ap()
    hd_dep = [Dep() for _ in range(NT)]
    y_dep = Dep()

    def sb(name, shape, dt, stack=None):
        if stack is None:
            return nc.alloc_sbuf_tensor(k.name(name), list(shape), dt)
        return stack.enter_context(nc.sbuf_tensor(k.name(name), list(shape), dt))

    def rot(name, n, shape, dt, stack):
        return Rot([(sb(name, shape, dt, stack), Dep()) for _ in range(n)])

    CF = sb("CF", [128, NCST], F32)
    CB = sb("CB", [128, 768], BF16)
    MK = sb("MK", [128, 512], BF16)
    cdep = Dep()
    k.dma("sp", CF[:], cst_d, writes=[cdep])
    k.dma("pool", CB[:], cst_d[:, 0:768], writes=[cdep])
    k.dma("pool", MK[:], msk_d, writes=[cdep])
    NGBt = sb("NGB", [128, 256], BF16)
    k.dma("pool", NGBt[:, 0:128], cst_d[:, C_NEG[0]:C_NEG[0] + 128], writes=[cdep])
    k.dma("pool", NGBt[:, 128:256], cst_d[:, C_NEG[1]:C_NEG[1] + 128], writes=[cdep])
    NGB = [NGBt[:, 0:128], NGBt[:, 128:256]]
    modT = sb("modT", [128, 48, 2], F32)
    A1 = sb("A1", [128, 8, 2], F32)
    A2 = sb("A2", [128, 8, 2], F32)
    mod_dep = Dep()
    TRI = [CF[:, C_TRI[d]:C_TRI[d] + 128] for d in range(2)]
    STR = [CF[:, C_STR[d]:C_STR[d] + 128] for d in range(2)]
    NEG = [CF[:, C_NEG[d]:C_NEG[d] + 256] for d in range(2)]
    IDF = CF[:, C_ID:C_ID + 128]
    ONEF = CF[:, C_ONE:C_ONE + 128]
    IDB = CB[:, C_ID:C_ID + 128]
    ONEB = CB[:, C_ONE:C_ONE + 128]
    TRIB = CB[:, 0:128]
    MSK = [MK[:, d * 256:(d + 1) * 256] for d in range(2)]

    def hsrc(l, c):
        if l == 0:
            if c < 2:
                return ctx_d[c * 128:(c + 1) * 128, :]
            return x_d[(c - 2) * 128:(c - 1) * 128, :]
        return hd[c * 128:(c + 1) * 128, :]

    def tcols(c):
        return slice(c * 128, (c + 1) * 128)

    for l in range(nlayers):
        ctx_out = l < 1
        tiles_out = list(range(NT)) if ctx_out else list(range(2, NT))
        w_in_l = w_in[l].rearrange("(k p) n -> p k n", p=128)
        with ExitStack() as ms:
            psum = [ms.enter_context(nc.psum_tensor(k.name("ps"), [128, 512], F32)) for _ in range(4)]
            psBC = [ms.enter_context(nc.psum_tensor(k.name("psbc"), [128, 1024], F32)) for _ in range(2)]
            pdep = [Dep() for _ in range(8)]
            PA = Rot([(psum[0], pdep[0]), (psum[1], pdep[1])])
            PT_ = Rot([(psum[2], pdep[2]), (psum[3], pdep[3])])
            PA4 = Rot([(psum[0], pdep[0]), (psum[1], pdep[1]), (psBC[0][:, 0:512], pdep[4]), (psBC[0][:, 512:1024], pdep[5])])
            hmodT = sb("hmodT", [128, 8, NTOK], BF16, ms)
            hmodT_d = [Dep() for _ in range(NT)]
            yT = sb("yT", [128, 8, NTOK], BF16, ms)
            yT_d = [Dep() for _ in range(NT)]
            wblk = rot("wblk", 2, [128, 8, 520], BF16, ms)

            with ExitStack() as s0:
                cinS = sb("cinS", [128, 8, 2], F32, s0)
                scS = sb("scS", [128, 8, 2], BF16, s0)
                bmS = sb("bmS", [128, 48], F32, s0)
                nwS = sb("nwS", [128, 16], F32, s0)
                d0 = Dep()
                k.dma("sp", cinS[:], cin_d, writes=[d0])
                k.dma("sp", bmS[:], bmT[l], writes=[d0])
                k.dma("sp", nwS[:, 0:8], nw1T[l], writes=[d0])
                k.dma("sp", nwS[:, 8:16], nw2T[l], writes=[d0])
                k.op("act", lambda e: e.activation(out=scS[:], in_=cinS[:], func=AF.Silu), reads=[d0], writes=[d0])
                wm = w_mod[l].rearrange("(k p) n -> p k n", p=128)
                pm, pmd = PA.next()
                for cb in range(12):
                    wt, wd = wblk.next()
                    k.dma("pool", wt[:, :, 0:512], wm[:, :, cb * 512:(cb + 1) * 512], writes=[wd])
                    for j in range(4):
                        cc = cb * 4 + j
                        for kk in range(8):
                            k.op("pe", lambda e, cc=cc, kk=kk, j=j, wt=wt: e.matmul(
                                pm[:, 2 * cc:2 * cc + 2], lhsT=wt[:, kk, j * 128:(j + 1) * 128], rhs=scS[:, kk, :],
                                start=(kk == 0), stop=(kk == 7)), reads=[wd, d0], writes=[pmd])
                k.op("dve", lambda e: e.tensor_tensor(
                    out=modT[:], in0=pm[:, 0:96].rearrange("p (c r) -> p c r", r=2),
                    in1=bmS[:].unsqueeze(2).to_broadcast([128, 48, 2]), op=ALU.add), reads=[pmd, d0, mod_dep], writes=[mod_dep])
                for c0 in (8, 32):
                    k.op("dve", lambda e, c0=c0: e.tensor_scalar(out=modT[:, c0:c0 + 8, :], in0=modT[:, c0:c0 + 8, :],
                                                                  scalar1=1.0, scalar2=None, op0=ALU.add), writes=[mod_dep])
                k.op("dve", lambda e: e.tensor_tensor(out=A1[:], in0=modT[:, 8:16, :],
                                                      in1=nwS[:, 0:8].unsqueeze(2).to_broadcast([128, 8, 2]), op=ALU.mult),
                     reads=[d0], writes=[mod_dep])
                k.op("dve", lambda e: e.tensor_tensor(out=A2[:], in0=modT[:, 32:40, :],
                                                      in1=nwS[:, 8:16].unsqueeze(2).to_broadcast([128, 8, 2]), op=ALU.mult),
                     reads=[d0], writes=[mod_dep])
                k.barrier()
                if l == 0:
                    dump("d_modT", modT[:].rearrange("p c r -> p (c r)"), [128, 96], [mod_dep], q="sp")
                    chk("p0")

            with ExitStack() as s1:
                hp_ = rot("hp", 4, [128, 1024], F32, s1)
                xnp = rot("xn", 4, [128, 1024], BF16, s1)
                jk = rot("jk", 2, [128, 1024], BF16, s1)
                smp = rot("sm", 4, [128, 4], F32, s1)
                def p1_tile(c, gid):
                    r = 1 if c < 2 else 0
                    ht, hdp = hp_.next(gid)
                    k.dma("sp", ht[:], hsrc(l, c), reads=[hd_dep[c]], writes=[hdp])
                    st, sd_ = smp.next(gid)
                    jt, jd = jk.next()
                    yield
                    k.op("act", lambda e: e.activation(out=jt[:], in_=ht[:], func=AF.Square, accum_out=st[:, 0:1]),
                         reads=[hdp], writes=[jd, sd_])
                    yield
                    k.op("act", lambda e: e.activation(out=st[:, 1:2], in_=st[:, 0:1], func=AF.Sqrt, scale=1.0 / 1024, bias=EPS),
                         writes=[sd_])
                    yield
                    k.op("dve", lambda e: e.reciprocal(out=st[:, 2:3], in_=st[:, 1:2]), writes=[sd_])
                    yield
                    xt, xd = xnp.next(gid)
                    k.op("dve", lambda e: e.tensor_scalar(out=xt[:], in0=ht[:], scalar1=st[:, 2:3], scalar2=None, op0=ALU.mult),
                         reads=[hdp, sd_], writes=[xd])
                    yield
                    pt, ptd = PT_.next()
                    ptb = pt[:].bitcast(BF16)
                    for kk in range(8):
                        k.op("pe", lambda e, kk=kk: e.transpose(ptb[:, kk * 128:(kk + 1) * 128], xt[:, kk * 128:(kk + 1) * 128], IDB),
                             reads=[xd, cdep], writes=[ptd])
                    for kk in range(8):
                        k.op("act", lambda e, kk=kk: e.activation(
                            out=hmodT[:, kk, tcols(c)], in_=ptb[:, kk * 128:(kk + 1) * 128], func=AF.Identity,
                            scale=A1[:, kk, r:r + 1], bias=modT[:, kk, r:r + 1]), reads=[ptd, mod_dep], writes=[hmodT_d[c]])
                    yield
                run_window([(lambda gid, c=c: p1_tile(c, gid)) for c in range(NT)], 4)
                k.barrier()
                if l == 0:
                    dump("d_hmodT", hmodT[:].rearrange("p k t -> p (k t)"), [128, 8 * NTOK], hmodT_d)
                    chk("p1")

            qTx = sb("qTx", [128, 2, NTOK], BF16, ms)
            k.op("pool", lambda e: e.memset(qTx[:], 0.0), writes=[Dep()])
            k.barrier()
            qT = qTx[:, 0, :]
            kT = sb("kT", [128, NTOK], BF16, ms)
            ktok = sb("ktok", [128, NT, 128], BF16, ms)
            vtok = sb("vtok", [128, NT, 2, 65], BF16, ms)
            gate = sb("gate", [128, NT, 128], BF16, ms)
            oacc = sb("oacc", [128, NT, 2, 65], F32, ms)
            lai = sb("lai", [128, NT, 2, 2, 2], F32, ms)
            aux = sb("aux", [128, 2312], F32, ms)
            ygs = sb("ygs", [128, NT, 260], BF16, ms)
            ssq = sb("ssq", [128, NT, 2], F32, ms)
            qT_d = [Dep() for _ in range(NT)]
            qT_d2 = [Dep() for _ in range(NT)]
            kT_d = [Dep() for _ in range(NT)]
            ktok_d = [Dep() for _ in range(NT)]
            vtok_d = [Dep() for _ in range(NT)]
            gate_d = [Dep() for _ in range(NT)]
            oacc_d = [Dep() for _ in range(NT)]
            lai_d = [Dep() for _ in range(NT)]
            aux_d = Dep()
            ygs_d = [Dep() for _ in range(NT)]
            lag = aux[:, 0:NTOK].rearrange("p (c d e) -> p c d e", c=NT, d=2)
            lag_d = [Dep() for _ in range(NT)]
            for c in range(NT):
                k.op("pool", lambda e, c=c: e.memset(vtok[:, c, :, 64:65], 1.0), writes=[vtok_d[c]])
            usb = rot("usb", 2, [128, 512], F32, ms)
            ropep = rot("ropet", 2, [128, 2, 256], F32, ms)
            qkp = rot("qk", 2, [128, 256], BF16, ms)
            shr4 = sb("shr4", [128, 1024], F32, ms)
            t1p = Rot([(shr4[:, 0:256], Dep()), (shr4[:, 256:512], Dep())])
            t2p = Rot([(shr4[:, 512:768], Dep()), (shr4[:, 768:1024], Dep())])
            prm = sb("prm", [128, 64], F32, ms)
            prm_d = Dep()
            nwb = sb("nwb", [128, 256], F32, ms)
            nwb_d = Dep()
            S32 = [sb("S32", [128, 2, 65], F32, ms) for _ in range(2)]
            Sbf = [sb("Sbf", [128, 2, 65], BF16, ms) for _ in range(2)]
            S_d = [Dep() for _ in range(2)]
            S_dh = [[Dep(), Dep()] for _ in range(2)]
            Sbf_d = [Dep() for _ in range(2)]
            Dw = [rot("Dw", 3, [128, 256], BF16, ms) for _ in range(2)]
            PTw = [rot("PTw", 4, [128, 256], BF16, ms) for _ in range(2)]
            t1w = [rot("t1w", 2, [128, 2, 65], F32, ms) for _ in range(2)]
            kgw = [rot("kgw", 2, [128, 128], BF16, ms) for _ in range(2)]

            fin = rot("fin", 6, [128, 2, 64], F32, ms)
            fin2 = rot("fin2", 6, [128, 2, 64], F32, ms)
            finb = rot("finb", 4, [128, 128], BF16, ms)
            fsm = rot("fsm", 6, [128, 8], F32, ms)

            def load_wblk(colranges):
                wt, wd = wblk.next()
                o = 0
                for (c0, n) in colranges:
                    k.dma("pool", wt[:, :, o:o + n], w_in_l[:, :, c0:c0 + n], writes=[wd])
                    o += n
                return wt, wd, o

            def inproj_tok(wt, wd, ncols, c, pt, ptd, off=0, w0=0):
                for kk in range(8):
                    k.op("pe", lambda e, kk=kk: e.matmul(pt[:, off:off + ncols], lhsT=hmodT[:, kk, tcols(c)], rhs=wt[:, kk, w0:w0 + ncols],
                                                          start=(kk == 0), stop=(kk == 7)), reads=[hmodT_d[c], wd], writes=[ptd])

            def transpose_to(dstT, dst_d, src, src_d, nrows_out, c):
                pt, ptd = PT_.next()
                ptb = pt[:].bitcast(BF16)
                k.op("pe", lambda e: e.transpose(ptb[0:nrows_out, 0:128], src, IDB), reads=[src_d, cdep], writes=[ptd])
                k.op("act", lambda e: e.copy(out=dstT[0:nrows_out, tcols(c)], in_=ptb[0:nrows_out, 0:128]), reads=[ptd], writes=[dst_d])

            def softplus_neg(dst, src, reads, writes):
                k.op("act", lambda e: e.activation(out=dst, in_=src, func=AF.Exp, scale=-1.0), reads=reads, writes=writes)
                k.op("act", lambda e: e.activation(out=dst, in_=dst, func=AF.Ln, bias=1.0), writes=writes)
                k.op("dve", lambda e: e.tensor_scalar(out=dst, in0=dst, scalar1=-1.0, scalar2=None, op0=ALU.mult), writes=writes)

            SM = sb("SM", [128, NT, 2, 5, 2], F32, ms)
            SM_d = Dep()
            EGLD = sb("EGLD", [128, NT, 2], F32, ms)
            EGLD_d = Dep()
            KG = [sb("KG", [128, NT, 128], BF16, ms) for _ in range(2)]
            KG_d = [Dep() for _ in range(2)]
            _kg0 = KG[0][:].rearrange("p c e -> p (c e)").bitcast(F32)
            _kg1 = KG[1][:].rearrange("p c e -> p (c e)")
            egw = [Rot([(_kg0[:, (2 * d_ + i_) * 256:(2 * d_ + i_ + 1) * 256], Dep()) for i_ in range(2)]) for d_ in range(2)]
            qgw = [Rot([(_kg1[0:64, (3 * d_ + i_) * 384:(3 * d_ + i_ + 1) * 384], Dep()) for i_ in range(3)]) for d_ in range(2)]

            def gla_zero_qg():
                for d_ in range(2):
                    for (t_, dd_) in qgw[d_].items:
                        k.op("pool", lambda e, t_=t_: e.memset(t_, 0.0), writes=[dd_])

            def scalar_pre(shared_qk):
                for d in range(2):
                    pa, pad = PA.next()
                    la_all = lai[:, :, d, 0, :]
                    ig_all = lai[:, :, d, 1, :]
                    for i_, L_ in enumerate([TRI[d], STR[d], ONEF]):
                        k.op("pe", lambda e, i_=i_, L_=L_: e.matmul(pa[:, 36 * i_:36 * i_ + 36], lhsT=L_, rhs=la_all, start=True, stop=True),
                             reads=lai_d + [cdep], writes=[pad])
                    v3 = lambda i_: pa[:, 36 * i_:36 * i_ + 36].rearrange("p (c h) -> p c h", h=2)
                    k.op("dve", lambda e: e.tensor_tensor(out=SM[:, :, d, 0, :], in0=ig_all, in1=v3(0), op=ALU.subtract), reads=[pad] + lai_d, writes=[SM_d])
                    k.op("dve", lambda e: e.tensor_tensor(out=SM[:, :, d, 4, :], in0=ig_all, in1=v3(1), op=ALU.add), reads=[pad] + lai_d, writes=[SM_d])
                    k.op("act", lambda e: e.activation(out=SM[:, :, d, 1, :], in_=v3(0), func=AF.Exp), reads=[pad], writes=[SM_d])
                    k.op("act", lambda e: e.activation(out=SM[:, :, d, 2, :], in_=SM[:, :, d, 4, :], func=AF.Exp), writes=[SM_d])
                    k.op("act", lambda e: e.activation(out=SM[:, :, d, 3, :], in_=v3(2), func=AF.Exp), reads=[pad], writes=[SM_d])
                    if not shared_qk:
                        for h in range(2):
                            r_ = slice(64 * h, 64 * h + 64)
                            k.op("pool", lambda e, h=h, r_=r_: e.tensor_copy(out=EGLD[r_, :, d], in_=SM[r_, :, d, 3, h]), reads=[SM_d], writes=[EGLD_d])
                    if shared_qk:
                        kin = ktok[:, :, 0:64].unsqueeze(2).to_broadcast([128, NT, 2, 64])
                    else:
                        kin = ktok[:].rearrange("p c (h e) -> p c h e", h=2)
                    k.op("pool", lambda e, kin=kin: e.tensor_tensor(out=KG[d][:].rearrange("p c (h e) -> p c h e", h=2), in0=kin,
                                                                    in1=SM[:, :, d, 2, :].unsqueeze(3).to_broadcast([128, NT, 2, 64]), op=ALU.mult),
                         reads=ktok_d + [SM_d], writes=[KG_d[d]])

            xdeps = {}

            def xd(t, i):
                key = (id(t), i)
                if key not in xdeps:
                    xdeps[key] = Dep()
                return xdeps[key]

            kvpar = [0, 0]

            def scalar_P(d, c, rows, shared_qk, res, with_out=True):
                A, Ad = psum[2 + d], pdep[2 + d]
                pgb = A[:, 0:256]
                pss = A[:, 256:512]
                kvo = 130 * (kvpar[d] % 3)
                kvpar[d] += 1
                kv = psum[d][:, kvo:kvo + 130]
                res["kv"] = kv
                for h in range(2):
                    r_ = rows(h)
                    k.op("pe", lambda e, h=h, r_=r_: e.matmul(kv[r_, h * 65:(h + 1) * 65], lhsT=KG[d][:, c, h * 64:(h + 1) * 64], rhs=vtok[:, c, h, :],
                                                               start=True, stop=True), reads=[KG_d[d], vtok_d[c]], writes=[pdep[d]])
                if not with_out:
                    yield
                    return
                for h in range(2):
                    k.op("pe", lambda e, h=h: e.matmul(pgb[:, h * 128:(h + 1) * 128], lhsT=lai[:, c, d, 0, h:h + 1].to_broadcast([128, 128]),
                                                        rhs=TRI[d], start=True, stop=False), reads=[lai_d[c], cdep], writes=[Ad])
                    k.op("pe", lambda e, h=h: e.matmul(pgb[:, h * 128:(h + 1) * 128], lhsT=IDB, rhs=NGB[d], start=False, stop=True), reads=[cdep], writes=[Ad])
                if shared_qk:
                    k.op("pe", lambda e: e.matmul(pss[:, 0:128], lhsT=kT[0:64, tcols(c)], rhs=qT[0:64, tcols(c)], start=True, stop=True),
                         reads=[kT_d[c], qT_d[c]], writes=[Ad])
                else:
                    k.op("pe", lambda e: e.matmul(pss, lhsT=kT[:, tcols(c)], rhs=qTx[:, :, tcols(c)], start=True, stop=True),
                         reads=[kT_d[c], qT_d[c], qT_d2[c]], writes=[Ad])
                yield
                Dt, Dd = Dw[d].next()
                Dd2 = xd(Dt, 2)
                for h in range(2):
                    k.op("act", lambda e, h=h: e.activation(out=Dt[:, h * 128:(h + 1) * 128], in_=pgb[:, h * 128:(h + 1) * 128], func=AF.Exp,
                                                             bias=SM[:, c, d, 0, h:h + 1]), reads=[Ad, SM_d], writes=[Dd if h == 0 else Dd2])
                yield
                Pt, Pd = PTw[d].next()
                if shared_qk:
                    k.op("dve", lambda e: e.tensor_tensor(out=Pt[:].rearrange("p (h i) -> p h i", h=2),
                                                          in0=pss[:, 0:128].unsqueeze(1).to_broadcast([128, 2, 128]),
                                                          in1=Dt[:].rearrange("p (h i) -> p h i", h=2), op=ALU.mult), reads=[Ad, Dd, Dd2], writes=[Pd])
                else:
                    k.op("dve", lambda e: e.tensor_tensor(out=Pt[:], in0=pss, in1=Dt[:], op=ALU.mult), reads=[Ad, Dd, Dd2], writes=[Pd])
                res["P"] = (Pt, Pd)
                yield

            def scalar_S(d, c, rows, shared_qk, first, with_out, res, oa=None):
                oacc_, oacc_d_ = oa if oa is not None else (oacc, oacc_d)
                b1, b1d = psBC[d][:, 0:512], pdep[4 + 2 * d]
                b2, b2d = psum[d], pdep[d]
                po_a = b1[:, 0:130]
                po_b = b1[:, 130:260]
                kv = res["kv"]
                if with_out:
                    Pt, Pd = res["P"]
                    for h in range(2):
                        k.op("pe", lambda e, h=h: e.matmul(po_a[:, h * 65:(h + 1) * 65], lhsT=Pt[:, h * 128:(h + 1) * 128], rhs=vtok[:, c, h, :],
                                                            start=True, stop=True), reads=[Pd, vtok_d[c]], writes=[b1d])
                    if shared_qk:
                        k.op("pe", lambda e: e.matmul(po_b, lhsT=qT[0:64, tcols(c)], rhs=Sbf[d][0:64].rearrange("p h e -> p (h e)"), start=True, stop=True),
                             reads=[qT_d[c], Sbf_d[d]], writes=[b1d])
                    else:
                        for h in range(2):
                            k.op("pe", lambda e, h=h: e.matmul(po_b, lhsT=qTx[:, h, tcols(c)], rhs=Sbf[d][:].rearrange("p h e -> p (h e)"), start=(h == 0), stop=(h == 1)),
                                 reads=[qT_d[c], qT_d2[c], Sbf_d[d]], writes=[b1d])
                yield
                if shared_qk:
                    for h in range(2):
                        r_ = rows(h)
                        k.op("dve", lambda e, h=h, r_=r_: e.scalar_tensor_tensor(out=S32[d][r_, h, :], in0=S32[d][r_, h, :], scalar=SM[r_, c, d, 3, h:h + 1],
                                                                                   in1=kv[r_, h * 65:(h + 1) * 65], op0=ALU.mult, op1=ALU.add),
                             reads=[b2d, SM_d], writes=[S_dh[d][h]])
                else:
                    k.op("dve", lambda e: e.scalar_tensor_tensor(out=S32[d][:].rearrange("p h e -> p (h e)"), in0=S32[d][:].rearrange("p h e -> p (h e)"),
                                                                 scalar=EGLD[:, c, d:d + 1], in1=kv, op0=ALU.mult, op1=ALU.add),
                         reads=[b2d, EGLD_d], writes=S_dh[d])
                if with_out:
                    t1, t1d = t1w[d].next()
                    t1d2 = xd(t1, 2)
                    k.op("dve", lambda e: e.tensor_tensor(out=t1[:], in0=po_b.rearrange("p (h e) -> p h e", h=2),
                                                          in1=SM[:, c, d, 1, :].unsqueeze(2).to_broadcast([128, 2, 65]), op=ALU.mult),
                         reads=[b1d, SM_d], writes=[t1d])
                yield
                k.op("act", lambda e: e.copy(out=Sbf[d][:], in_=S32[d][:]), reads=S_dh[d], writes=[Sbf_d[d]])
                if with_out:
                    if not first:
                        k.op("pool", lambda e: e.tensor_tensor(out=t1[:], in0=t1[:], in1=oacc_[:, c], op=ALU.add), reads=[oacc_d_[c], t1d2], writes=[t1d])
                    yield
                    k.op("dve", lambda e: e.tensor_tensor(out=oacc_[:, c], in0=po_a.rearrange("p (h e) -> p h e", h=2), in1=t1[:], op=ALU.add),
                         reads=[b1d, t1d, t1d2], writes=[oacc_d_[c]])
                yield

            def gla_P(d, c, res):
                la_c = lag[:, c, d, :]
                A, Ad = psum[2 + d], pdep[2 + d]
                b2, b2d = psBC[d][:, 512:1024], pdep[5 + 2 * d]
                pgt = A[0:64, 0:128]
                pss = A[:, 256:512]
                pga = b2[:, 0:64]
                eg, egd = egw[d].next()
                k.op("pe", lambda e: e.matmul(pgt, lhsT=la_c, rhs=TRI[d], start=True, stop=True), reads=[lag_d[c], cdep], writes=[Ad])
                k.op("pe", lambda e: e.matmul(pga, lhsT=STR[d], rhs=la_c, start=True, stop=True), reads=[lag_d[c], cdep], writes=[b2d])
                yield
                egd2 = xd(eg, 2)
                k.op("act", lambda e: e.activation(out=eg[0:64, 0:128], in_=pgt, func=AF.Exp), reads=[Ad], writes=[egd])
                k.op("act", lambda e: e.activation(out=eg[0:64, 128:256], in_=pgt, func=AF.Exp, scale=-1.0), reads=[Ad], writes=[egd2])
                gw_, gwd_ = gww[d].next()
                k.op("act", lambda e: e.activation(out=gw_[:, 0:64], in_=pga, func=AF.Exp), reads=[b2d], writes=[gwd_])
                el, eld = eglw[d].next()
                last = 127 if d == 0 else 0
                k.op("act", lambda e: e.activation(out=el[:, 0:1], in_=pgt[:, last:last + 1], func=AF.Exp), reads=[Ad], writes=[eld])
                yield
                qg, qgd = qgw[d].next()
                qgd2 = xd(qg, 2)
                for h in range(2):
                    r_ = slice(32 * h, 32 * h + 32)
                    k.op("dve", lambda e, h=h, r_=r_: e.tensor_tensor(out=qg[r_, 128 * (1 + h):128 * (2 + h)], in0=qT[r_, tcols(c)], in1=eg[r_, 0:128], op=ALU.mult),
                         reads=[qT_d[c], egd], writes=[qgd if h == 0 else qgd2])
                qgd3 = xd(qg, 3)
                k.op("pool", lambda e: e.tensor_tensor(out=qg[0:64, 0:128], in0=kT[0:64, tcols(c)], in1=eg[0:64, 128:256], op=ALU.mult),
                     reads=[kT_d[c], egd2], writes=[qgd3])
                kg, kgd = kgw[d].next()
                k.op("pool", lambda e: e.tensor_tensor(out=kg[:, 0:64], in0=ktok[:, c, 0:64], in1=gw_[:, 0:64], op=ALU.mult),
                     reads=[ktok_d[c], gwd_], writes=[kgd])
                yield
                k.op("pe", lambda e: e.matmul(pss, lhsT=qg[0:64, 0:128], rhs=qg[0:64, 128:384], start=True, stop=True), reads=[qgd, qgd2, qgd3], writes=[Ad])
                kvo = 130 * (kvpar[d] % 3)
                kvpar[d] += 1
                kv = psum[d][0:64, kvo:kvo + 130]
                res["kv"] = kv
                k.op("pe", lambda e: e.matmul(kv, lhsT=kg[:, 0:64], rhs=vtok[:, c].rearrange("p h e -> p (h e)"), start=True, stop=True),
                     reads=[kgd, vtok_d[c]], writes=[pdep[d]])
                yield
                Pt, Pd = PTw[d].next()
                k.op("dve", lambda e: e.tensor_tensor(out=Pt[:], in0=pss, in1=MSK[d], op=ALU.mult), reads=[Ad, cdep], writes=[Pd])
                res["P"] = (Pt, Pd, qg, [qgd, qgd2], kg, kgd, el, eld)
                yield

            def gla_S(d, c, first, with_out, res):
                Pt, Pd, qg, qgds, kg, kgd, eg, egd = res["P"]
                b1, b1d = psBC[d][:, 0:512], pdep[4 + 2 * d]
                b2, b2d = psum[d], pdep[d]
                po = b1[:, 0:130]
                kv = res["kv"]
                if with_out:
                    for h in range(2):
                        k.op("pe", lambda e, h=h: e.matmul(po[:, h * 65:(h + 1) * 65], lhsT=Pt[:, h * 128:(h + 1) * 128], rhs=vtok[:, c, h, :],
                                                            start=True, stop=False), reads=[Pd, vtok_d[c]], writes=[b1d])
                        k.op("pe", lambda e, h=h: e.matmul(po[:, h * 65:(h + 1) * 65], lhsT=qg[0:64, 128 * (1 + h):128 * (2 + h)], rhs=Sbf[d][0:64, h, :],
                                                            start=False, stop=True), reads=qgds + [Sbf_d[d]], writes=[b1d])
                yield
                last = 127 if d == 0 else 0
                for h in range(2):
                    r_ = slice(32 * h, 32 * h + 32)
                    k.op("dve", lambda e, h=h, r_=r_: e.scalar_tensor_tensor(out=S32[d][r_, h, :], in0=S32[d][r_, h, :], scalar=eg[r_, 0:1],
                                                                               in1=kv[r_, h * 65:(h + 1) * 65], op0=ALU.mult, op1=ALU.add),
                         reads=[b2d, egd], writes=[S_dh[d][h]])
                yield
                k.op("pool", lambda e: e.tensor_copy(out=Sbf[d][0:64], in_=S32[d][0:64]), reads=S_dh[d], writes=[Sbf_d[d]])
                if with_out:
                    if first:
                        k.op("dve", lambda e: e.tensor_copy(out=oacc[:, c], in_=po.rearrange("p (h e) -> p h e", h=2)), reads=[b1d], writes=[oacc_d[c]])
                    else:
                        k.op("dve", lambda e: e.tensor_tensor(out=oacc[:, c], in0=po.rearrange("p (h e) -> p h e", h=2), in1=oacc[:, c], op=ALU.add),
                             reads=[b1d], writes=[oacc_d[c]])
                yield

            gww = [rot("gww", 2, [128, 64], F32, ms) for _ in range(2)]
            eglw = [rot("eglw", 4, [64, 2], F32, ms) for _ in range(2)]

            def run_scan(P_fn, S_fn, sep=False, LA=2):
                for d in range(2):
                    k.op("dve", lambda e, d=d: e.memset(S32[d][:], 0.0), writes=S_dh[d])
                    k.op("pool", lambda e, d=d: e.memset(Sbf[d][:], 0.0), writes=[Sbf_d[d]])
                    k.op("dve", lambda e, d=d: e.memset(psum[d][:, 0:390], 0.0), writes=[pdep[d]])
                    kvpar[d] = 0
                order = [[0, 1] + list(range(2, NT)), [1, 0] + list(range(NT - 1, 1, -1))]
                seen = set()
                resP = [dict(), dict()]
                for i in range(NT + LA):
                    gens = []
                    for d in range(2):
                        if i < NT:
                            c = order[d][i]
                            wo = ctx_out or c >= 2
                            resP[d][i] = {}
                            gens.append(P_fn(d, c, resP[d][i], wo))
                    for d in range(2):
                        j = i - LA
                        if j >= 0:
                            c = order[d][j]
                            wo = ctx_out or c >= 2
                            first = sep or (c not in seen)
                            if wo:
                                seen.add(c)
                            gens.append(S_fn(d, c, first, wo, resP[d].pop(j)))
                    alive = list(gens)
                    while alive:
                        nxt = []
                        for g in alive:
                            try:
                                next(g)
                                nxt.append(g)
                            except StopIteration:
                                pass
                        alive = nxt

            def head_rms(o3, o3_reads, c, center, nw_off, gate_ap, gate_reads, dst, dst_writes, gid=None):
                sm, smd = fsm.next(gid)
                f1, f1d = fin.next(gid)
                if center:
                    k.op("dve", lambda e: e.tensor_reduce(out=sm[:, 0:2], in_=o3, axis=AX.X, op=ALU.add), reads=o3_reads, writes=[smd])
                    k.op("dve", lambda e: e.tensor_scalar(out=sm[:, 0:2], in0=sm[:, 0:2], scalar1=-1.0 / 64, scalar2=None, op0=ALU.mult), writes=[smd])
                    yield
                    k.op("dve", lambda e: e.tensor_tensor(out=f1[:], in0=o3, in1=sm[:, 0:2].unsqueeze(2).to_broadcast([128, 2, 64]), op=ALU.add),
                         reads=o3_reads + [smd], writes=[f1d])
                else:
                    k.op("pool", lambda e: e.tensor_copy(out=f1[:], in_=o3), reads=o3_reads, writes=[f1d])
                f2, f2d = fin2.next(gid)
                k.op("act", lambda e: e.activation(out=f2[:].rearrange("p h e -> p (h e)"), in_=f1[:].rearrange("p h e -> p (h e)"), func=AF.Square), reads=[f1d], writes=[f2d])
                yield
                k.op("dve", lambda e: e.tensor_reduce(out=sm[:, 2:4], in_=f2[:], axis=AX.X, op=ALU.add), reads=[f2d], writes=[smd])
                yield
                k.op("act", lambda e: e.activation(out=sm[:, 4:6], in_=sm[:, 2:4], func=AF.Sqrt, scale=1.0 / 64, bias=EPS), writes=[smd])
                yield
                k.op("dve", lambda e: e.reciprocal(out=sm[:, 6:8], in_=sm[:, 4:6]), writes=[smd])
                k.op("pool", lambda e: e.tensor_tensor(out=f2[:], in0=gate_ap.rearrange("p (h e) -> p h e", h=2), in1=nwb[:, nw_off:nw_off + 128].rearrange("p (h e) -> p h e", h=2), op=ALU.mult),
                     reads=[nwb_d] + gate_reads, writes=[f2d])
                yield
                k.op("dve", lambda e: e.tensor_tensor(out=f1[:], in0=f1[:], in1=sm[:, 6:8].unsqueeze(2).to_broadcast([128, 2, 64]), op=ALU.mult),
                     reads=[smd], writes=[f1d])
                yield
                k.op("dve", lambda e: e.tensor_tensor(out=dst.rearrange("p (h e) -> p h e", h=2), in0=f1[:], in1=f2[:], op=ALU.mult),
                     reads=[f1d, f2d], writes=dst_writes)
                yield

            def y_to_yT(src, src_d, chunk, c):
                pt, ptd = PT_.next()
                ptb = pt[:].bitcast(BF16)
                k.op("pe", lambda e: e.transpose(ptb[:, 0:128], src, IDB), reads=[src_d, cdep], writes=[ptd])
                k.op("act", lambda e: e.copy(out=yT[:, chunk, tcols(c)], in_=ptb[:, 0:128]), reads=[ptd], writes=[yT_d[c]])

            def prep_qkvg(mix, hp, qc, kc, vc, gc, gate_func, extra_cols, extra_cb=None):
                wt, wd, ncols = load_wblk([(qc, 128), (kc, 128), (vc, 128), (gc, 128)] + extra_cols)

                def tile(c, gid):
                    pa, pad = PA4.next(gid)
                    inproj_tok(wt, wd, 512, c, pa, pad)
                    if extra_cols:
                        pe_, ped = PT_.next()
                        inproj_tok(wt, wd, ncols - 512, c, pe_, ped, off=0, w0=512)
                        extra_cb(c, pe_, ped)
                    yield
                    ut, ud = usb.next(gid)
                    k.op("act", lambda e: e.copy(out=ut[:], in_=pa[:]), reads=[pad], writes=[ud])
                    rt, rd = ropep.next(gid)
                    k.dma("sp", rt[:], rope_d[mix, c], writes=[rd])
                    yield
                    qk, qkd = qkp.next(gid)
                    if mix == 0 and c >= 2:
                        t1, t1d = t1p.next(gid)
                        t2, t2d = t2p.next(gid)
                        k.op("dve", lambda e: e.tensor_tensor(out=t1[:], in0=ut[:, 0:256], in1=rt[:, 0, :], op=ALU.mult),
                             reads=[ud, rd], writes=[t1d])
                        u4 = ut[:, 0:256].rearrange("p (g b e) -> p g b e", b=2, e=16)
                        s4 = rt[:, 1, :].rearrange("p (g b e) -> p g b e", b=2, e=16)
                        o4 = t2[:].rearrange("p (g b e) -> p g b e", b=2, e=16)
                        k.op("pool", lambda e: e.tensor_tensor(out=o4[:, :, 0, :], in0=u4[:, :, 1, :], in1=s4[:, :, 0, :], op=ALU.mult),
                             reads=[ud, rd], writes=[t2d])
                        k.op("pool", lambda e: e.tensor_tensor(out=o4[:, :, 1, :], in0=u4[:, :, 0, :], in1=s4[:, :, 1, :], op=ALU.mult),
                             reads=[ud, rd], writes=[t2d])
                        yield
                        k.op("dve", lambda e: e.tensor_tensor(out=qk[:], in0=t1[:], in1=t2[:], op=ALU.add),
                             reads=[t1d, t2d], writes=[qkd])
                    else:
                        k.op("dve", lambda e: e.tensor_tensor(out=qk[:], in0=ut[:, 0:256], in1=rt[:, 0, :], op=ALU.mult),
                             reads=[ud, rd], writes=[qkd])
                    k.op("pool", lambda e: e.tensor_copy(out=vtok[:, c, :, 0:64], in_=ut[:, 256:384].rearrange("p (h e) -> p h e", h=2)),
                         reads=[ud], writes=[vtok_d[c]])
                    k.op("act", lambda e: e.activation(out=gate[:, c, :], in_=ut[:, 384:512], func=gate_func), reads=[ud], writes=[gate_d[c]])
                    yield
                    k.op("pool", lambda e: e.tensor_copy(out=ktok[:, c, :], in_=qk[:, 128:256]), reads=[qkd], writes=[ktok_d[c]])
                    ptq, ptqd = PT_.next()
                    ptqb = ptq[:].bitcast(BF16)
                    k.op("pe", lambda e: e.transpose(ptqb[:, 0:128], qk[:, 0:128], IDB), reads=[qkd, cdep], writes=[ptqd])
                    k.op("pe", lambda e: e.transpose(ptqb[:, 128:256], qk[:, 128:256], IDB), reads=[qkd, cdep], writes=[ptqd])
                    k.op("act", lambda e: e.copy(out=qTx[0:64, 0, tcols(c)], in_=ptqb[0:64, 0:128]), reads=[ptqd], writes=[qT_d[c]])
                    k.op("act", lambda e: e.copy(out=qTx[64:128, 1, tcols(c)], in_=ptqb[64:128, 0:128]), reads=[ptqd], writes=[qT_d2[c]])
                    k.op("act", lambda e: e.copy(out=kT[:, tcols(c)], in_=ptqb[:, 128:256]), reads=[ptqd], writes=[kT_d[c]])
                    yield

                return [(lambda gid, c=c: tile(c, gid)) for c in range(NT)]

            fin_pending = set()

            def merged_run(pend, prep):
                merged = []
                fi = 0
                for i, p in enumerate(prep):
                    while fi < len(pend) and fi < i + 3:
                        merged.append(pend[fi])
                        fi += 1
                    merged.append(p)
                merged.extend(pend[fi:])
                run_window(merged, 3 if pend else 2, limits={"prep": 2})

            def guard_prep(facts):
                def mk(c, f):
                    def g(gid):
                        assert c not in fin_pending, "prep tile started before finalize of the same tile finished"
                        return f(gid)
                    g.ready = lambda: c not in fin_pending
                    g.kind = "prep"
                    return g
                return [mk(c, f) for c, f in enumerate(facts)]

            def ret_setup(hp):
                k.dma("sp", prm[:, 0:4].rearrange("p (d h) -> p d h", d=2), ret_logit[l][:, 2 * hp:2 * hp + 2].partition_broadcast(128), writes=[prm_d])
                softplus_neg(prm[:, 0:4], prm[:, 0:4], [prm_d], [prm_d])
                if hp == 0:
                    k.dma("sp", nwb[:], ret_nw[l:l + 1, :].partition_broadcast(128), writes=[nwb_d])
                for c in range(NT):
                    k.op("pool", lambda e, c=c: e.memset(lai[:, c], 0.0), writes=[lai_d[c]])
                    k.op("pool", lambda e, c=c: e.tensor_copy(out=lai[:, c, :, 0, :], in_=prm[:, 0:4].rearrange("p (d h) -> p d h", d=2)),
                         reads=[prm_d], writes=[lai_d[c]])

            def ret_fin(hp):
                def tile(c, gid):
                    fb, fbd = finb.next(gid)
                    yield from head_rms(oacc[:, c, :, 0:64], [oacc_d[c]], c, True, 128 * hp, gate[:, c, :], [gate_d[c]], fb[:], [fbd], gid)
                    y_to_yT(fb[:], fbd, 0 + hp, c)
                    fin_pending.discard(c)
                    yield
                for c in tiles_out:
                    fin_pending.add(c)
                return [(lambda gid, c=c: tile(c, gid)) for c in tiles_out]

            def mixer_ret_all():
                pend = []
                R64 = lambda h: slice(64 * h, 64 * h + 64)
                for hp in range(2):
                    base = 128 * hp
                    ret_setup(hp)
                    if hp == 0:
                        chk("r1")
                    prep = guard_prep(prep_qkvg(0, hp, base, 256 + base, 512 + base, 768 + base, AF.Silu, []))
                    merged_run(pend, prep)
                    if l == 0 and hp == 0:
                        k.barrier()
                        dump("d_qT", qT[:], [128, NTOK], qT_d)
                        dump("d_kT", kT[:], [128, NTOK], kT_d)
                        dump("d_vtok", vtok[:].rearrange("p c h e -> p (c h e)"), [128, NT * 130], vtok_d)
                        dump("d_gate", gate[:].rearrange("p c e -> p (c e)"), [128, NT * 128], gate_d)
                        dump("d_lai", lai[:].rearrange("p c d s h -> p (c d s h)"), [128, NT * 8], lai_d, q="sp")
                        chk("r2")
                    scalar_pre(False)
                    run_scan(lambda d, c, res, wo: scalar_P(d, c, R64, False, res, wo),
                             lambda d, c, first, wo, res: scalar_S(d, c, R64, False, first, wo, res))
                    if l == 0 and hp == 0:
                        k.barrier()
                        dump("d_oacc", oacc[:].rearrange("p c h e -> p (c h e)"), [128, NT * 130], oacc_d, q="sp")
                        chk("r3")
                    pend = ret_fin(hp)
                run_window(pend, 2)

            def mixer_mlstm_all():
                pend = []
                R64 = lambda h: slice(64 * h, 64 * h + 64)
                oacc2 = ygs[:].rearrange("p a b -> p (a b)").bitcast(F32).rearrange("p (c h e) -> p c h e", c=NT, h=2)
                oas = [(oacc, oacc_d), (oacc2, ygs_d)]
                k.dma("sp", nwb[:], ml_nw[l:l + 1, :].partition_broadcast(128), writes=[nwb_d])

                def gates_cb(c, pe_, ped):
                    sm, smd = gsm.next()
                    k.op("dve", lambda e: e.tensor_tensor(out=sm[:, 0:8], in0=pe_[:, 0:8], in1=prm[:, 0:8], op=ALU.add),
                         reads=[ped, prm_d], writes=[smd])
                    g4 = sm[:, 0:8].rearrange("p (d s h) -> p d s h", d=2, s=2)
                    k.op("pool", lambda e: e.tensor_copy(out=lai[:, c, :, 1, :], in_=g4[:, :, 0, :]), reads=[smd], writes=[lai_d[c]])
                    k.op("act", lambda e: e.activation(out=sm[:, 8:12].rearrange("p (d h) -> p d h", d=2), in_=g4[:, :, 1, :], func=AF.Exp, scale=-1.0), writes=[smd])
                    k.op("act", lambda e: e.activation(out=sm[:, 8:12], in_=sm[:, 8:12], func=AF.Ln, bias=1.0), writes=[smd])
                    k.op("dve", lambda e: e.tensor_scalar(out=lai[:, c, :, 0, :], in0=sm[:, 8:12].rearrange("p (d h) -> p d h", d=2), scalar1=-1.0, scalar2=None, op0=ALU.mult),
                         reads=[smd], writes=[lai_d[c]])

                def fin_facts(hp):
                    def tile(c, gid):
                        sm, smd = fsm.next(gid)
                        f0, f0d = fin2.next(gid)
                        for d_, (oa_t, oa_dd) in enumerate(oas):
                            o_ = 4 * d_
                            k.op("dve", lambda e: e.tensor_scalar(out=sm[:, o_ + 2:o_ + 4], in0=oa_t[:, c, :, 64], scalar1=-1.0, scalar2=None, op0=ALU.mult),
                                 reads=[oa_dd[c]], writes=[smd])
                            k.op("dve", lambda e: e.tensor_tensor(out=sm[:, o_:o_ + 2], in0=oa_t[:, c, :, 64], in1=sm[:, o_ + 2:o_ + 4], op=ALU.max),
                                 reads=[oa_dd[c]], writes=[smd])
                            yield
                            k.op("dve", lambda e: e.tensor_scalar(out=sm[:, o_:o_ + 2], in0=sm[:, o_:o_ + 2], scalar1=1.0, scalar2=None, op0=ALU.max), writes=[smd])
                            k.op("dve", lambda e: e.reciprocal(out=sm[:, o_ + 2:o_ + 4], in_=sm[:, o_:o_ + 2]), writes=[smd])
                            yield
                        k.op("dve", lambda e: e.tensor_tensor(out=f0[:], in0=oacc[:, c, :, 0:64], in1=sm[:, 2:4].unsqueeze(2).to_broadcast([128, 2, 64]), op=ALU.mult),
                             reads=[oacc_d[c], smd], writes=[f0d])
                        f9, f9d = fin.next(gid)
                        k.op("dve", lambda e: e.tensor_tensor(out=f9[:], in0=oacc2[:, c, :, 0:64], in1=sm[:, 6:8].unsqueeze(2).to_broadcast([128, 2, 64]), op=ALU.mult),
                             reads=[ygs_d[c], smd], writes=[f9d])
                        yield
                        k.op("pool", lambda e: e.tensor_tensor(out=f0[:], in0=f0[:], in1=f9[:], op=ALU.add), reads=[f9d], writes=[f0d])
                        yield
                        fb, fbd = finb.next(gid)
                        yield from head_rms(f0[:], [f0d], c, False, 128 * hp, gate[:, c, :], [gate_d[c]], fb[:], [fbd], gid)
                        y_to_yT(fb[:], fbd, 6 + hp, c)
                        fin_pending.discard(c)
                        yield
                    for c in tiles_out:
                        fin_pending.add(c)
                    return [(lambda gid, c=c: tile(c, gid)) for c in tiles_out]

                for hp in range(2):
                    base = 2584 + 128 * hp
                    gcols = [(3608 + d * 8 + s_ * 4 + 2 * hp, 2) for d in range(2) for s_ in range(2)]
                    k.dma("sp", prm[:, 0:8].rearrange("p (d s h) -> p d s h", d=2, s=2), ml_gb[l][:, :, 2 * hp:2 * hp + 2].partition_broadcast(128), writes=[prm_d])
                    prep = guard_prep(prep_qkvg(1, hp, base, 256 + base, 512 + base, 768 + base, AF.Sigmoid, gcols, gates_cb))
                    merged_run(pend, prep)
                    scalar_pre(False)
                    run_scan(lambda d, c, res, wo: scalar_P(d, c, R64, False, res, wo),
                             lambda d, c, first, wo, res: scalar_S(d, c, R64, False, True, wo, res, oa=oas[d]), sep=True)
                    pend = fin_facts(hp)
                run_window(pend, 2)

            gsm = rot("gsm", 4, [128, 12], F32, ms)

            def mixer_gla_all():
                gla_zero_qg()
                k.dma("sp", nwb[:], gla_nw[l:l + 1, :].partition_broadcast(128), writes=[nwb_d])
                pend = []
                for hp in range(2):
                    wt, wd, ncols = load_wblk([(1024 + 64 * hp, 64), (1152 + 64 * hp, 64), (1280 + 128 * hp, 128), (1536 + 128 * hp, 128), (1792, 16)])
                    k.op("dve", lambda e: e.memset(prm32[:], 0.0), writes=[prm32_d])
                    for d in range(2):
                        k.dma("sp", prm32[0:16, d * 64:(d + 1) * 64], gla_gw[l, d][:, 64 * hp:64 * hp + 64], writes=[prm32_d])
                        k.dma("sp", prm32[16:17, d * 64:(d + 1) * 64], gla_gb[l, d:d + 1, 64 * hp:64 * hp + 64], writes=[prm32_d])

                    def ptile(c, gid, wt=wt, wd=wd, ncols=ncols):
                        pa, pad = PA4.next(gid)
                        inproj_tok(wt, wd, ncols, c, pa, pad)
                        yield
                        ut, ud = usb.next(gid)
                        k.op("act", lambda e: e.copy(out=ut[:, 0:400], in_=pa[:, 0:400]), reads=[pad], writes=[ud])
                        yield
                        qk, qkd = qkp.next(gid)
                        k.op("dve", lambda e: e.tensor_scalar(out=qk[:, 0:64], in0=ut[:, 0:64], scalar1=32.0 ** -0.5, scalar2=None, op0=ALU.mult),
                             reads=[ud], writes=[qkd])
                        k.op("pool", lambda e: e.tensor_copy(out=ktok[:, c, 0:64], in_=ut[:, 64:128]), reads=[ud], writes=[ktok_d[c]])
                        k.op("pool", lambda e: e.tensor_copy(out=vtok[:, c, :, 0:64], in_=ut[:, 128:256].rearrange("p (h e) -> p h e", h=2)),
                             reads=[ud], writes=[vtok_d[c]])
                        k.op("act", lambda e: e.activation(out=gate[:, c, :], in_=ut[:, 256:384], func=AF.Silu), reads=[ud], writes=[gate_d[c]])
                        pt, ptd = PT_.next()
                        k.op("pe", lambda e: e.transpose(pt[0:16, 0:128], ut[:, 384:400], IDF), reads=[ud, cdep], writes=[ptd])
                        lt, ltd = lrp.next(gid)
                        k.op("pool", lambda e: e.memset(lt[:], 1.0), writes=[ltd])
                        k.op("act", lambda e: e.copy(out=lt[0:16, :], in_=pt[0:16, 0:128]), reads=[ptd], writes=[ltd])
                        yield
                        k.op("dve", lambda e: e.tensor_copy(out=qk[:, 64:128], in_=ktok[:, c, 0:64]), reads=[ktok_d[c]], writes=[qkd])
                        pz, pzd = PT_.next()
                        k.op("pe", lambda e: e.matmul(pz[:, 0:128], lhsT=lt[:], rhs=prm32[:, 0:128], start=True, stop=True),
                             reads=[ltd, prm32_d], writes=[pzd])
                        dst = lag[:, c].rearrange("p d e -> p (d e)")
                        k.op("act", lambda e: e.activation(out=dst, in_=pz[:, 0:128], func=AF.Exp, scale=-1.0), reads=[pzd], writes=[lag_d[c]])
                        yield
                        k.op("act", lambda e: e.activation(out=dst, in_=dst, func=AF.Ln, bias=1.0), writes=[lag_d[c]])
                        ptq, ptqd = PT_.next()
                        ptqb = ptq[:].bitcast(BF16)
                        k.op("pe", lambda e: e.transpose(ptqb[0:64, 0:128], qk[:, 0:64], IDB), reads=[qkd, cdep], writes=[ptqd])
                        k.op("pe", lambda e: e.transpose(ptqb[0:64, 128:256], qk[:, 64:128], IDB), reads=[qkd, cdep], writes=[ptqd])
                        k.op("act", lambda e: e.copy(out=qT[0:64, tcols(c)], in_=ptqb[0:64, 0:128]), reads=[ptqd], writes=[qT_d[c]])
                        k.op("act", lambda e: e.copy(out=kT[0:64, tcols(c)], in_=ptqb[0:64, 128:256]), reads=[ptqd], writes=[kT_d[c]])
                        yield
                        k.op("dve", lambda e: e.tensor_scalar(out=dst, in0=dst, scalar1=-1.0 / 16.0, scalar2=None, op0=ALU.mult), writes=[lag_d[c]])
                        yield

                    prep = guard_prep([(lambda gid, c=c, ptile=ptile: ptile(c, gid)) for c in range(NT)])
                    merged_run(pend, prep)
                    run_scan(lambda d, c, res, wo: gla_P(d, c, res), gla_S)

                    def ftile(c, gid, hp=hp):
                        fb, fbd = finb.next(gid)
                        yield from head_rms(oacc[:, c, :, 0:64], [oacc_d[c]], c, False, 128 * hp, gate[:, c, :], [gate_d[c]], fb[:], [fbd], gid)
                        y_to_yT(fb[:], fbd, 2 + hp, c)
                        fin_pending.discard(c)
                        yield
                    for c in tiles_out:
                        fin_pending.add(c)
                    pend = [(lambda gid, c=c, ftile=ftile: ftile(c, gid)) for c in tiles_out]
                run_window(pend, 2)

            prm32 = sb("prm32", [32, 128], F32, ms)
            prm32_d = Dep()
            lrp = rot("lrT", 2, [32, 128], F32, ms)
            cvw = sb("cvw", [128, 8], F32, ms)
            cvw_d = Dep()
            accp = Rot([(shr4[:, 0:512], Dep()), (shr4[:, 512:1024], Dep())])

            def mixer_ssd(g):
                cols = [(1808 + 128 * g, 128)] + [(2576 + 4 * d + 2 * g, 2) for d in range(2)]
                wt, wd, ncols = load_wblk(cols)
                k.dma("sp", prm[:, 0:4].rearrange("p (d h) -> p d h", d=2), ssd_dtb[l][:, 2 * g:2 * g + 2].partition_broadcast(128), writes=[prm_d])
                k.dma("sp", prm[:, 4:8].rearrange("p (d h) -> p d h", d=2), ssd_alog[l][:, 2 * g:2 * g + 2].partition_broadcast(128), writes=[prm_d])
                k.dma("sp", prm[:, 8:10], ssd_dsk[l:l + 1, 2 * g:2 * g + 2].partition_broadcast(128), writes=[prm_d])
                k.op("act", lambda e: e.activation(out=prm[:, 4:8], in_=prm[:, 4:8], func=AF.Exp), reads=[prm_d], writes=[prm_d])
                k.op("dve", lambda e: e.tensor_scalar(out=prm[:, 4:8], in0=prm[:, 4:8], scalar1=-1.0, scalar2=None, op0=ALU.mult), writes=[prm_d])
                for c in range(NT):
                    pa, pad = PA.next()
                    inproj_tok(wt, wd, ncols, c, pa, pad)
                    k.op("act", lambda e, pa=pa, c=c: e.activation(out=gate[:, c, :], in_=pa[:, 0:128], func=AF.Silu), reads=[pad], writes=[gate_d[c]])
                    sm, smd = fsm.next()
                    k.op("dve", lambda e, sm=sm, pa=pa: e.tensor_tensor(out=sm[:, 0:4], in0=pa[:, 128:132], in1=prm[:, 0:4], op=ALU.add),
                         reads=[pad, prm_d], writes=[smd])
                    k.op("act", lambda e, sm=sm: e.activation(out=sm[:, 0:4], in_=sm[:, 0:4], func=AF.Exp), writes=[smd])
                    k.op("act", lambda e, sm=sm: e.activation(out=sm[:, 0:4], in_=sm[:, 0:4], func=AF.Ln, bias=1.0), writes=[smd])
                    k.op("act", lambda e, sm=sm, c=c: e.activation(out=lai[:, c, :, 1, :], in_=sm[:, 0:4].rearrange("p (d h) -> p d h", d=2), func=AF.Ln),
                         reads=[smd], writes=[lai_d[c]])
                    k.op("dve", lambda e, sm=sm, c=c: e.tensor_tensor(out=lai[:, c, :, 0, :], in0=sm[:, 0:4].rearrange("p (d h) -> p d h", d=2),
                                                                       in1=prm[:, 4:8].rearrange("p (d h) -> p d h", d=2), op=ALU.mult),
                         reads=[smd, prm_d], writes=[lai_d[c]])
                blocks = [(2064 + 128 * g, 128, 128 * g, "x"), (2320 + 64 * g, 64, 256 + 64 * g, "B"), (2448 + 64 * g, 64, 384 + 64 * g, "C")]
                XcT = qgw_x
                for (c0, M, ch0, nm) in blocks:
                    wt, wd, _ = load_wblk([(c0, M)])
                    k.dma("sp", cvw[0:M, 0:5], ssd_cw[l][:, ch0:ch0 + M].rearrange("w c -> c w"), writes=[cvw_d], allow_slow_non_contiguous=True)
                    k.dma("sp", cvw[0:M, 5:6], ssd_cb[l:l + 1, ch0:ch0 + M].rearrange("o c -> c o"), writes=[cvw_d], allow_slow_non_contiguous=True)
                    k.op("dve", lambda e: e.memset(aux[:], 0.0), writes=[aux_d])
                    pieces = [(0, 256)] + [(256 + 512 * i, 512) for i in range(4)]
                    for (t0, n) in pieces:
                        pa, pad = PA.next()
                        for kk in range(8):
                            k.op("pe", lambda e, kk=kk, pa=pa, t0=t0, n=n, M=M, wt=wt: e.matmul(pa[0:M, 0:n], lhsT=wt[:, kk, 0:M], rhs=hmodT[:, kk, t0:t0 + n],
                                                                                            start=(kk == 0), stop=(kk == 7)),
                                 reads=[wd] + hmodT_d, writes=[pad])
                        o = t0 + 2 if t0 < 256 else t0 + 6
                        k.op("act", lambda e, pa=pa, o=o, n=n, M=M: e.copy(out=aux[0:M, o:o + n], in_=pa[0:M, 0:n]), reads=[pad], writes=[aux_d])
                    for (t0, n) in pieces:
                        o = t0 if t0 < 256 else t0 + 4
                        ac, acd = accp.next()
                        k.op("dve", lambda e, ac=ac, o=o, n=n, M=M: e.tensor_scalar(out=ac[0:M, 0:n], in0=aux[0:M, o:o + n], scalar1=cvw[0:M, 0:1], scalar2=None, op0=ALU.mult),
                             reads=[aux_d, cvw_d], writes=[acd])
                        for w in range(1, 5):
                            k.op("dve", lambda e, ac=ac, o=o, n=n, M=M, w=w: e.scalar_tensor_tensor(out=ac[0:M, 0:n], in0=aux[0:M, o + w:o + w + n], scalar=cvw[0:M, w:w + 1],
                                                                                                 in1=ac[0:M, 0:n], op0=ALU.mult, op1=ALU.add),
                                 reads=[aux_d, cvw_d], writes=[acd])
                        dstT, dst_d = {"x": (XcT, XcT_d), "B": (kT, kT_d), "C": (qT, qT_d)}[nm]
                        tl = list(range(t0 // 128, (t0 + n) // 128))
                        k.op("act", lambda e, ac=ac, n=n, M=M, dstT=dstT, t0=t0: e.activation(out=dstT[0:M, t0:t0 + n], in_=ac[0:M, 0:n], func=AF.Silu, bias=cvw[0:M, 5:6]),
                             reads=[acd, cvw_d], writes=[dst_d[c] for c in tl])
                for c in range(NT):
                    pt, ptd = PT_.next()
                    ptb = pt[:].bitcast(BF16)
                    k.op("pe", lambda e, ptb=ptb, c=c: e.transpose(ptb[:, 0:128], XcT[:, tcols(c)], IDB), reads=[XcT_d[c], cdep], writes=[ptd])
                    k.op("pe", lambda e, ptb=ptb, c=c: e.transpose(ptb[:, 128:192], kT[0:64, tcols(c)], IDB[0:64, 0:64]), reads=[kT_d[c], cdep], writes=[ptd])
                    k.op("act", lambda e, ptb=ptb, c=c: e.copy(out=vtok[:, c, :, 0:64], in_=ptb[:, 0:128].rearrange("p (h e) -> p h e", h=2)),
                         reads=[ptd], writes=[vtok_d[c]])
                    k.op("act", lambda e, ptb=ptb, c=c: e.copy(out=ktok[:, c, 0:64], in_=ptb[:, 128:192]), reads=[ptd], writes=[ktok_d[c]])
                scalar_pre(True)
                R0 = lambda h: slice(0, 64)
                run_scan(lambda d, c, res, wo: scalar_P(d, c, R0, True, res, wo),
                         lambda d, c, first, wo, res: scalar_S(d, c, R0, True, first, wo, res))
                for c in tiles_out:
                    f1, f1d = fin.next()
                    k.op("dve", lambda e, f1=f1, c=c: e.tensor_tensor(out=f1[:], in0=vtok[:, c, :, 0:64], in1=prm[:, 8:10].unsqueeze(2).to_broadcast([128, 2, 64]), op=ALU.mult),
                         reads=[vtok_d[c], prm_d], writes=[f1d])
                    k.op("dve", lambda e, f1=f1, c=c: e.tensor_tensor(out=f1[:], in0=f1[:], in1=oacc[:, c, :, 0:64], op=ALU.add), reads=[oacc_d[c]], writes=[f1d])
                    k.op("pool", lambda e, f1=f1, c=c: e.tensor_tensor(out=f1[:], in0=f1[:], in1=gate[:, c, :].rearrange("p (h e) -> p h e", h=2), op=ALU.mult),
                         reads=[gate_d[c]], writes=[f1d])
                    k.op("pool", lambda e, f1=f1, c=c: e.tensor_copy(out=ygs[:, c, 128 * g:128 * g + 128].rearrange("p (h e) -> p h e", h=2), in_=f1[:]),
                         reads=[f1d], writes=[ygs_d[c]])
                    jt, jd = fin2.next()
                    k.op("act", lambda e, f1=f1, jt=jt, c=c: e.activation(out=jt[:].rearrange("p h e -> p (h e)"), in_=f1[:].rearrange("p h e -> p (h e)"), func=AF.Square,
                                                                           accum_out=ssq[:, c, g:g + 1]), reads=[f1d], writes=[jd, ygs_d[c]])

            def ssd_finish():
                k.dma("sp", nwb[:], ssd_nw[l:l + 1, :].partition_broadcast(128), writes=[nwb_d])
                for c in tiles_out:
                    sm, smd = fsm.next()
                    k.op("dve", lambda e, sm=sm, c=c: e.tensor_tensor(out=sm[:, 0:1], in0=ssq[:, c, 0:1], in1=ssq[:, c, 1:2], op=ALU.add), reads=[ygs_d[c]], writes=[smd])
                    k.op("act", lambda e, sm=sm: e.activation(out=sm[:, 1:2], in_=sm[:, 0:1], func=AF.Sqrt, scale=1.0 / 256, bias=EPS), writes=[smd])
                    k.op("dve", lambda e, sm=sm: e.reciprocal(out=sm[:, 2:3], in_=sm[:, 1:2]), writes=[smd])
                    t1, t1d = t1p.next()
                    k.op("dve", lambda e, sm=sm, t1=t1, c=c: e.scalar_tensor_tensor(out=t1[:], in0=ygs[:, c, 0:256], scalar=sm[:, 2:3], in1=nwb[:], op0=ALU.mult, op1=ALU.mult),
                         reads=[ygs_d[c], smd, nwb_d], writes=[t1d])
                    qk, qkd = qkp.next()
                    k.op("pool", lambda e, qk=qk, t1=t1: e.tensor_copy(out=qk[:], in_=t1[:]), reads=[t1d], writes=[qkd])
                    for g in range(2):
                        y_to_yT(qk[:, 128 * g:128 * g + 128], qkd, 4 + g, c)

            qgw_x = sb("XcT", [128, NTOK], BF16, ms)
            XcT_d = [Dep() for _ in range(NT)]

            def dump_yT(tag):
                if l == 0:
                    k.barrier()
                    dump("d_yT_" + tag, yT[:].rearrange("p k t -> p (k t)"), [128, 8 * NTOK], yT_d)
                    chk(tag)
            print("SBUF remaining (mixer phase)", nc.sbuf_bytes_remaining)
            mixer_ret_all()
            k.barrier()
            dump_yT("ret")
            mixer_gla_all()
            k.barrier()
            dump_yT("gla")
            for g in range(2):
                mixer_ssd(g)
            ssd_finish()
            k.barrier()
            dump_yT("ssd")
            mixer_mlstm_all()
            k.barrier()
            dump_yT("mlstm")

            wo_t = hmodT
            wo_d = Dep()
            k.dma("pool", wo_t[:, :, 0:1024], w_out[l].rearrange("(k p) n -> p k n", p=128), writes=[wo_d] + hmodT_d)
            G1b = [aux[:, 0:1024], aux[:, 1024:2048]]
            g1d = Dep()
            for r in range(2 if ctx_out else 1):
                for kk in range(8):
                    pa, pad = PA.next()
                    k.op("pe", lambda e, pa=pa, kk=kk, r=r: e.matmul(pa[:, 0:128], lhsT=modT[:, 16 + kk, r:r + 1].to_broadcast([128, 128]), rhs=IDF,
                                                                      start=True, stop=True), reads=[mod_dep, cdep], writes=[pad])
                    k.op("act", lambda e, pa=pa, kk=kk, r=r: e.copy(out=G1b[r][:, kk * 128:(kk + 1) * 128], in_=pa[:, 0:128]), reads=[pad], writes=[g1d, aux_d])
            ygf = ygs[:].rearrange("p a b -> p (a b)").bitcast(F32)
            hop = Rot([(ygf[:, 0:1024], Dep()), (ygf[:, 1024:2048], Dep())])
            for c in tiles_out:
                r = 1 if c < 2 else 0
                ht, hdp = hop.next()
                k.dma("sp", ht, hsrc(l, c), reads=[hd_dep[c]], writes=[hdp])
                for half in range(2):
                    pa, pad = PA.next()
                    for kk in range(8):
                        k.op("pe", lambda e, pa=pa, kk=kk, half=half, c=c: e.matmul(pa[:], lhsT=yT[:, kk, tcols(c)], rhs=wo_t[:, kk, half * 512:(half + 1) * 512],
                                                                                     start=(kk == 0), stop=(kk == 7)), reads=[yT_d[c], wo_d], writes=[pad])
                    ut, ud = usb.next()
                    k.op("dve", lambda e, ut=ut, pa=pa, r=r, half=half: e.tensor_tensor(out=ut[:], in0=pa[:], in1=G1b[r][:, half * 512:(half + 1) * 512], op=ALU.mult),
                         reads=[pad, g1d], writes=[ud])
                    k.op("pool", lambda e, ut=ut, ht=ht, half=half: e.tensor_tensor(out=ht[:, half * 512:(half + 1) * 512], in0=ht[:, half * 512:(half + 1) * 512], in1=ut[:], op=ALU.add),
                         reads=[ud], writes=[hdp])
                k.dma("sp", hd[c * 128:(c + 1) * 128, :], ht, reads=[hdp], writes=[hd_dep[c]])
            k.barrier()
            if l == 0:
                dump("d_hmid", hd, [NTOK, 1024], hd_dep, q="sp")
                chk("outproj")

        with ExitStack() as es:
            psum = [es.enter_context(nc.psum_tensor(k.name("pm"), [128, 512], F32)) for _ in range(8)]
            pdep = [Dep() for _ in range(8)]
            PG = Rot([(psum[0], pdep[0]), (psum[1], pdep[1])])
            PF = Rot([(psum[2], pdep[2]), (psum[3], pdep[3]), (psum[4], pdep[4]), (psum[5], pdep[5])])
            PC = Rot([(psum[6], pdep[6]), (psum[7], pdep[7])])
            H = sb("H", [128, NT, 1024], F32, es)
            H_d = [Dep() for _ in range(NT)]
            XN = sb("XN", [128, NT, 1024], BF16, es)
            XN_d = [Dep() for _ in range(NT)]
            G2b = sb("G2b", [128, 2, 1024], F32, es)
            g2d = Dep()
            AFF = sb("AFF", [128, NT, 16], F32, es)
            POS = sb("POS", [128, NT, 16], F32, es)
            MKT = sb("MKT", [128, NT, 16], BF16, es)
            aff_d = [Dep() for _ in range(NT)]
            pos_d = Dep()
            mkt_d = Dep()
            POST = sb("POST", [16, NTOK], BF16, es)
            post_d = Dep()
            esel_dep = Dep()
            RW = sb("RW", [128, 8, 16], BF16, es)
            rw_d = Dep()
            k.dma("pool", RW[:], router_w[l].rearrange("(k p) n -> p k n", p=128), writes=[rw_d])
            msm = rot("msm", 2, [128, 8], F32, es)
            XSM = sb("XSM", [128, 8, 288], BF16, es)
            XSM_d = Dep()
            jk2 = Rot([(XSM[:].rearrange("p a b -> p (a b)")[:, 0:1024], XSM_d)])
            tiles = list(range(NT)) if ctx_out else list(range(2, NT))

            for r in range(2 if ctx_out else 1):
                for kk in range(8):
                    pa, pad = PG.next()
                    k.op("pe", lambda e, pa=pa, kk=kk, r=r: e.matmul(pa[:, 0:128], lhsT=modT[:, 40 + kk, r:r + 1].to_broadcast([128, 128]), rhs=IDF,
                                                                      start=True, stop=True), reads=[mod_dep, cdep], writes=[pad])
                    k.op("act", lambda e, pa=pa, kk=kk, r=r: e.copy(out=G2b[:, r, kk * 128:(kk + 1) * 128], in_=pa[:, 0:128]), reads=[pad], writes=[g2d])

            with ExitStack() as rs:
                AFFT = sb("AFFT", [16, NTOK], F32, rs)
                afft_d = Dep()
                CMPB = sb("CMPB", [16, 2048], F32, rs)
                CMPC = sb("CMPC", [16, 256], F32, rs)
                bis = sb("bis", [16, 16], F32, rs)
                bis_d = Dep()
                hm2 = rot("hm2", 2, [128, 8, 128], BF16, rs)
                def r_tile(c, gid):
                    r = 1 if c < 2 else 0
                    k.dma("sp", H[:, c, :], hd[c * 128:(c + 1) * 128, :], reads=[hd_dep[c]], writes=[H_d[c]])
                    st, sd_ = msm.next(gid)
                    jt, jd = jk2.next()
                    yield
                    k.op("act", lambda e: e.activation(out=jt[:], in_=H[:, c, :], func=AF.Square, accum_out=st[:, 0:1]), reads=[H_d[c]], writes=[jd, sd_])
                    yield
                    k.op("act", lambda e: e.activation(out=st[:, 1:2], in_=st[:, 0:1], func=AF.Sqrt, scale=1.0 / 1024, bias=EPS), writes=[sd_])
                    yield
                    k.op("dve", lambda e: e.reciprocal(out=st[:, 2:3], in_=st[:, 1:2]), writes=[sd_])
                    yield
                    k.op("dve", lambda e: e.tensor_scalar(out=XN[:, c, :], in0=H[:, c, :], scalar1=st[:, 2:3], scalar2=None, op0=ALU.mult),
                         reads=[H_d[c], sd_], writes=[XN_d[c]])
                    yield
                    pt, ptd = PF.next()
                    ptb = pt[:].bitcast(BF16)
                    for kk in range(8):
                        k.op("pe", lambda e, kk=kk: e.transpose(ptb[:, kk * 128:(kk + 1) * 128], XN[:, c, kk * 128:(kk + 1) * 128], IDB),
                             reads=[XN_d[c], cdep], writes=[ptd])
                    hm, hmd = hm2.next(gid)
                    for kk in range(8):
                        k.op("act", lambda e, kk=kk: e.activation(out=hm[:, kk, :], in_=ptb[:, kk * 128:(kk + 1) * 128], func=AF.Identity,
                                                                   scale=A2[:, kk, r:r + 1], bias=modT[:, 24 + kk, r:r + 1]),
                             reads=[ptd, mod_dep], writes=[hmd])
                    yield
                    pl_, pld = PG.next()
                    for kk in range(8):
                        k.op("pe", lambda e, kk=kk: e.matmul(pl_[:, 0:16], lhsT=hm[:, kk, :], rhs=RW[:, kk, :], start=(kk == 0), stop=(kk == 7)),
                             reads=[hmd, rw_d], writes=[pld])
                    k.op("dve", lambda e: e.tensor_reduce(out=st[:, 3:4], in_=pl_[:, 0:16], axis=AX.X, op=ALU.max), reads=[pld], writes=[sd_])
                    yield
                    k.op("dve", lambda e: e.tensor_scalar(out=st[:, 3:4], in0=st[:, 3:4], scalar1=-1.0, scalar2=None, op0=ALU.mult), writes=[sd_])
                    yield
                    k.op("act", lambda e: e.activation(out=AFF[:, c, :], in_=pl_[:, 0:16], func=AF.Exp, bias=st[:, 3:4], accum_out=st[:, 4:5]),
                         reads=[pld], writes=[sd_, aff_d[c]])
                    yield
                    k.op("dve", lambda e: e.reciprocal(out=st[:, 5:6], in_=st[:, 4:5]), writes=[sd_])
                    yield
                    k.op("dve", lambda e: e.tensor_scalar(out=AFF[:, c, :], in0=AFF[:, c, :], scalar1=st[:, 5:6], scalar2=None, op0=ALU.mult),
                         reads=[sd_], writes=[aff_d[c]])
                    yield
                    pt2, pt2d = PG.next()
                    k.op("pe", lambda e: e.transpose(pt2[0:16, 0:128], AFF[:, c, :], IDF), reads=[aff_d[c], cdep], writes=[pt2d])
                    k.op("act", lambda e: e.copy(out=AFFT[:, tcols(c)], in_=pt2[0:16, 0:128]), reads=[pt2d], writes=[afft_d])
                    yield
                run_window([(lambda gid, c=c: r_tile(c, gid)) for c in tiles], 2)
                segs = [(256, 2048, 256.0, CMPB)] + ([(0, 256, 32.0, CMPC)] if ctx_out else [])
                bdeps = [Dep(), Dep()]

                def bis_gen(si, t0, n, kcap, CM):
                    lo = bis[:, 4 * si + 0:4 * si + 1]
                    cntc = bis[:, 4 * si + 1:4 * si + 2]
                    gew = bis[:, 4 * si + 2:4 * si + 3]
                    bd = bdeps[si]
                    k.op("dve", lambda e: e.memset(lo, 0.0), writes=[bd])
                    w = 0.5
                    for it in range(24):
                        k.op("dve", lambda e, w=w: e.tensor_scalar(out=CM[:, 0:n], in0=AFFT[:, t0:t0 + n], scalar1=lo, scalar2=w, op0=ALU.subtract, op1=ALU.is_gt),
                             reads=[afft_d], writes=[bd])
                        yield
                        k.op("dve", lambda e: e.tensor_reduce(out=cntc, in_=CM[:, 0:n], axis=AX.X, op=ALU.add), writes=[bd])
                        yield
                        k.op("dve", lambda e, w=w: e.tensor_scalar(out=gew, in0=cntc, scalar1=kcap, scalar2=w, op0=ALU.is_ge, op1=ALU.mult), writes=[bd])
                        yield
                        k.op("dve", lambda e: e.tensor_tensor(out=lo, in0=lo, in1=gew, op=ALU.add), writes=[bd])
                        yield
                        w *= 0.5
                run_window([(lambda gid, si=si, sg=sg: bis_gen(si, *sg)) for si, sg in enumerate(segs)], 2)
                for si, (t0, n, kcap, CM) in enumerate(segs):
                    lo = bis[:, 4 * si + 0:4 * si + 1]
                    bis_d = bdeps[si]
                    CMPB_ = CM
                    k.op("dve", lambda e, lo=lo, t0=t0, n=n: e.tensor_scalar(out=CMPB_[:, 0:n], in0=AFFT[:, t0:t0 + n], scalar1=lo, scalar2=None, op0=ALU.is_gt),
                         reads=[afft_d], writes=[bis_d])
                    for c in range(t0 // 128, (t0 + n) // 128):
                        pt2, pt2d = PG.next()
                        cc = c * 128 - t0
                        k.op("pe", lambda e, pt2=pt2, cc=cc: e.transpose(pt2[:, 0:16], CMPB_[:, cc:cc + 128], IDF[0:16, 0:16]), reads=[bis_d, cdep], writes=[pt2d])
                        k.op("act", lambda e, pt2=pt2, c=c: e.copy(out=MKT[:, c, :], in_=pt2[:, 0:16]), reads=[pt2d], writes=[mkt_d])
                        k.op("dve", lambda e, pt2=pt2, c=c: e.tensor_copy(out=POS[:, c, :], in_=pt2[:, 0:16]), reads=[pt2d], writes=[pos_d])
                k.barrier()
            SEL = sb("SEL", [128, NT, 256], BF16, es)
            SEL_d = Dep()
            HID = sb("HID", [128, 12, 288], BF16, es)
            HID_d = Dep()
            YE = sb("YE", [128, 3, 1024], BF16, es)
            YE_d = Dep()
            YE2 = sb("YE2", [128, 3, 1024], BF16, es)
            YE2_d = Dep()
            wp = rot("wp", 4, [128, 8, 512], BF16, es)
            selT = rot("selT", 4, [128, 2, 512], BF16, es)
            sgp = rot("sg", 2, [128, 288], BF16, es)
            affc = sb("affc", [128, 3, 2], F32, es)
            affc_d = Dep()
            for (c_lo, c_hi) in ([(2, NT)] + ([(0, 2)] if ctx_out else [])):
                for c in range(c_lo, c_hi):
                    pp, ppd = PG.next()
                    prev = list(range(c_lo, c))
                    for i, cp in enumerate(prev):
                        k.op("pe", lambda e, pp=pp, cp=cp, i=i: e.matmul(pp[:, 0:16], lhsT=ONEB, rhs=MKT[:, cp, :], start=(i == 0), stop=False),
                             reads=[mkt_d, cdep], writes=[ppd])
                    k.op("pe", lambda e, pp=pp, c=c, prev=prev: e.matmul(pp[:, 0:16], lhsT=TRIB, rhs=MKT[:, c, :], start=(len(prev) == 0), stop=True),
                         reads=[mkt_d, cdep], writes=[ppd])
                    k.op("dve", lambda e, pp=pp, c=c: e.tensor_tensor(out=POS[:, c, :], in0=pp[:, 0:16], in1=POS[:, c, :], op=ALU.mult), reads=[ppd], writes=[pos_d])
                    pt2, pt2d = PG.next()
                    k.op("pe", lambda e, pt2=pt2, c=c: e.transpose(pt2[0:16, 0:128], POS[:, c, :], IDF), reads=[pos_d, cdep], writes=[pt2d])
                    k.op("act", lambda e, pt2=pt2, c=c: e.copy(out=POST[:, tcols(c)], in_=pt2[0:16, 0:128]), reads=[pt2d], writes=[post_d])
            AFFH = sb("AFFH", [128, NT, 16], BF16, es)
            AFFL = sb("AFFL", [128, NT, 16], BF16, es)
            afh_d = Dep()
            for c in tiles:
                st, sd_ = msm.next()
                k.op("act", lambda e, c=c: e.copy(out=AFFH[:, c, :], in_=AFF[:, c, :]), reads=[aff_d[c]], writes=[afh_d])
                sg, sgd = sgp.next()
                k.op("dve", lambda e, c=c, sg=sg: e.tensor_tensor(out=sg[:, 0:16], in0=AFF[:, c, :], in1=AFFH[:, c, :], op=ALU.subtract), reads=[aff_d[c], afh_d], writes=[sgd])
                k.op("act", lambda e, c=c, sg=sg: e.copy(out=AFFL[:, c, :], in_=sg[:, 0:16]), reads=[sgd], writes=[afh_d])

            print("SBUF remaining (moe phase)", nc.sbuf_bytes_remaining)
            if l == 0:
                k.barrier()
                chk("moe_r")
            IOC = CF[:, C_IOC:C_IOC + 256]
            wg_l = [ewg[l, e_].rearrange("(k p) n -> p k n", p=128) for e_ in range(16)]
            wu_l = [ewu[l, e_].rearrange("(k p) n -> p k n", p=128) for e_ in range(16)]
            wd_l = [ewd[l, e_].rearrange("(j p) n -> p j n", p=128) for e_ in range(16)]
            ncc = 3 if ctx_out else 2
            NX = 288 if ctx_out else 256
            YEs = [(YE, YE_d), (YE2, YE2_d)]

            def st_A(ex):
                for c in tiles:
                    n = 32 if c < 2 else 256
                    k.op("dve", lambda e, c=c, n=n: e.tensor_scalar(out=SEL[:, c, 0:n], in0=IOC[:, 0:n], scalar1=POS[:, c, ex:ex + 1], scalar2=None, op0=ALU.is_equal),
                         reads=[pos_d, cdep], writes=[SEL_d])

            def st_B(ex):
                pa_, pad_ = PG.next()
                for cc in range(ncc):
                    tl = [2 + i for i in range(16)] if cc < 2 else [0, 1]
                    M = 128 if cc < 2 else 32
                    co = (cc * 128) if cc < 2 else 0
                    seq = [(c, AFFH) for c in tl] + [(c, AFFL) for c in tl]
                    for i, (c, src) in enumerate(seq):
                        k.op("pe", lambda e, cc=cc, c=c, i=i, n_=len(seq), M=M, co=co, src=src: e.matmul(pa_[0:M, 4 * cc:4 * cc + 1], lhsT=SEL[:, c, co:co + M], rhs=src[:, c, ex:ex + 1],
                                                                                                   start=(i == 0), stop=(i == n_ - 1)), reads=[SEL_d, afh_d], writes=[pad_])
                for cc in range(ncc):
                    M = 128 if cc < 2 else 32
                    k.op("dve", lambda e, cc=cc, M=M: e.tensor_copy(out=affc[0:M, cc, 0:1], in_=pa_[0:M, 4 * cc:4 * cc + 1]),
                         reads=[pad_], writes=[affc_d])
                for kk in range(8):
                    pg_, pgd = PG.next()
                    lt = list(range(2, NT))
                    for i, c in enumerate(lt):
                        k.op("pe", lambda e, kk=kk, c=c, i=i, pg_=pg_: e.matmul(pg_[:, 0:256], lhsT=XN[:, c, kk * 128:(kk + 1) * 128], rhs=SEL[:, c, 0:256],
                                                                                 start=(i == 0), stop=(i == 15)), reads=[XN_d[c], SEL_d], writes=[pgd])
                    k.op("act", lambda e, kk=kk, pg_=pg_: e.activation(out=XSM[:, kk, 0:256], in_=pg_[:, 0:256], func=AF.Identity, scale=A2[:, kk, 0:1], bias=modT[:, 24 + kk, 0:1]),
                         reads=[pgd, mod_dep], writes=[XSM_d])
                    if ctx_out:
                        for i, c in enumerate([0, 1]):
                            k.op("pe", lambda e, kk=kk, c=c, i=i, pg_=pg_: e.matmul(pg_[:, 256:288], lhsT=XN[:, c, kk * 128:(kk + 1) * 128], rhs=SEL[:, c, 0:32],
                                                                                     start=(i == 0), stop=(i == 1)), reads=[XN_d[c], SEL_d], writes=[pgd])
                        k.op("act", lambda e, kk=kk, pg_=pg_: e.activation(out=XSM[:, kk, 256:288], in_=pg_[:, 256:288], func=AF.Identity, scale=A2[:, kk, 1:2], bias=modT[:, 24 + kk, 1:2]),
                             reads=[pgd, mod_dep], writes=[XSM_d])

            def st_C(ex):
                for jg in range(3):
                    wgt, wgd = wp.next()
                    k.dma("pool", wgt[:], wg_l[ex][:, :, jg * 512:(jg + 1) * 512], writes=[wgd])
                    wut, wud = wp.next()
                    k.dma("pool", wut[:], wu_l[ex][:, :, jg * 512:(jg + 1) * 512], writes=[wud])
                    for jj in range(4):
                        j = jg * 4 + jj
                        pgt_, pgtd = PF.next()
                        put_, putd = PF.next()
                        for kk in range(8):
                            k.op("pe", lambda e, kk=kk, jj=jj, pgt_=pgt_, wgt=wgt: e.matmul(pgt_[:, 0:NX], lhsT=wgt[:, kk, jj * 128:(jj + 1) * 128], rhs=XSM[:, kk, 0:NX],
                                                                                         start=(kk == 0), stop=(kk == 7)), reads=[wgd, XSM_d], writes=[pgtd])
                        for kk in range(8):
                            k.op("pe", lambda e, kk=kk, jj=jj, put_=put_, wut=wut: e.matmul(put_[:, 0:NX], lhsT=wut[:, kk, jj * 128:(jj + 1) * 128], rhs=XSM[:, kk, 0:NX],
                                                                                         start=(kk == 0), stop=(kk == 7)), reads=[wud, XSM_d], writes=[putd])
                        sg, sgd = sgp.next()
                        k.op("act", lambda e, sg=sg, pgt_=pgt_: e.activation(out=sg[:, 0:NX], in_=pgt_[:, 0:NX], func=AF.Silu), reads=[pgtd], writes=[sgd])
                        k.op("dve", lambda e, sg=sg, put_=put_, j=j: e.tensor_tensor(out=HID[:, j, 0:NX], in0=put_[:, 0:NX], in1=sg[:, 0:NX], op=ALU.mult),
                             reads=[putd, sgd], writes=[HID_d])

            def st_D(ex, yb):
                YEt, YEd = YEs[yb]
                accs = [(PF.next()) for _ in range(4)] + [PG.next(), PG.next()]
                for jg in range(3):
                    wdt, wdd = wp.next()
                    wv = wdt[:].rearrange("p a b -> p (a b)").rearrange("p (j n) -> p j n", j=4)
                    k.dma("pool", wv, wd_l[ex][:, jg * 4:(jg + 1) * 4, :], writes=[wdd])
                    for cc in range(ncc):
                        M = 128 if cc < 2 else 32
                        co = cc * 128
                        for half in range(2):
                            pa2, pa2d = accs[cc * 2 + half]
                            for jj in range(4):
                                j = jg * 4 + jj
                                k.op("pe", lambda e, pa2=pa2, M=M, co=co, half=half, jj=jj, j=j, wv=wv: e.matmul(pa2[0:M, :], lhsT=HID[:, j, co:co + M], rhs=wv[:, jj, half * 512:(half + 1) * 512],
                                                                                                              start=(j == 0), stop=(j == 11)), reads=[HID_d, wdd], writes=[pa2d])
                for cc in range(ncc):
                    M = 128 if cc < 2 else 32
                    r = 1 if cc == 2 else 0
                    for half in range(2):
                        pa2, pa2d = accs[cc * 2 + half]
                        k.op("dve", lambda e, pa2=pa2, M=M, cc=cc, half=half, r=r: e.scalar_tensor_tensor(out=YEt[0:M, cc, half * 512:(half + 1) * 512], in0=pa2[0:M, :], scalar=affc[0:M, cc, 0:1],
                                                                                                         in1=G2b[0:M, r, half * 512:(half + 1) * 512], op0=ALU.mult, op1=ALU.mult),
                             reads=[pa2d, affc_d, g2d], writes=[YEd])

            def st_E(exs):
                blocks = [(256 + 512 * i, 512, 0) for i in range(4)] + ([(0, 256, 1)] if ctx_out else [])
                for (t0, n, isctx) in blocks:
                    sts = []
                    for bi, ex in enumerate(exs):
                        pb, pbd = PC.next()
                        k.op("pe", lambda e, pb=pb, ex=ex: e.matmul(pb[:, 0:n], lhsT=IDB[0:16, ex:ex + 1].to_broadcast([16, 128]), rhs=POST[:, t0:t0 + n], start=True, stop=True),
                             reads=[cdep, post_d], writes=[pbd])
                        st_, std_ = selT.next()
                        nch = 1 if isctx else 2
                        for j in range(nch):
                            k.op("dve", lambda e, pb=pb, st_=st_, j=j: e.tensor_scalar(out=st_[:, j, 0:n], in0=pb[:, 0:n], scalar1=CF[:, C_IOP + j:C_IOP + j + 1], scalar2=None, op0=ALU.is_equal),
                                 reads=[pbd, cdep], writes=[std_])
                        sts.append((st_, std_))
                    for ti in range(n // 128):
                        c = t0 // 128 + ti
                        for half in range(2):
                            pc_, pcd = PF.next()
                            mms = []
                            for bi in range(len(exs)):
                                st_, std_ = sts[bi]
                                YEt, YEd = YEs[bi]
                                if isctx:
                                    mms.append((st_[0:32, 0, ti * 128:(ti + 1) * 128], YEt[0:32, 2, half * 512:(half + 1) * 512], std_, YEd))
                                else:
                                    for j in range(2):
                                        mms.append((st_[:, j, ti * 128:(ti + 1) * 128], YEt[:, j, half * 512:(half + 1) * 512], std_, YEd))
                            for i, (lh, rh, d1, d2) in enumerate(mms):
                                k.op("pe", lambda e, pc_=pc_, lh=lh, rh=rh, i=i, nm=len(mms): e.matmul(pc_[:], lhsT=lh, rhs=rh, start=(i == 0), stop=(i == nm - 1)),
                                     reads=[d1, d2], writes=[pcd])
                            k.op("dve", lambda e, pc_=pc_, c=c, half=half: e.tensor_tensor(out=H[:, c, half * 512:(half + 1) * 512], in0=pc_[:], in1=H[:, c, half * 512:(half + 1) * 512], op=ALU.add),
                                 reads=[pcd], writes=[H_d[c]])

            st_A(0)
            for p_ in range(8):
                e0, e1 = 2 * p_, 2 * p_ + 1
                st_B(e0)
                st_A(e1)
                st_C(e0)
                st_D(e0, 0)
                st_B(e1)
                if e1 + 1 < 16:
                    st_A(e1 + 1)
                st_C(e1)
                st_D(e1, 1)
                st_E([e0, e1])
            if l < nlayers - 1:
                for c in tiles:
                    k.dma("sp", hd[c * 128:(c + 1) * 128, :], H[:, c, :], reads=[H_d[c]], writes=[hd_dep[c]])
                k.barrier()
                dump("d_hend", hd, [NTOK, 1024], hd_dep, q="sp")
                chk("moe0")
            else:
                FN = G2b[:, 0, :]
                cmb = Rot([(XN[:, 2 * i:2 * i + 2, :].bitcast(F32).rearrange("p a b -> p (a b)"), Dep()) for i in range(4)])
                k.dma("sp", FN, fnw.partition_broadcast(128), reads=[], writes=[g2d])
                for c in range(2, NT):
                    st, sd_ = msm.next()
                    jt, jd = jk2.next()
                    k.op("act", lambda e, c=c, st=st, jt=jt: e.activation(out=jt[:], in_=H[:, c, :], func=AF.Square, accum_out=st[:, 0:1]), reads=[H_d[c]], writes=[jd, sd_])
                    k.op("act", lambda e, st=st: e.activation(out=st[:, 1:2], in_=st[:, 0:1], func=AF.Sqrt, scale=1.0 / 1024, bias=EPS), writes=[sd_])
                    k.op("dve", lambda e, st=st: e.reciprocal(out=st[:, 2:3], in_=st[:, 1:2]), writes=[sd_])
                    cm, cmd = cmb.next()
                    k.op("dve", lambda e, st=st, c=c, cm=cm: e.scalar_tensor_tensor(out=cm, in0=H[:, c, :], scalar=st[:, 2:3], in1=FN, op0=ALU.mult, op1=ALU.mult),
                         reads=[H_d[c], sd_, g2d] + XN_d, writes=[cmd])
                    k.dma("sp", y_d[(c - 2) * 128:(c - 1) * 128, :], cm, reads=[cmd], writes=[y_dep])
            k.barrier()
    k.barrier()


def _consts():
    j = np.arange(128)[:, None]
    i = np.arange(128)[None, :]
    cst = np.zeros((128, NCST), np.float32)
    tri_f = (j <= i).astype(np.float32)
    tri_r = (j >= i).astype(np.float32)
    cst[:, 0:128] = tri_f
    cst[:, 128:256] = tri_r
    cst[:, 256:384] = (j > i).astype(np.float32)
    cst[:, 384:512] = (j < i).astype(np.float32)
    cst[:, 512:640] = np.eye(128, dtype=np.float32)
    cst[:, 640:768] = 1.0
    cst[:, 768:1024] = np.tile(np.where(j <= i, 0.0, NEGV), (1, 2))
    cst[:, 1024:1280] = np.tile(np.where(j >= i, 0.0, NEGV), (1, 2))
    cst[:, 1280:1536] = np.arange(1, 257, dtype=np.float32)[None, :]
    cst[:, 1536] = np.arange(1, 129, dtype=np.float32)
    cst[:, 1537] = np.arange(129, 257, dtype=np.float32)
    msk = np.concatenate([tri_f, tri_f, tri_r, tri_r], axis=1).astype(np.float32)
    esel = np.zeros((16, 16, 128), np.float32)
    for e in range(16):
        esel[e, e, :] = 1.0
    esel = esel.reshape(16, 2048)
    rope = np.zeros((2, 18, 128, 2, 256), np.float32)
    inv = (10000.0 ** (-np.arange(16, dtype=np.float32) / 16)).astype(np.float32)
    for c in range(18):
        for p in range(128):
            if c < 2:
                cosf = np.ones(128, np.float32)
                sinf = np.zeros(128, np.float32)
            else:
                t = (c - 2) * 128 + p
                pos = (np.float32(t // 64), np.float32(t % 64))
                cosf = np.zeros(128, np.float32)
                sinf = np.zeros(128, np.float32)
                for h in range(2):
                    for a in range(2):
                        ang = (pos[a] * inv).astype(np.float32)
                        cs, sn = np.cos(ang), np.sin(ang)
                        b0 = h * 64 + a * 32
                        cosf[b0:b0 + 16] = cs
                        cosf[b0 + 16:b0 + 32] = cs
                        sinf[b0:b0 + 16] = -sn
                        sinf[b0 + 16:b0 + 32] = sn
            rope[0, c, p, 0, 0:128] = cosf * 0.125
            rope[0, c, p, 0, 128:256] = cosf
            rope[0, c, p, 1, 0:128] = sinf * 0.125
            rope[0, c, p, 1, 128:256] = sinf
            rope[1, c, p, 0, 0:128] = 1.0
            rope[1, c, p, 0, 128:256] = 0.125
    return cst, msk, esel, rope


_CACHE = {}


def kernel(**inputs):
    f = lambda a: np.ascontiguousarray(np.asarray(a, dtype=np.float32))
    inp = {kk: f(v) for kk, v in inputs.items()}
    if "nc" not in _CACHE:
        _CACHE["nc"] = build(2)
        _CACHE["consts"] = _consts()
    nc = _CACHE["nc"]
    cst, msk, esel, rope = _CACHE["consts"]
    shared = {
        "w_mod": inp["w_mod"],
        "bmT": np.ascontiguousarray(inp["b_mod"].reshape(2, 48, 128).transpose(0, 2, 1)),
        "nw1T": np.ascontiguousarray(inp["norm1_w"].reshape(2, 8, 128).transpose(0, 2, 1)),
        "nw2T": np.ascontiguousarray(inp["norm2_w"].reshape(2, 8, 128).transpose(0, 2, 1)),
        "w_in": inp["w_in"], "w_out": inp["w_out"],
        "ret_decay_logit": inp["ret_decay_logit"], "ret_norm_w": inp["ret_norm_w"],
        "gla_gate_w": inp["gla_gate_w"], "gla_gate_b": inp["gla_gate_b"], "gla_norm_w": inp["gla_norm_w"],
        "ssd_conv_w": inp["ssd_conv_w"], "ssd_conv_b": inp["ssd_conv_b"], "ssd_dt_bias": inp["ssd_dt_bias"],
        "ssd_a_log": inp["ssd_a_log"], "ssd_d": inp["ssd_d"], "ssd_norm_w": inp["ssd_norm_w"],
        "mlstm_gate_b": inp["mlstm_gate_b"], "mlstm_norm_w": inp["mlstm_norm_w"],
        "router_w": inp["router_w"], "ewg": inp["expert_w_gate"], "ewu": inp["expert_w_up"], "ewd": inp["expert_w_down"],
        "fnw": inp["final_norm_w"].reshape(1, 1024),
        "rope": rope, "cst": cst, "msk": msk, "esel": esel,
    }
    in_maps = []
    for b in range(8):
        m = dict(shared)
        m["x"] = inp["x"][b]
        m["ctx"] = inp["ctx"][b]
        cin = np.stack([inp["c"][b].reshape(8, 128).T, inp["c_ctx"].reshape(8, 128).T], axis=-1)
        m["cin"] = np.ascontiguousarray(cin.astype(np.float32))
        in_maps.append(m)
    res = run_bass_kernel_spmd(nc, in_maps, core_ids=list(range(8)))
    return np.stack([np.asarray(r["y"], dtype=np.float32) for r in res.results], axis=0)
```

```python
import math
from contextlib import ExitStack
import numpy as np
import concourse.bass as bass
import concourse.mybir as mybir
from concourse.bass_utils import run_bass_kernel_spmd

F32 = mybir.dt.float32
BF16 = mybir.dt.bfloat16
AF = mybir.ActivationFunctionType
ALU = mybir.AluOpType
AX = mybir.AxisListType

NT = 18
NTOK = 2304
EPS = 1e-6
NEGV = -1.0e5
C_TRI = (0, 128)
C_STR = (256, 384)
C_ID = 512
C_ONE = 640
C_NEG = (768, 1024)
C_IOC = 1280
C_IOP = 1536
NCST = 1540


class Dep:
    __slots__ = ("w", "r")

    def __init__(self):
        self.w = None
        self.r = {}


class KB:
    SEMCAP = 20000
    ND = 8

    def __init__(self):
        self.nc = nc = bass.Bass("TRN2", target_bir_lowering=False)
        self.E = {"pe": nc.tensor, "act": nc.scalar, "dve": nc.vector, "pool": nc.gpsimd, "sp": nc.sync}
        self.cnt = {e: 0 for e in self.E}
        self.sems = {e: [] for e in self.E}
        self.waited = {e: {} for e in self.E}
        self.dmak = {"sp": 0, "pool": 0}
        self.dsems = {q: [nc.alloc_semaphore(f"d_{q}_{i}") for i in range(self.ND)] for q in self.dmak}
        self.latest = {}
        self.uid = 0

    def name(self, s):
        self.uid += 1
        return f"{s}_{self.uid}"

    def _tok(self, e):
        kk = self.cnt[e]
        i = kk // self.SEMCAP
        if i >= len(self.sems[e]):
            self.sems[e].append(self.nc.alloc_semaphore(f"s_{e}_{i}"))
        self.cnt[e] = kk + 1
        t = (self.sems[e][i], kk % self.SEMCAP + 1, f"{e}{i}", e)
        self.latest[t[2]] = t
        return t

    def _wait(self, e, toks):
        best = {}
        for t in toks:
            if t is None:
                continue
            if e == "pe" and t[3] == "pe":
                continue
            if best.get(t[2], (None, 0))[1] < t[1]:
                best[t[2]] = t
        for key, t in best.items():
            if self.waited[e].get(key, 0) >= t[1]:
                continue
            self.E[e].wait_ge(t[0], t[1])
            self.waited[e][key] = t[1]

    @staticmethod
    def _deps(reads, writes):
        toks = []
        for d in reads:
            toks.append(d.w)
        for d in writes:
            toks.append(d.w)
            toks.extend(d.r.values())
        return toks

    @staticmethod
    def _mark(t, reads, writes):
        for d in reads:
            d.r[t[2]] = t
        for d in writes:
            d.w = t
            d.r = {}

    def op(self, e, fn, reads=(), writes=(), pe_serial=False):
        self._wait(e, self._deps(reads, writes))
        if pe_serial and e == "pe" and self.cnt["pe"] > 0:
            kk = self.cnt["pe"] - 1
            i = kk // self.SEMCAP
            key = f"pe{i}"
            val = kk % self.SEMCAP + 1
            if self.waited["pe"].get(key, 0) < val:
                self.E["pe"].wait_ge(self.sems["pe"][i], val)
                self.waited["pe"][key] = val
        ins = fn(self.E[e])
        t = self._tok(e)
        ins.then_inc(t[0], 1)
        self._mark(t, reads, writes)
        return t

    def dma(self, q, out, in_, reads=(), writes=(), **kw):
        kk = self.dmak[q]
        slot = kk % self.ND
        val = 16 * (kk // self.ND + 1)
        sem = self.dsems[q][slot]
        key = f"d{q}{slot}"
        toks = self._deps(reads, writes)
        if kk >= self.ND:
            toks.append((sem, val - 16, key, "dma"))
        self._wait(q, toks)
        self.E[q].dma_start(out=out, in_=in_, **kw).then_inc(sem, 16)
        self.dmak[q] = kk + 1
        t = (sem, val, key, "dma")
        self.latest[key] = t
        self._mark(t, reads, writes)
        return t

    def barrier(self):
        toks = list(self.latest.values())
        for e in self.E:
            self._wait(e, toks)


LIVE = set()


class Rot:
    def __init__(self, items):
        self.items = items
        self.i = 0
        self.owner = [None] * len(items)

    def next(self, owner=None):
        j = self.i % len(self.items)
        if self.owner[j] is not None and self.owner[j] in LIVE and self.owner[j] != owner:
            raise AssertionError("rotating buffer reused while its previous owner generator is still live")
        self.owner[j] = owner
        it = self.items[j]
        self.i += 1
        return it


_GID = [0]


def run_window(facts, W, limits=None):
    limits = limits or {}
    active = []
    nxt = 0
    while True:
        while len(active) < W and nxt < len(facts):
            f = facts[nxt]
            rdy = getattr(f, "ready", None)
            kind = getattr(f, "kind", None)
            nk = sum(1 for a in active if a[2] == kind)
            if (rdy is not None and not rdy()) or (kind in limits and nk >= limits[kind]):
                assert active, "window deadlock"
                break
            _GID[0] += 1
            gid = _GID[0]
            LIVE.add(gid)
            active.append((gid, f(gid), kind))
            nxt += 1
        if not active:
            break
        still = []
        for gid, g, kind in active:
            try:
                next(g)
                still.append((gid, g, kind))
            except StopIteration:
                LIVE.discard(gid)
        active = still


class _Stop(Exception):
    pass


def build(nlayers=2, stop=None, dbg=()):
    k = KB()
    try:
        _build(k, nlayers, stop, dbg)
    except _Stop:
        pass
    k.barrier()
    return k.nc


def _build(k, nlayers, stop, dbg):
    nc = k.nc

    def chk(tag):
        if stop == tag:
            raise _Stop()

    def dump(name, src, shape, reads, q="pool"):
        if name not in dbg:
            return
        o = nc.dram_tensor(name, list(shape), F32, kind="ExternalOutput").ap()
        k.dma(q, o, src, reads=reads, writes=[Dep()])

    def din(name, shape):
        return nc.dram_tensor(name, list(shape), F32, kind="ExternalInput").ap()

    x_d = din("x", [2048, 1024])
    ctx_d = din("ctx", [256, 1024])
    cin_d = din("cin", [128, 8, 2])
    w_mod = din("w_mod", [2, 1024, 6144])
    bmT = din("bmT", [2, 128, 48])
    nw1T = din("nw1T", [2, 128, 8])
    nw2T = din("nw2T", [2, 128, 8])
    w_in = din("w_in", [2, 1024, 3624])
    w_out = din("w_out", [2, 1024, 1024])
    ret_logit = din("ret_decay_logit", [2, 2, 4])
    ret_nw = din("ret_norm_w", [2, 256])
    gla_gw = din("gla_gate_w", [2, 2, 16, 128])
    gla_gb = din("gla_gate_b", [2, 2, 128])
    gla_nw = din("gla_norm_w", [2, 256])
    ssd_cw = din("ssd_conv_w", [2, 5, 512])
    ssd_cb = din("ssd_conv_b", [2, 512])
    ssd_dtb = din("ssd_dt_bias", [2, 2, 4])
    ssd_alog = din("ssd_a_log", [2, 2, 4])
    ssd_dsk = din("ssd_d", [2, 4])
    ssd_nw = din("ssd_norm_w", [2, 256])
    ml_gb = din("mlstm_gate_b", [2, 2, 2, 4])
    ml_nw = din("mlstm_norm_w", [2, 256])
    router_w = din("router_w", [2, 1024, 16])
    ewg = din("ewg", [2, 16, 1024, 1536])
    ewu = din("ewu", [2, 16, 1024, 1536])
    ewd = din("ewd", [2, 16, 1536, 1024])
    fnw = din("fnw", [1, 1024])
    rope_d = din("rope", [2, 18, 128, 2, 256])
    cst_d = din("cst", [128, NCST])
    msk_d = din("msk", [128, 512])
    esel_d = din("esel", [16, 2048])
    y_d = nc.dram_tensor("y", [2048, 1024], F32, kind="ExternalOutput").ap()
    hd = nc.dram_tensor("hd", [NTOK, 1024], F32, kind="Internal").ap()
    hd_dep = [Dep() for _ in range(NT)]
    y_dep = Dep()

    def sb(name, shape, dt, stack=None):
        if stack is None:
            return nc.alloc_sbuf_tensor(k.name(name), list(shape), dt)
        return stack.enter_context(nc.sbuf_tensor(k.name(name), list(shape), dt))

    def rot(name, n, shape, dt, stack):
        return Rot([(sb(name, shape, dt, stack), Dep()) for _ in range(n)])

    CF = sb("CF", [128, NCST], F32)
    CB = sb("CB", [128, 768], BF16)
    MK = sb("MK", [128, 512], BF16)
    cdep = Dep()
    k.dma("sp", CF[:], cst_d, writes=[cdep])
    k.dma("pool", CB[:], cst_d[:, 0:768], writes=[cdep])
    k.dma("pool", MK[:], msk_d, writes=[cdep])
    NGBt = sb("NGB", [128, 256], BF16)
    k.dma("pool", NGBt[:, 0:128], cst_d[:, C_NEG[0]:C_NEG[0] + 128], writes=[cdep])
    k.dma("pool", NGBt[:, 128:256], cst_d[:, C_NEG[1]:C_NEG[1] + 128], writes=[cdep])
    NGB = [NGBt[:, 0:128], NGBt[:, 128:256]]
    modT = sb("modT", [128, 48, 2], F32)
    A1 = sb("A1", [128, 8, 2], F32)
    A2 = sb("A2", [128, 8, 2], F32)
    mod_dep = Dep()
    TRI = [CF[:, C_TRI[d]:C_TRI[d] + 128] for d in range(2)]
    STR = [CF[:, C_STR[d]:C_STR[d] + 128] for d in range(2)]
    NEG = [CF[:, C_NEG[d]:C_NEG[d] + 256] for d in range(2)]
    IDF = CF[:, C_ID:C_ID + 128]
    ONEF = CF[:, C_ONE:C_ONE + 128]
    IDB = CB[:, C_ID:C_ID + 128]
    ONEB = CB[:, C_ONE:C_ONE + 128]
    TRIB = CB[:, 0:128]
    MSK = [MK[:, d * 256:(d + 1) * 256] for d in range(2)]

    def hsrc(l, c):
        if l == 0:
            if c < 2:
                return ctx_d[c * 128:(c + 1) * 128, :]
            return x_d[(c - 2) * 128:(c - 1) * 128, :]
        return hd[c * 128:(c + 1) * 128, :]

    def tcols(c):
        return slice(c * 128, (c + 1) * 128)

    for l in range(nlayers):
        ctx_out = l < 1
        tiles_out = list(range(NT)) if ctx_out else list(range(2, NT))
        w_in_l = w_in[l].rearrange("(k p) n -> p k n", p=128)
        with ExitStack() as ms:
            psum = [ms.enter_context(nc.psum_tensor(k.name("ps"), [128, 512], F32)) for _ in range(4)]
            psBC = [ms.enter_context(nc.psum_tensor(k.name("psbc"), [128, 1024], F32)) for _ in range(2)]
            pdep = [Dep() for _ in range(8)]
            PA = Rot([(psum[0], pdep[0]), (psum[1], pdep[1])])
            PT_ = Rot([(psum[2], pdep[2]), (psum[3], pdep[3])])
            PA4 = Rot([(psum[0], pdep[0]), (psum[1], pdep[1]), (psBC[0][:, 0:512], pdep[4]), (psBC[0][:, 512:1024], pdep[5])])
            hmodT = sb("hmodT", [128, 8, NTOK], BF16, ms)
            hmodT_d = [Dep() for _ in range(NT)]
            yT = sb("yT", [128, 8, NTOK], BF16, ms)
            yT_d = [Dep() for _ in range(NT)]
            wblk = rot("wblk", 2, [128, 8, 520], BF16, ms)

            with ExitStack() as s0:
                cinS = sb("cinS", [128, 8, 2], F32, s0)
                scS = sb("scS", [128, 8, 2], BF16, s0)
                bmS = sb("bmS", [128, 48], F32, s0)
                nwS = sb("nwS", [128, 16], F32, s0)
                d0 = Dep()
                k.dma("sp", cinS[:], cin_d, writes=[d0])
                k.dma("sp", bmS[:], bmT[l], writes=[d0])
                k.dma("sp", nwS[:, 0:8], nw1T[l], writes=[d0])
                k.dma("sp", nwS[:, 8:16], nw2T[l], writes=[d0])
                k.op("act", lambda e: e.activation(out=scS[:], in_=cinS[:], func=AF.Silu), reads=[d0], writes=[d0])
                wm = w_mod[l].rearrange("(k p) n -> p k n", p=128)
                pm, pmd = PA.next()
                for cb in range(12):
                    wt, wd = wblk.next()
                    k.dma("pool", wt[:, :, 0:512], wm[:, :, cb * 512:(cb + 1) * 512], writes=[wd])
                    for j in range(4):
                        cc = cb * 4 + j
                        for kk in range(8):
                            k.op("pe", lambda e, cc=cc, kk=kk, j=j, wt=wt: e.matmul(
                                pm[:, 2 * cc:2 * cc + 2], lhsT=wt[:, kk, j * 128:(j + 1) * 128], rhs=scS[:, kk, :],
                                start=(kk == 0), stop=(kk == 7)), reads=[wd, d0], writes=[pmd])
                k.op("dve", lambda e: e.tensor_tensor(
                    out=modT[:], in0=pm[:, 0:96].rearrange("p (c r) -> p c r", r=2),
                    in1=bmS[:].unsqueeze(2).to_broadcast([128, 48, 2]), op=ALU.add), reads=[pmd, d0, mod_dep], writes=[mod_dep])
                for c0 in (8, 32):
                    k.op("dve", lambda e, c0=c0: e.tensor_scalar(out=modT[:, c0:c0 + 8, :], in0=modT[:, c0:c0 + 8, :],
                                                                  scalar1=1.0, scalar2=None, op0=ALU.add), writes=[mod_dep])
                k.op("dve", lambda e: e.tensor_tensor(out=A1[:], in0=modT[:, 8:16, :],
                                                      in1=nwS[:, 0:8].unsqueeze(2).to_broadcast([128, 8, 2]), op=ALU.mult),
                     reads=[d0], writes=[mod_dep])
                k.op("dve", lambda e: e.tensor_tensor(out=A2[:], in0=modT[:, 32:40, :],
                                                      in1=nwS[:, 8:16].unsqueeze(2).to_broadcast([128, 8, 2]), op=ALU.mult),
                     reads=[d0], writes=[mod_dep])
                k.barrier()
                if l == 0:
                    dump("d_modT", modT[:].rearrange("p c r -> p (c r)"), [128, 96], [mod_dep], q="sp")
                    chk("p0")

            with ExitStack() as s1:
                hp_ = rot("hp", 2, [128, 1024], F32, s1)
                xnp = rot("xn", 2, [128, 1024], BF16, s1)
                jk = rot("jk", 1, [128, 1024], BF16, s1)
                smp = rot("sm", 2, [128, 4], F32, s1)
                def p1_tile(c, gid):
                    r = 1 if c < 2 else 0
                    ht, hdp = hp_.next(gid)
                    k.dma("sp", ht[:], hsrc(l, c), reads=[hd_dep[c]], writes=[hdp])
                    st, sd_ = smp.next(gid)
                    jt, jd = jk.next()
                    yield
                    k.op("act", lambda e: e.activation(out=jt[:], in_=ht[:], func=AF.Square, accum_out=st[:, 0:1]),
                         reads=[hdp], writes=[jd, sd_])
                    yield
                    k.op("act", lambda e: e.activation(out=st[:, 1:2], in_=st[:, 0:1], func=AF.Sqrt, scale=1.0 / 1024, bias=EPS),
                         writes=[sd_])
                    yield
                    k.op("dve", lambda e: e.reciprocal(out=st[:, 2:3], in_=st[:, 1:2]), writes=[sd_])
                    yield
                    xt, xd = xnp.next(gid)
                    k.op("dve", lambda e: e.tensor_scalar(out=xt[:], in0=ht[:], scalar1=st[:, 2:3], scalar2=None, op0=ALU.mult),
                         reads=[hdp, sd_], writes=[xd])
                    yield
                    pt, ptd = PT_.next()
                    ptb = pt[:].bitcast(BF16)
                    for kk in range(8):
                        k.op("pe", lambda e, kk=kk: e.transpose(ptb[:, kk * 128:(kk + 1) * 128], xt[:, kk * 128:(kk + 1) * 128], IDB),
                             reads=[xd, cdep], writes=[ptd])
                    for kk in range(8):
                        k.op("act", lambda e, kk=kk: e.activation(
                            out=hmodT[:, kk, tcols(c)], in_=ptb[:, kk * 128:(kk + 1) * 128], func=AF.Identity,
                            scale=A1[:, kk, r:r + 1], bias=modT[:, kk, r:r + 1]), reads=[ptd, mod_dep], writes=[hmodT_d[c]])
                    yield
                run_window([(lambda gid, c=c: p1_tile(c, gid)) for c in range(NT)], 2)
                k.barrier()
                if l == 0:
                    dump("d_hmodT", hmodT[:].rearrange("p k t -> p (k t)"), [128, 8 * NTOK], hmodT_d)
                    chk("p1")

            qTx = sb("qTx", [128, 2, NTOK], BF16, ms)
            k.op("pool", lambda e: e.memset(qTx[:], 0.0), writes=[Dep()])
            k.barrier()
            qT = qTx[:, 0, :]
            kT = sb("kT", [128, NTOK], BF16, ms)
            ktok = sb("ktok", [128, NT, 128], BF16, ms)
            vtok = sb("vtok", [128, NT, 2, 65], BF16, ms)
            gate = sb("gate", [128, NT, 128], BF16, ms)
            oacc = sb("oacc", [128, NT, 2, 65], F32, ms)
            lai = sb("lai", [128, NT, 2, 2, 2], F32, ms)
            aux = sb("aux", [128, 2312], F32, ms)
            ygs = sb("ygs", [128, NT, 260], BF16, ms)
            ssq = sb("ssq", [128, NT, 2], F32, ms)
            qT_d = [Dep() for _ in range(NT)]
            qT_d2 = [Dep() for _ in range(NT)]
            kT_d = [Dep() for _ in range(NT)]
            ktok_d = [Dep() for _ in range(NT)]
            vtok_d = [Dep() for _ in range(NT)]
            gate_d = [Dep() for _ in range(NT)]
            oacc_d = [Dep() for _ in range(NT)]
            lai_d = [Dep() for _ in range(NT)]
            aux_d = Dep()
            ygs_d = [Dep() for _ in range(NT)]
            lag = aux[:, 0:NTOK].rearrange("p (c d e) -> p c d e", c=NT, d=2)
            lag_d = [Dep() for _ in range(NT)]
            for c in range(NT):
                k.op("pool", lambda e, c=c: e.memset(vtok[:, c, :, 64:65], 1.0), writes=[vtok_d[c]])
            usb = rot("usb", 2, [128, 512], F32, ms)
            ropep = rot("ropet", 2, [128, 2, 256], F32, ms)
            qkp = rot("qk", 2, [128, 256], BF16, ms)
            shr4 = sb("shr4", [128, 1024], F32, ms)
            t1p = Rot([(shr4[:, 0:256], Dep()), (shr4[:, 256:512], Dep())])
            t2p = Rot([(shr4[:, 512:768], Dep()), (shr4[:, 768:1024], Dep())])
            prm = sb("prm", [128, 64], F32, ms)
            prm_d = Dep()
            nwb = sb("nwb", [128, 256], F32, ms)
            nwb_d = Dep()
            S32 = [sb("S32", [128, 2, 65], F32, ms) for _ in range(2)]
            Sbf = [sb("Sbf", [128, 2, 65], BF16, ms) for _ in range(2)]
            S_d = [Dep() for _ in range(2)]
            S_dh = [[Dep(), Dep()] for _ in range(2)]
            Sbf_d = [Dep() for _ in range(2)]
            Dw = [rot("Dw", 3, [128, 256], BF16, ms) for _ in range(2)]
            PTw = [rot("PTw", 4, [128, 256], BF16, ms) for _ in range(2)]
            t1w = [rot("t1w", 2, [128, 2, 65], F32, ms) for _ in range(2)]
            kgw = [rot("kgw", 2, [128, 128], BF16, ms) for _ in range(2)]

            fin = rot("fin", 6, [128, 2, 64], F32, ms)
            fin2 = rot("fin2", 6, [128, 2, 64], F32, ms)
            finb = rot("finb", 4, [128, 128], BF16, ms)
            fsm = rot("fsm", 6, [128, 8], F32, ms)

            def load_wblk(colranges):
                wt, wd = wblk.next()
                o = 0
                for (c0, n) in colranges:
                    k.dma("pool", wt[:, :, o:o + n], w_in_l[:, :, c0:c0 + n], writes=[wd])
                    o += n
                return wt, wd, o

            def inproj_tok(wt, wd, ncols, c, pt, ptd, off=0, w0=0):
                for kk in range(8):
                    k.op("pe", lambda e, kk=kk: e.matmul(pt[:, off:off + ncols], lhsT=hmodT[:, kk, tcols(c)], rhs=wt[:, kk, w0:w0 + ncols],
                                                          start=(kk == 0), stop=(kk == 7)), reads=[hmodT_d[c], wd], writes=[ptd])

            def transpose_to(dstT, dst_d, src, src_d, nrows_out, c):
                pt, ptd = PT_.next()
                ptb = pt[:].bitcast(BF16)
                k.op("pe", lambda e: e.transpose(ptb[0:nrows_out, 0:128], src, IDB), reads=[src_d, cdep], writes=[ptd])
                k.op("act", lambda e: e.copy(out=dstT[0:nrows_out, tcols(c)], in_=ptb[0:nrows_out, 0:128]), reads=[ptd], writes=[dst_d])

            def softplus_neg(dst, src, reads, writes):
                k.op("act", lambda e: e.activation(out=dst, in_=src, func=AF.Exp, scale=-1.0), reads=reads, writes=writes)
                k.op("act", lambda e: e.activation(out=dst, in_=dst, func=AF.Ln, bias=1.0), writes=writes)
                k.op("dve", lambda e: e.tensor_scalar(out=dst, in0=dst, scalar1=-1.0, scalar2=None, op0=ALU.mult), writes=writes)

            SM = sb("SM", [128, NT, 2, 5, 2], F32, ms)
            SM_d = Dep()
            EGLD = sb("EGLD", [128, NT, 2], F32, ms)
            EGLD_d = Dep()
            KG = [sb("KG", [128, NT, 128], BF16, ms) for _ in range(2)]
            KG_d = [Dep() for _ in range(2)]
            _kg0 = KG[0][:].rearrange("p c e -> p (c e)").bitcast(F32)
            _kg1 = KG[1][:].rearrange("p c e -> p (c e)")
            egw = [Rot([(_kg0[:, (2 * d_ + i_) * 256:(2 * d_ + i_ + 1) * 256], Dep()) for i_ in range(2)]) for d_ in range(2)]
            qgw = [Rot([(_kg1[0:64, (3 * d_ + i_) * 384:(3 * d_ + i_ + 1) * 384], Dep()) for i_ in range(3)]) for d_ in range(2)]

            def gla_zero_qg():
                for d_ in range(2):
                    for (t_, dd_) in qgw[d_].items:
                        k.op("pool", lambda e, t_=t_: e.memset(t_, 0.0), writes=[dd_])

            def scalar_pre(shared_qk):
                for d in range(2):
                    pa, pad = PA.next()
                    la_all = lai[:, :, d, 0, :]
                    ig_all = lai[:, :, d, 1, :]
                    for i_, L_ in enumerate([TRI[d], STR[d], ONEF]):
                        k.op("pe", lambda e, i_=i_, L_=L_: e.matmul(pa[:, 36 * i_:36 * i_ + 36], lhsT=L_, rhs=la_all, start=True, stop=True),
                             reads=lai_d + [cdep], writes=[pad])
                    v3 = lambda i_: pa[:, 36 * i_:36 * i_ + 36].rearrange("p (c h) -> p c h", h=2)
                    k.op("dve", lambda e: e.tensor_tensor(out=SM[:, :, d, 0, :], in0=ig_all, in1=v3(0), op=ALU.subtract), reads=[pad] + lai_d, writes=[SM_d])
                    k.op("dve", lambda e: e.tensor_tensor(out=SM[:, :, d, 4, :], in0=ig_all, in1=v3(1), op=ALU.add), reads=[pad] + lai_d, writes=[SM_d])
                    k.op("act", lambda e: e.activation(out=SM[:, :, d, 1, :], in_=v3(0), func=AF.Exp), reads=[pad], writes=[SM_d])
                    k.op("act", lambda e: e.activation(out=SM[:, :, d, 2, :], in_=SM[:, :, d, 4, :], func=AF.Exp), writes=[SM_d])
                    k.op("act", lambda e: e.activation(out=SM[:, :, d, 3, :], in_=v3(2), func=AF.Exp), reads=[pad], writes=[SM_d])
                    if not shared_qk:
                        for h in range(2):
                            r_ = slice(64 * h, 64 * h + 64)
                            k.op("pool", lambda e, h=h, r_=r_: e.tensor_copy(out=EGLD[r_, :, d], in_=SM[r_, :, d, 3, h]), reads=[SM_d], writes=[EGLD_d])
                    if shared_qk:
                        kin = ktok[:, :, 0:64].unsqueeze(2).to_broadcast([128, NT, 2, 64])
                    else:
                        kin = ktok[:].rearrange("p c (h e) -> p c h e", h=2)
                    k.op("pool", lambda e, kin=kin: e.tensor_tensor(out=KG[d][:].rearrange("p c (h e) -> p c h e", h=2), in0=kin,
                                                                    in1=SM[:, :, d, 2, :].unsqueeze(3).to_broadcast([128, NT, 2, 64]), op=ALU.mult),
                         reads=ktok_d + [SM_d], writes=[KG_d[d]])

            xdeps = {}

            def xd(t, i):
                key = (id(t), i)
                if key not in xdeps:
                    xdeps[key] = Dep()
                return xdeps[key]

            kvpar = [0, 0]

            def scalar_P(d, c, rows, shared_qk, res, with_out=True):
                A, Ad = psum[2 + d], pdep[2 + d]
                pgb = A[:, 0:256]
                pss = A[:, 256:512]
                kvo = 130 * (kvpar[d] % 3)
                kvpar[d] += 1
                kv = psum[d][:, kvo:kvo + 130]
                res["kv"] = kv
                for h in range(2):
                    r_ = rows(h)
                    k.op("pe", lambda e, h=h, r_=r_: e.matmul(kv[r_, h * 65:(h + 1) * 65], lhsT=KG[d][:, c, h * 64:(h + 1) * 64], rhs=vtok[:, c, h, :],
                                                               start=True, stop=True), reads=[KG_d[d], vtok_d[c]], writes=[pdep[d]])
                if not with_out:
                    yield
                    return
                for h in range(2):
                    k.op("pe", lambda e, h=h: e.matmul(pgb[:, h * 128:(h + 1) * 128], lhsT=lai[:, c, d, 0, h:h + 1].to_broadcast([128, 128]),
                                                        rhs=TRI[d], start=True, stop=False), reads=[lai_d[c], cdep], writes=[Ad])
                    k.op("pe", lambda e, h=h: e.matmul(pgb[:, h * 128:(h + 1) * 128], lhsT=IDB, rhs=NGB[d], start=False, stop=True), reads=[cdep], writes=[Ad])
                if shared_qk:
                    k.op("pe", lambda e: e.matmul(pss[:, 0:128], lhsT=kT[0:64, tcols(c)], rhs=qT[0:64, tcols(c)], start=True, stop=True),
                         reads=[kT_d[c], qT_d[c]], writes=[Ad])
                else:
                    k.op("pe", lambda e: e.matmul(pss, lhsT=kT[:, tcols(c)], rhs=qTx[:, :, tcols(c)], start=True, stop=True),
                         reads=[kT_d[c], qT_d[c], qT_d2[c]], writes=[Ad])
                yield
                Dt, Dd = Dw[d].next()
                Dd2 = xd(Dt, 2)
                for h in range(2):
                    k.op("act", lambda e, h=h: e.activation(out=Dt[:, h * 128:(h + 1) * 128], in_=pgb[:, h * 128:(h + 1) * 128], func=AF.Exp,
                                                             bias=SM[:, c, d, 0, h:h + 1]), reads=[Ad, SM_d], writes=[Dd if h == 0 else Dd2])
                yield
                Pt, Pd = PTw[d].next()
                if shared_qk:
                    k.op("dve", lambda e: e.tensor_tensor(out=Pt[:].rearrange("p (h i) -> p h i", h=2),
                                                          in0=pss[:, 0:128].unsqueeze(1).to_broadcast([128, 2, 128]),
                                                          in1=Dt[:].rearrange("p (h i) -> p h i", h=2), op=ALU.mult), reads=[Ad, Dd, Dd2], writes=[Pd])
                else:
                    k.op("dve", lambda e: e.tensor_tensor(out=Pt[:], in0=pss, in1=Dt[:], op=ALU.mult), reads=[Ad, Dd, Dd2], writes=[Pd])
                res["P"] = (Pt, Pd)
                yield

            def scalar_S(d, c, rows, shared_qk, first, with_out, res, oa=None):
                oacc_, oacc_d_ = oa if oa is not None else (oacc, oacc_d)
                b1, b1d = psBC[d][:, 0:512], pdep[4 + 2 * d]
                b2, b2d = psum[d], pdep[d]
                po_a = b1[:, 0:130]
                po_b = b1[:, 130:260]
                kv = res["kv"]
                if with_out:
                    Pt, Pd = res["P"]
                    for h in range(2):
                        k.op("pe", lambda e, h=h: e.matmul(po_a[:, h * 65:(h + 1) * 65], lhsT=Pt[:, h * 128:(h + 1) * 128], rhs=vtok[:, c, h, :],
                                                            start=True, stop=True), reads=[Pd, vtok_d[c]], writes=[b1d])
                    if shared_qk:
                        k.op("pe", lambda e: e.matmul(po_b, lhsT=qT[0:64, tcols(c)], rhs=Sbf[d][0:64].rearrange("p h e -> p (h e)"), start=True, stop=True),
                             reads=[qT_d[c], Sbf_d[d]], writes=[b1d])
                    else:
                        for h in range(2):
                            k.op("pe", lambda e, h=h: e.matmul(po_b, lhsT=qTx[:, h, tcols(c)], rhs=Sbf[d][:].rearrange("p h e -> p (h e)"), start=(h == 0), stop=(h == 1)),
                                 reads=[qT_d[c], qT_d2[c], Sbf_d[d]], writes=[b1d])
                yield
                if shared_qk:
                    for h in range(2):
                        r_ = rows(h)
                        k.op("dve", lambda e, h=h, r_=r_: e.scalar_tensor_tensor(out=S32[d][r_, h, :], in0=S32[d][r_, h, :], scalar=SM[r_, c, d, 3, h:h + 1],
                                                                                   in1=kv[r_, h * 65:(h + 1) * 65], op0=ALU.mult, op1=ALU.add),
                             reads=[b2d, SM_d], writes=[S_dh[d][h]])
                else:
                    k.op("dve", lambda e: e.scalar_tensor_tensor(out=S32[d][:].rearrange("p h e -> p (h e)"), in0=S32[d][:].rearrange("p h e -> p (h e)"),
                                                                 scalar=EGLD[:, c, d:d + 1], in1=kv, op0=ALU.mult, op1=ALU.add),
                         reads=[b2d, EGLD_d], writes=S_dh[d])
                if with_out:
                    t1, t1d = t1w[d].next()
                    t1d2 = xd(t1, 2)
                    k.op("dve", lambda e: e.tensor_tensor(out=t1[:], in0=po_b.rearrange("p (h e) -> p h e", h=2),
                                                          in1=SM[:, c, d, 1, :].unsqueeze(2).to_broadcast([128, 2, 65]), op=ALU.mult),
                         reads=[b1d, SM_d], writes=[t1d])
                yield
                k.op("act", lambda e: e.copy(out=Sbf[d][:], in_=S32[d][:]), reads=S_dh[d], writes=[Sbf_d[d]])
                if with_out:
                    if not first:
                        k.op("pool", lambda e: e.tensor_tensor(out=t1[:], in0=t1[:], in1=oacc_[:, c], op=ALU.add), reads=[oacc_d_[c], t1d2], writes=[t1d])
                    yield
                    k.op("dve", lambda e: e.tensor_tensor(out=oacc_[:, c], in0=po_a.rearrange("p (h e) -> p h e", h=2), in1=t1[:], op=ALU.add),
                         reads=[b1d, t1d, t1d2], writes=[oacc_d_[c]])
                yield

            def gla_P(d, c, res):
                la_c = lag[:, c, d, :]
                A, Ad = psum[2 + d], pdep[2 + d]
                b2, b2d = psBC[d][:, 512:1024], pdep[5 + 2 * d]
                pgt = A[0:64, 0:128]
                pss = A[:, 256:512]
                pga = b2[:, 0:64]
                eg, egd = egw[d].next()
                k.op("pe", lambda e: e.matmul(pgt, lhsT=la_c, rhs=TRI[d], start=True, stop=True), reads=[lag_d[c], cdep], writes=[Ad])
                k.op("pe", lambda e: e.matmul(pga, lhsT=STR[d], rhs=la_c, start=True, stop=True), reads=[lag_d[c], cdep], writes=[b2d])
                yield
                egd2 = xd(eg, 2)
                k.op("act", lambda e: e.activation(out=eg[0:64, 0:128], in_=pgt, func=AF.Exp), reads=[Ad], writes=[egd])
                k.op("act", lambda e: e.activation(out=eg[0:64, 128:256], in_=pgt, func=AF.Exp, scale=-1.0), reads=[Ad], writes=[egd2])
                gw_, gwd_ = gww[d].next()
                k.op("act", lambda e: e.activation(out=gw_[:, 0:64], in_=pga, func=AF.Exp), reads=[b2d], writes=[gwd_])
                el, eld = eglw[d].next()
                last = 127 if d == 0 else 0
                k.op("act", lambda e: e.activation(out=el[:, 0:1], in_=pgt[:, last:last + 1], func=AF.Exp), reads=[Ad], writes=[eld])
                yield
                qg, qgd = qgw[d].next()
                qgd2 = xd(qg, 2)
                for h in range(2):
                    r_ = slice(32 * h, 32 * h + 32)
                    k.op("dve", lambda e, h=h, r_=r_: e.tensor_tensor(out=qg[r_, 128 * (1 + h):128 * (2 + h)], in0=qT[r_, tcols(c)], in1=eg[r_, 0:128], op=ALU.mult),
                         reads=[qT_d[c], egd], writes=[qgd if h == 0 else qgd2])
                qgd3 = xd(qg, 3)
                k.op("pool", lambda e: e.tensor_tensor(out=qg[0:64, 0:128], in0=kT[0:64, tcols(c)], in1=eg[0:64, 128:256], op=ALU.mult),
                     reads=[kT_d[c], egd2], writes=[qgd3])
                kg, kgd = kgw[d].next()
                k.op("pool", lambda e: e.tensor_tensor(out=kg[:, 0:64], in0=ktok[:, c, 0:64], in1=gw_[:, 0:64], op=ALU.mult),
                     reads=[ktok_d[c], gwd_], writes=[kgd])
                yield
                k.op("pe", lambda e: e.matmul(pss, lhsT=qg[0:64, 0:128], rhs=qg[0:64, 128:384], start=True, stop=True), reads=[qgd, qgd2, qgd3], writes=[Ad])
                kvo = 130 * (kvpar[d] % 3)
                kvpar[d] += 1
                kv = psum[d][0:64, kvo:kvo + 130]
                res["kv"] = kv
                k.op("pe", lambda e: e.matmul(kv, lhsT=kg[:, 0:64], rhs=vtok[:, c].rearrange("p h e -> p (h e)"), start=True, stop=True),
                     reads=[kgd, vtok_d[c]], writes=[pdep[d]])
                yield
                Pt, Pd = PTw[d].next()
                k.op("dve", lambda e: e.tensor_tensor(out=Pt[:], in0=pss, in1=MSK[d], op=ALU.mult), reads=[Ad, cdep], writes=[Pd])
                res["P"] = (Pt, Pd, qg, [qgd, qgd2], kg, kgd, el, eld)
                yield

            def gla_S(d, c, first, with_out, res):
                Pt, Pd, qg, qgds, kg, kgd, eg, egd = res["P"]
                b1, b1d = psBC[d][:, 0:512], pdep[4 + 2 * d]
                b2, b2d = psum[d], pdep[d]
                po = b1[:, 0:130]
                kv = res["kv"]
                if with_out:
                    for h in range(2):
                        k.op("pe", lambda e, h=h: e.matmul(po[:, h * 65:(h + 1) * 65], lhsT=Pt[:, h * 128:(h + 1) * 128], rhs=vtok[:, c, h, :],
                                                            start=True, stop=False), reads=[Pd, vtok_d[c]], writes=[b1d])
                        k.op("pe", lambda e, h=h: e.matmul(po[:, h * 65:(h + 1) * 65], lhsT=qg[0:64, 128 * (1 + h):128 * (2 + h)], rhs=Sbf[d][0:64, h, :],
                                                            start=False, stop=True), reads=qgds + [Sbf_d[d]], writes=[b1d])
                yield
                last = 127 if d == 0 else 0
                for h in range(2):
                    r_ = slice(32 * h, 32 * h + 32)
                    k.op("dve", lambda e, h=h, r_=r_: e.scalar_tensor_tensor(out=S32[d][r_, h, :], in0=S32[d][r_, h, :], scalar=eg[r_, 0:1],
                                                                               in1=kv[r_, h * 65:(h + 1) * 65], op0=ALU.mult, op1=ALU.add),
                         reads=[b2d, egd], writes=[S_dh[d][h]])
                yield
                k.op("pool", lambda e: e.tensor_copy(out=Sbf[d][0:64], in_=S32[d][0:64]), reads=S_dh[d], writes=[Sbf_d[d]])
                if with_out:
                    if first:
                        k.op("dve", lambda e: e.tensor_copy(out=oacc[:, c], in_=po.rearrange("p (h e) -> p h e", h=2)), reads=[b1d], writes=[oacc_d[c]])
                    else:
                        k.op("dve", lambda e: e.tensor_tensor(out=oacc[:, c], in0=po.rearrange("p (h e) -> p h e", h=2), in1=oacc[:, c], op=ALU.add),
                             reads=[b1d], writes=[oacc_d[c]])
                yield

            gww = [rot("gww", 2, [128, 64], F32, ms) for _ in range(2)]
            eglw = [rot("eglw", 4, [64, 2], F32, ms) for _ in range(2)]

            def run_scan(P_fn, S_fn, sep=False, LA=2):
                for d in range(2):
                    k.op("dve", lambda e, d=d: e.memset(S32[d][:], 0.0), writes=S_dh[d])
                    k.op("pool", lambda e, d=d: e.memset(Sbf[d][:], 0.0), writes=[Sbf_d[d]])
                    k.op("dve", lambda e, d=d: e.memset(psum[d][:, 0:390], 0.0), writes=[pdep[d]])
                    kvpar[d] = 0
                order = [[0, 1] + list(range(2, NT)), [1, 0] + list(range(NT - 1, 1, -1))]
                seen = set()
                resP = [dict(), dict()]
                for i in range(NT + LA):
                    gens = []
                    for d in range(2):
                        if i < NT:
                            c = order[d][i]
                            wo = ctx_out or c >= 2
                            resP[d][i] = {}
                            gens.append(P_fn(d, c, resP[d][i], wo))
                    for d in range(2):
                        j = i - LA
                        if j >= 0:
                            c = order[d][j]
                            wo = ctx_out or c >= 2
                            first = sep or (c not in seen)
                            if wo:
                                seen.add(c)
                            gens.append(S_fn(d, c, first, wo, resP[d].pop(j)))
                    alive = list(gens)
                    while alive:
                        nxt = []
                        for g in alive:
                            try:
                                next(g)
                                nxt.append(g)
                            except StopIteration:
                                pass
                        alive = nxt

            def head_rms(o3, o3_reads, c, center, nw_off, gate_ap, gate_reads, dst, dst_writes, gid=None):
                sm, smd = fsm.next(gid)
                f1, f1d = fin.next(gid)
                if center:
                    k.op("dve", lambda e: e.tensor_reduce(out=sm[:, 0:2], in_=o3, axis=AX.X, op=ALU.add), reads=o3_reads, writes=[smd])
                    k.op("dve", lambda e: e.tensor_scalar(out=sm[:, 0:2], in0=sm[:, 0:2], scalar1=-1.0 / 64, scalar2=None, op0=ALU.mult), writes=[smd])
                    yield
                    k.op("dve", lambda e: e.tensor_tensor(out=f1[:], in0=o3, in1=sm[:, 0:2].unsqueeze(2).to_broadcast([128, 2, 64]), op=ALU.add),
                         reads=o3_reads + [smd], writes=[f1d])
                else:
                    k.op("pool", lambda e: e.tensor_copy(out=f1[:], in_=o3), reads=o3_reads, writes=[f1d])
                f2, f2d = fin2.next(gid)
                k.op("act", lambda e: e.activation(out=f2[:].rearrange("p h e -> p (h e)"), in_=f1[:].rearrange("p h e -> p (h e)"), func=AF.Square), reads=[f1d], writes=[f2d])
                yield
                k.op("dve", lambda e: e.tensor_reduce(out=sm[:, 2:4], in_=f2[:], axis=AX.X, op=ALU.add), reads=[f2d], writes=[smd])
                yield
                k.op("act", lambda e: e.activation(out=sm[:, 4:6], in_=sm[:, 2:4], func=AF.Sqrt, scale=1.0 / 64, bias=EPS), writes=[smd])
                yield
                k.op("dve", lambda e: e.reciprocal(out=sm[:, 6:8], in_=sm[:, 4:6]), writes=[smd])
                k.op("pool", lambda e: e.tensor_tensor(out=f2[:], in0=gate_ap.rearrange("p (h e) -> p h e", h=2), in1=nwb[:, nw_off:nw_off + 128].rearrange("p (h e) -> p h e", h=2), op=ALU.mult),
                     reads=[nwb_d] + gate_reads, writes=[f2d])
                yield
                k.op("dve", lambda e: e.tensor_tensor(out=f1[:], in0=f1[:], in1=sm[:, 6:8].unsqueeze(2).to_broadcast([128, 2, 64]), op=ALU.mult),
                     reads=[smd], writes=[f1d])
                yield
                k.op("dve", lambda e: e.tensor_tensor(out=dst.rearrange("p (h e) -> p h e", h=2), in0=f1[:], in1=f2[:], op=ALU.mult),
                     reads=[f1d, f2d], writes=dst_writes)
                yield

            def y_to_yT(src, src_d, chunk, c):
                pt, ptd = PT_.next()
                ptb = pt[:].bitcast(BF16)
                k.op("pe", lambda e: e.transpose(ptb[:, 0:128], src, IDB), reads=[src_d, cdep], writes=[ptd])
                k.op("act", lambda e: e.copy(out=yT[:, chunk, tcols(c)], in_=ptb[:, 0:128]), reads=[ptd], writes=[yT_d[c]])

            def prep_qkvg(mix, hp, qc, kc, vc, gc, gate_func, extra_cols, extra_cb=None):
                wt, wd, ncols = load_wblk([(qc, 128), (kc, 128), (vc, 128), (gc, 128)] + extra_cols)

                def tile(c, gid):
                    pa, pad = PA4.next(gid)
                    inproj_tok(wt, wd, 512, c, pa, pad)
                    if extra_cols:
                        pe_, ped = PT_.next()
                        inproj_tok(wt, wd, ncols - 512, c, pe_, ped, off=0, w0=512)
                        extra_cb(c, pe_, ped)
                    yield
                    ut, ud = usb.next(gid)
                    k.op("act", lambda e: e.copy(out=ut[:], in_=pa[:]), reads=[pad], writes=[ud])
                    rt, rd = ropep.next(gid)
                    k.dma("sp", rt[:], rope_d[mix, c], writes=[rd])
                    yield
                    qk, qkd = qkp.next(gid)
                    if mix == 0 and c >= 2:
                        t1, t1d = t1p.next(gid)
                        t2, t2d = t2p.next(gid)
                        k.op("dve", lambda e: e.tensor_tensor(out=t1[:], in0=ut[:, 0:256], in1=rt[:, 0, :], op=ALU.mult),
                             reads=[ud, rd], writes=[t1d])
                        u4 = ut[:, 0:256].rearrange("p (g b e) -> p g b e", b=2, e=16)
                        s4 = rt[:, 1, :].rearrange("p (g b e) -> p g b e", b=2, e=16)
                        o4 = t2[:].rearrange("p (g b e) -> p g b e", b=2, e=16)
                        k.op("pool", lambda e: e.tensor_tensor(out=o4[:, :, 0, :], in0=u4[:, :, 1, :], in1=s4[:, :, 0, :], op=ALU.mult),
                             reads=[ud, rd], writes=[t2d])
                        k.op("pool", lambda e: e.tensor_tensor(out=o4[:, :, 1, :], in0=u4[:, :, 0, :], in1=s4[:, :, 1, :], op=ALU.mult),
                             reads=[ud, rd], writes=[t2d])
                        yield
                        k.op("dve", lambda e: e.tensor_tensor(out=qk[:], in0=t1[:], in1=t2[:], op=ALU.add),
                             reads=[t1d, t2d], writes=[qkd])
                    else:
                        k.op("dve", lambda e: e.tensor_tensor(out=qk[:], in0=ut[:, 0:256], in1=rt[:, 0, :], op=ALU.mult),
                             reads=[ud, rd], writes=[qkd])
                    k.op("pool", lambda e: e.tensor_copy(out=vtok[:, c, :, 0:64], in_=ut[:, 256:384].rearrange("p (h e) -> p h e", h=2)),
                         reads=[ud], writes=[vtok_d[c]])
                    k.op("act", lambda e: e.activation(out=gate[:, c, :], in_=ut[:, 384:512], func=gate_func), reads=[ud], writes=[gate_d[c]])
                    yield
                    k.op("pool", lambda e: e.tensor_copy(out=ktok[:, c, :], in_=qk[:, 128:256]), reads=[qkd], writes=[ktok_d[c]])
                    ptq, ptqd = PT_.next()
                    ptqb = ptq[:].bitcast(BF16)
                    k.op("pe", lambda e: e.transpose(ptqb[:, 0:128], qk[:, 0:128], IDB), reads=[qkd, cdep], writes=[ptqd])
                    k.op("pe", lambda e: e.transpose(ptqb[:, 128:256], qk[:, 128:256], IDB), reads=[qkd, cdep], writes=[ptqd])
                    k.op("act", lambda e: e.copy(out=qTx[0:64, 0, tcols(c)], in_=ptqb[0:64, 0:128]), reads=[ptqd], writes=[qT_d[c]])
                    k.op("act", lambda e: e.copy(out=qTx[64:128, 1, tcols(c)], in_=ptqb[64:128, 0:128]), reads=[ptqd], writes=[qT_d2[c]])
                    k.op("act", lambda e: e.copy(out=kT[:, tcols(c)], in_=ptqb[:, 128:256]), reads=[ptqd], writes=[kT_d[c]])
                    yield

                return [(lambda gid, c=c: tile(c, gid)) for c in range(NT)]

            fin_pending = set()

            def merged_run(pend, prep):
                merged = []
                fi = 0
                for i, p in enumerate(prep):
                    while fi < len(pend) and fi < i + 3:
                        merged.append(pend[fi])
                        fi += 1
                    merged.append(p)
                merged.extend(pend[fi:])
                run_window(merged, 4 if pend else 2, limits={"prep": 2, None: 3})

            def guard_prep(facts):
                def mk(c, f):
                    def g(gid):
                        assert c not in fin_pending, "prep tile started before finalize of the same tile finished"
                        return f(gid)
                    g.ready = lambda: c not in fin_pending
                    g.kind = "prep"
                    return g
                return [mk(c, f) for c, f in enumerate(facts)]

            def ret_setup(hp):
                k.dma("sp", prm[:, 0:4].rearrange("p (d h) -> p d h", d=2), ret_logit[l][:, 2 * hp:2 * hp + 2].partition_broadcast(128), writes=[prm_d])
                softplus_neg(prm[:, 0:4], prm[:, 0:4], [prm_d], [prm_d])
                if hp == 0:
                    k.dma("sp", nwb[:], ret_nw[l:l + 1, :].partition_broadcast(128), writes=[nwb_d])
                for c in range(NT):
                    k.op("pool", lambda e, c=c: e.memset(lai[:, c], 0.0), writes=[lai_d[c]])
                    k.op("pool", lambda e, c=c: e.tensor_copy(out=lai[:, c, :, 0, :], in_=prm[:, 0:4].rearrange("p (d h) -> p d h", d=2)),
                         reads=[prm_d], writes=[lai_d[c]])

            def ret_fin(hp):
                def tile(c, gid):
                    fb, fbd = finb.next(gid)
                    yield from head_rms(oacc[:, c, :, 0:64], [oacc_d[c]], c, True, 128 * hp, gate[:, c, :], [gate_d[c]], fb[:], [fbd], gid)
                    y_to_yT(fb[:], fbd, 0 + hp, c)
                    fin_pending.discard(c)
                    yield
                for c in tiles_out:
                    fin_pending.add(c)
                return [(lambda gid, c=c: tile(c, gid)) for c in tiles_out]

            def mixer_ret_all():
                pend = []
                R64 = lambda h: slice(64 * h, 64 * h + 64)
                for hp in range(2):
                    base = 128 * hp
                    ret_setup(hp)
                    if hp == 0:
                        chk("r1")
                    prep = guard_prep(prep_qkvg(0, hp, base, 256 + base, 512 + base, 768 + base, AF.Silu, []))
                    merged_run(pend, prep)
                    if l == 0 and hp == 0:
                        k.barrier()
                        dump("d_qT", qT[:], [128, NTOK], qT_d)
                        dump("d_kT", kT[:], [128, NTOK], kT_d)
                        dump("d_vtok", vtok[:].rearrange("p c h e -> p (c h e)"), [128, NT * 130], vtok_d)
                        dump("d_gate", gate[:].rearrange("p c e -> p (c e)"), [128, NT * 128], gate_d)
                        dump("d_lai", lai[:].rearrange("p c d s h -> p (c d s h)"), [128, NT * 8], lai_d, q="sp")
                        chk("r2")
                    scalar_pre(False)
                    run_scan(lambda d, c, res, wo: scalar_P(d, c, R64, False, res, wo),
                             lambda d, c, first, wo, res: scalar_S(d, c, R64, False, first, wo, res))
                    if l == 0 and hp == 0:
                        k.barrier()
                        dump("d_oacc", oacc[:].rearrange("p c h e -> p (c h e)"), [128, NT * 130], oacc_d, q="sp")
                        chk("r3")
                    pend = ret_fin(hp)
                run_window(pend, 2)

            def mixer_mlstm_all():
                pend = []
                R64 = lambda h: slice(64 * h, 64 * h + 64)
                oacc2 = ygs[:].rearrange("p a b -> p (a b)").bitcast(F32).rearrange("p (c h e) -> p c h e", c=NT, h=2)
                oas = [(oacc, oacc_d), (oacc2, ygs_d)]
                k.dma("sp", nwb[:], ml_nw[l:l + 1, :].partition_broadcast(128), writes=[nwb_d])

                def gates_cb(c, pe_, ped):
                    sm, smd = gsm.next()
                    k.op("dve", lambda e: e.tensor_tensor(out=sm[:, 0:8], in0=pe_[:, 0:8], in1=prm[:, 0:8], op=ALU.add),
                         reads=[ped, prm_d], writes=[smd])
                    g4 = sm[:, 0:8].rearrange("p (d s h) -> p d s h", d=2, s=2)
                    k.op("pool", lambda e: e.tensor_copy(out=lai[:, c, :, 1, :], in_=g4[:, :, 0, :]), reads=[smd], writes=[lai_d[c]])
                    k.op("act", lambda e: e.activation(out=sm[:, 8:12].rearrange("p (d h) -> p d h", d=2), in_=g4[:, :, 1, :], func=AF.Exp, scale=-1.0), writes=[smd])
                    k.op("act", lambda e: e.activation(out=sm[:, 8:12], in_=sm[:, 8:12], func=AF.Ln, bias=1.0), writes=[smd])
                    k.op("dve", lambda e: e.tensor_scalar(out=lai[:, c, :, 0, :], in0=sm[:, 8:12].rearrange("p (d h) -> p d h", d=2), scalar1=-1.0, scalar2=None, op0=ALU.mult),
                         reads=[smd], writes=[lai_d[c]])

                def fin_facts(hp):
                    def tile(c, gid):
                        sm, smd = fsm.next(gid)
                        f0, f0d = fin2.next(gid)
                        for d_, (oa_t, oa_dd) in enumerate(oas):
                            o_ = 4 * d_
                            k.op("dve", lambda e: e.tensor_scalar(out=sm[:, o_ + 2:o_ + 4], in0=oa_t[:, c, :, 64], scalar1=-1.0, scalar2=None, op0=ALU.mult),
                                 reads=[oa_dd[c]], writes=[smd])
                            k.op("dve", lambda e: e.tensor_tensor(out=sm[:, o_:o_ + 2], in0=oa_t[:, c, :, 64], in1=sm[:, o_ + 2:o_ + 4], op=ALU.max),
                                 reads=[oa_dd[c]], writes=[smd])
                            yield
                            k.op("dve", lambda e: e.tensor_scalar(out=sm[:, o_:o_ + 2], in0=sm[:, o_:o_ + 2], scalar1=1.0, scalar2=None, op0=ALU.max), writes=[smd])
                            k.op("dve", lambda e: e.reciprocal(out=sm[:, o_ + 2:o_ + 4], in_=sm[:, o_:o_ + 2]), writes=[smd])
                            yield
                        k.op("dve", lambda e: e.tensor_tensor(out=f0[:], in0=oacc[:, c, :, 0:64], in1=sm[:, 2:4].unsqueeze(2).to_broadcast([128, 2, 64]), op=ALU.mult),
                             reads=[oacc_d[c], smd], writes=[f0d])
                        f9, f9d = fin.next(gid)
                        k.op("dve", lambda e: e.tensor_tensor(out=f9[:], in0=oacc2[:, c, :, 0:64], in1=sm[:, 6:8].unsqueeze(2).to_broadcast([128, 2, 64]), op=ALU.mult),
                             reads=[ygs_d[c], smd], writes=[f9d])
                        yield
                        k.op("pool", lambda e: e.tensor_tensor(out=f0[:], in0=f0[:], in1=f9[:], op=ALU.add), reads=[f9d], writes=[f0d])
                        yield
                        fb, fbd = finb.next(gid)
                        yield from head_rms(f0[:], [f0d], c, False, 128 * hp, gate[:, c, :], [gate_d[c]], fb[:], [fbd], gid)
                        y_to_yT(fb[:], fbd, 6 + hp, c)
                        fin_pending.discard(c)
                        yield
                    for c in tiles_out:
                        fin_pending.add(c)
                    return [(lambda gid, c=c: tile(c, gid)) for c in tiles_out]

                for hp in range(2):
                    base = 2584 + 128 * hp
                    gcols = [(3608 + d * 8 + s_ * 4 + 2 * hp, 2) for d in range(2) for s_ in range(2)]
                    k.dma("sp", prm[:, 0:8].rearrange("p (d s h) -> p d s h", d=2, s=2), ml_gb[l][:, :, 2 * hp:2 * hp + 2].partition_broadcast(128), writes=[prm_d])
                    prep = guard_prep(prep_qkvg(1, hp, base, 256 + base, 512 + base, 768 + base, AF.Sigmoid, gcols, gates_cb))
                    merged_run(pend, prep)
                    scalar_pre(False)
                    run_scan(lambda d, c, res, wo: scalar_P(d, c, R64, False, res, wo),
                             lambda d, c, first, wo, res: scalar_S(d, c, R64, False, True, wo, res, oa=oas[d]), sep=True)
                    pend = fin_facts(hp)
                run_window(pend, 2)

            gsm = rot("gsm", 4, [128, 12], F32, ms)

            def mixer_gla_all():
                gla_zero_qg()
                k.dma("sp", nwb[:], gla_nw[l:l + 1, :].partition_broadcast(128), writes=[nwb_d])
                pend = []
                for hp in range(2):
                    wt, wd, ncols = load_wblk([(1024 + 64 * hp, 64), (1152 + 64 * hp, 64), (1280 + 128 * hp, 128), (1536 + 128 * hp, 128), (1792, 16)])
                    k.op("dve", lambda e: e.memset(prm32[:], 0.0), writes=[prm32_d])
                    for d in range(2):
                        k.dma("sp", prm32[0:16, d * 64:(d + 1) * 64], gla_gw[l, d][:, 64 * hp:64 * hp + 64], writes=[prm32_d])
                        k.dma("sp", prm32[16:17, d * 64:(d + 1) * 64], gla_gb[l, d:d + 1, 64 * hp:64 * hp + 64], writes=[prm32_d])

                    def ptile(c, gid, wt=wt, wd=wd, ncols=ncols):
                        pa, pad = PA4.next(gid)
                        inproj_tok(wt, wd, ncols, c, pa, pad)
                        yield
                        ut, ud = usb.next(gid)
                        k.op("act", lambda e: e.copy(out=ut[:, 0:400], in_=pa[:, 0:400]), reads=[pad], writes=[ud])
                        yield
                        qk, qkd = qkp.next(gid)
                        k.op("dve", lambda e: e.tensor_scalar(out=qk[:, 0:64], in0=ut[:, 0:64], scalar1=32.0 ** -0.5, scalar2=None, op0=ALU.mult),
                             reads=[ud], writes=[qkd])
                        k.op("pool", lambda e: e.tensor_copy(out=ktok[:, c, 0:64], in_=ut[:, 64:128]), reads=[ud], writes=[ktok_d[c]])
                        k.op("pool", lambda e: e.tensor_copy(out=vtok[:, c, :, 0:64], in_=ut[:, 128:256].rearrange("p (h e) -> p h e", h=2)),
                             reads=[ud], writes=[vtok_d[c]])
                        k.op("act", lambda e: e.activation(out=gate[:, c, :], in_=ut[:, 256:384], func=AF.Silu), reads=[ud], writes=[gate_d[c]])
                        pt, ptd = PT_.next()
                        k.op("pe", lambda e: e.transpose(pt[0:16, 0:128], ut[:, 384:400], IDF), reads=[ud, cdep], writes=[ptd])
                        lt, ltd = lrp.next(gid)
                        k.op("pool", lambda e: e.memset(lt[:], 1.0), writes=[ltd])
                        k.op("act", lambda e: e.copy(out=lt[0:16, :], in_=pt[0:16, 0:128]), reads=[ptd], writes=[ltd])
                        yield
                        k.op("dve", lambda e: e.tensor_copy(out=qk[:, 64:128], in_=ktok[:, c, 0:64]), reads=[ktok_d[c]], writes=[qkd])
                        pz, pzd = PT_.next()
                        k.op("pe", lambda e: e.matmul(pz[:, 0:128], lhsT=lt[:], rhs=prm32[:, 0:128], start=True, stop=True),
                             reads=[ltd, prm32_d], writes=[pzd])
                        dst = lag[:, c].rearrange("p d e -> p (d e)")
                        k.op("act", lambda e: e.activation(out=dst, in_=pz[:, 0:128], func=AF.Exp, scale=-1.0), reads=[pzd], writes=[lag_d[c]])
                        yield
                        k.op("act", lambda e: e.activation(out=dst, in_=dst, func=AF.Ln, bias=1.0), writes=[lag_d[c]])
                        ptq, ptqd = PT_.next()
                        ptqb = ptq[:].bitcast(BF16)
                        k.op("pe", lambda e: e.transpose(ptqb[0:64, 0:128], qk[:, 0:64], IDB), reads=[qkd, cdep], writes=[ptqd])
                        k.op("pe", lambda e: e.transpose(ptqb[0:64, 128:256], qk[:, 64:128], IDB), reads=[qkd, cdep], writes=[ptqd])
                        k.op("act", lambda e: e.copy(out=qT[0:64, tcols(c)], in_=ptqb[0:64, 0:128]), reads=[ptqd], writes=[qT_d[c]])
                        k.op("act", lambda e: e.copy(out=kT[0:64, tcols(c)], in_=ptqb[0:64, 128:256]), reads=[ptqd], writes=[kT_d[c]])
                        yield
                        k.op("dve", lambda e: e.tensor_scalar(out=dst, in0=dst, scalar1=-1.0 / 16.0, scalar2=None, op0=ALU.mult), writes=[lag_d[c]])
                        yield

                    prep = guard_prep([(lambda gid, c=c, ptile=ptile: ptile(c, gid)) for c in range(NT)])
                    merged_run(pend, prep)
                    run_scan(lambda d, c, res, wo: gla_P(d, c, res), gla_S)

                    def ftile(c, gid, hp=hp):
                        fb, fbd = finb.next(gid)
                        yield from head_rms(oacc[:, c, :, 0:64], [oacc_d[c]], c, False, 128 * hp, gate[:, c, :], [gate_d[c]], fb[:], [fbd], gid)
                        y_to_yT(fb[:], fbd, 2 + hp, c)
                        fin_pending.discard(c)
                        yield
                    for c in tiles_out:
                        fin_pending.add(c)
                    pend = [(lambda gid, c=c, ftile=ftile: ftile(c, gid)) for c in tiles_out]
                run_window(pend, 2)

            prm32 = sb("prm32", [32, 128], F32, ms)
            prm32_d = Dep()
            lrp = rot("lrT", 2, [32, 128], F32, ms)
            cvw = sb("cvw", [128, 8], F32, ms)
            cvw_d = Dep()
            accp = Rot([(shr4[:, 0:512], Dep()), (shr4[:, 512:1024], Dep())])

            def mixer_ssd(g):
                cols = [(1808 + 128 * g, 128)] + [(2576 + 4 * d + 2 * g, 2) for d in range(2)]
                wt, wd, ncols = load_wblk(cols)
                k.dma("sp", prm[:, 0:4].rearrange("p (d h) -> p d h", d=2), ssd_dtb[l][:, 2 * g:2 * g + 2].partition_broadcast(128), writes=[prm_d])
                k.dma("sp", prm[:, 4:8].rearrange("p (d h) -> p d h", d=2), ssd_alog[l][:, 2 * g:2 * g + 2].partition_broadcast(128), writes=[prm_d])
                k.dma("sp", prm[:, 8:10], ssd_dsk[l:l + 1, 2 * g:2 * g + 2].partition_broadcast(128), writes=[prm_d])
                k.op("act", lambda e: e.activation(out=prm[:, 4:8], in_=prm[:, 4:8], func=AF.Exp), reads=[prm_d], writes=[prm_d])
                k.op("dve", lambda e: e.tensor_scalar(out=prm[:, 4:8], in0=prm[:, 4:8], scalar1=-1.0, scalar2=None, op0=ALU.mult), writes=[prm_d])
                for c in range(NT):
                    pa, pad = PA.next()
                    inproj_tok(wt, wd, ncols, c, pa, pad)
                    k.op("act", lambda e, pa=pa, c=c: e.activation(out=gate[:, c, :], in_=pa[:, 0:128], func=AF.Silu), reads=[pad], writes=[gate_d[c]])
                    sm, smd = fsm.next()
                    k.op("dve", lambda e, sm=sm, pa=pa: e.tensor_tensor(out=sm[:, 0:4], in0=pa[:, 128:132], in1=prm[:, 0:4], op=ALU.add),
                         reads=[pad, prm_d], writes=[smd])
                    k.op("act", lambda e, sm=sm: e.activation(out=sm[:, 0:4], in_=sm[:, 0:4], func=AF.Exp), writes=[smd])
                    k.op("act", lambda e, sm=sm: e.activation(out=sm[:, 0:4], in_=sm[:, 0:4], func=AF.Ln, bias=1.0), writes=[smd])
                    k.op("act", lambda e, sm=sm, c=c: e.activation(out=lai[:, c, :, 1, :], in_=sm[:, 0:4].rearrange("p (d h) -> p d h", d=2), func=AF.Ln),
                         reads=[smd], writes=[lai_d[c]])
                    k.op("dve", lambda e, sm=sm, c=c: e.tensor_tensor(out=lai[:, c, :, 0, :], in0=sm[:, 0:4].rearrange("p (d h) -> p d h", d=2),
                                                                       in1=prm[:, 4:8].rearrange("p (d h) -> p d h", d=2), op=ALU.mult),
                         reads=[smd, prm_d], writes=[lai_d[c]])
                blocks = [(2064 + 128 * g, 128, 128 * g, "x"), (2320 + 64 * g, 64, 256 + 64 * g, "B"), (2448 + 64 * g, 64, 384 + 64 * g, "C")]
                XcT = qgw_x
                for (c0, M, ch0, nm) in blocks:
                    wt, wd, _ = load_wblk([(c0, M)])
                    k.dma("sp", cvw[0:M, 0:5], ssd_cw[l][:, ch0:ch0 + M].rearrange("w c -> c w"), writes=[cvw_d], allow_slow_non_contiguous=True)
                    k.dma("sp", cvw[0:M, 5:6], ssd_cb[l:l + 1, ch0:ch0 + M].rearrange("o c -> c o"), writes=[cvw_d], allow_slow_non_contiguous=True)
                    k.op("dve", lambda e: e.memset(aux[:], 0.0), writes=[aux_d])
                    pieces = [(0, 256)] + [(256 + 512 * i, 512) for i in range(4)]
                    for (t0, n) in pieces:
                        pa, pad = PA.next()
                        for kk in range(8):
                            k.op("pe", lambda e, kk=kk, pa=pa, t0=t0, n=n, M=M, wt=wt: e.matmul(pa[0:M, 0:n], lhsT=wt[:, kk, 0:M], rhs=hmodT[:, kk, t0:t0 + n],
                                                                                            start=(kk == 0), stop=(kk == 7)),
                                 reads=[wd] + hmodT_d, writes=[pad])
                        o = t0 + 2 if t0 < 256 else t0 + 6
                        k.op("act", lambda e, pa=pa, o=o, n=n, M=M: e.copy(out=aux[0:M, o:o + n], in_=pa[0:M, 0:n]), reads=[pad], writes=[aux_d])
                    for (t0, n) in pieces:
                        o = t0 if t0 < 256 else t0 + 4
                        ac, acd = accp.next()
                        k.op("dve", lambda e, ac=ac, o=o, n=n, M=M: e.tensor_scalar(out=ac[0:M, 0:n], in0=aux[0:M, o:o + n], scalar1=cvw[0:M, 0:1], scalar2=None, op0=ALU.mult),
                             reads=[aux_d, cvw_d], writes=[acd])
                        for w in range(1, 5):
                            k.op("dve", lambda e, ac=ac, o=o, n=n, M=M, w=w: e.scalar_tensor_tensor(out=ac[0:M, 0:n], in0=aux[0:M, o + w:o + w + n], scalar=cvw[0:M, w:w + 1],
                                                                                                 in1=ac[0:M, 0:n], op0=ALU.mult, op1=ALU.add),
                                 reads=[aux_d, cvw_d], writes=[acd])
                        dstT, dst_d = {"x": (XcT, XcT_d), "B": (kT, kT_d), "C": (qT, qT_d)}[nm]
                        tl = list(range(t0 // 128, (t0 + n) // 128))
                        k.op("act", lambda e, ac=ac, n=n, M=M, dstT=dstT, t0=t0: e.activation(out=dstT[0:M, t0:t0 + n], in_=ac[0:M, 0:n], func=AF.Silu, bias=cvw[0:M, 5:6]),
                             reads=[acd, cvw_d], writes=[dst_d[c] for c in tl])
                for c in range(NT):
                    pt, ptd = PT_.next()
                    ptb = pt[:].bitcast(BF16)
                    k.op("pe", lambda e, ptb=ptb, c=c: e.transpose(ptb[:, 0:128], XcT[:, tcols(c)], IDB), reads=[XcT_d[c], cdep], writes=[ptd])
                    k.op("pe", lambda e, ptb=ptb, c=c: e.transpose(ptb[:, 128:192], kT[0:64, tcols(c)], IDB[0:64, 0:64]), reads=[kT_d[c], cdep], writes=[ptd])
                    k.op("act", lambda e, ptb=ptb, c=c: e.copy(out=vtok[:, c, :, 0:64], in_=ptb[:, 0:128].rearrange("p (h e) -> p h e", h=2)),
                         reads=[ptd], writes=[vtok_d[c]])
                    k.op("act", lambda e, ptb=ptb, c=c: e.copy(out=ktok[:, c, 0:64], in_=ptb[:, 128:192]), reads=[ptd], writes=[ktok_d[c]])
                scalar_pre(True)
                R0 = lambda h: slice(0, 64)
                run_scan(lambda d, c, res, wo: scalar_P(d, c, R0, True, res, wo),
                         lambda d, c, first, wo, res: scalar_S(d, c, R0, True, first, wo, res))
                for c in tiles_out:
                    f1, f1d = fin.next()
                    k.op("dve", lambda e, f1=f1, c=c: e.tensor_tensor(out=f1[:], in0=vtok[:, c, :, 0:64], in1=prm[:, 8:10].unsqueeze(2).to_broadcast([128, 2, 64]), op=ALU.mult),
                         reads=[vtok_d[c], prm_d], writes=[f1d])
                    k.op("dve", lambda e, f1=f1, c=c: e.tensor_tensor(out=f1[:], in0=f1[:], in1=oacc[:, c, :, 0:64], op=ALU.add), reads=[oacc_d[c]], writes=[f1d])
                    k.op("pool", lambda e, f1=f1, c=c: e.tensor_tensor(out=f1[:], in0=f1[:], in1=gate[:, c, :].rearrange("p (h e) -> p h e", h=2), op=ALU.mult),
                         reads=[gate_d[c]], writes=[f1d])
                    k.op("pool", lambda e, f1=f1, c=c: e.tensor_copy(out=ygs[:, c, 128 * g:128 * g + 128].rearrange("p (h e) -> p h e", h=2), in_=f1[:]),
                         reads=[f1d], writes=[ygs_d[c]])
                    jt, jd = fin2.next()
                    k.op("act", lambda e, f1=f1, jt=jt, c=c: e.activation(out=jt[:].rearrange("p h e -> p (h e)"), in_=f1[:].rearrange("p h e -> p (h e)"), func=AF.Square,
                                                                           accum_out=ssq[:, c, g:g + 1]), reads=[f1d], writes=[jd, ygs_d[c]])

            def ssd_finish():
                k.dma("sp", nwb[:], ssd_nw[l:l + 1, :].partition_broadcast(128), writes=[nwb_d])
                for c in tiles_out:
                    sm, smd = fsm.next()
                    k.op("dve", lambda e, sm=sm, c=c: e.tensor_tensor(out=sm[:, 0:1], in0=ssq[:, c, 0:1], in1=ssq[:, c, 1:2], op=ALU.add), reads=[ygs_d[c]], writes=[smd])
                    k.op("act", lambda e, sm=sm: e.activation(out=sm[:, 1:2], in_=sm[:, 0:1], func=AF.Sqrt, scale=1.0 / 256, bias=EPS), writes=[smd])
                    k.op("dve", lambda e, sm=sm: e.reciprocal(out=sm[:, 2:3], in_=sm[:, 1:2]), writes=[smd])
                    t1, t1d = t1p.next()
                    k.op("dve", lambda e, sm=sm, t1=t1, c=c: e.scalar_tensor_tensor(out=t1[:], in0=ygs[:, c, 0:256], scalar=sm[:, 2:3], in1=nwb[:], op0=ALU.mult, op1=ALU.mult),
                         reads=[ygs_d[c], smd, nwb_d], writes=[t1d])
                    qk, qkd = qkp.next()
                    k.op("pool", lambda e, qk=qk, t1=t1: e.tensor_copy(out=qk[:], in_=t1[:]), reads=[t1d], writes=[qkd])
                    for g in range(2):
                        y_to_yT(qk[:, 128 * g:128 * g + 128], qkd, 4 + g, c)

            qgw_x = sb("XcT", [128, NTOK], BF16, ms)
            XcT_d = [Dep() for _ in range(NT)]

            def dump_yT(tag):
                if l == 0:
                    k.barrier()
                    dump("d_yT_" + tag, yT[:].rearrange("p k t -> p (k t)"), [128, 8 * NTOK], yT_d)
                    chk(tag)
            print("SBUF remaining (mixer phase)", nc.sbuf_bytes_remaining)
            mixer_ret_all()
            k.barrier()
            dump_yT("ret")
            mixer_gla_all()
            k.barrier()
            dump_yT("gla")
            for g in range(2):
                mixer_ssd(g)
            ssd_finish()
            k.barrier()
            dump_yT("ssd")
            mixer_mlstm_all()
            k.barrier()
            dump_yT("mlstm")

            wo_t = hmodT
            wo_d = Dep()
            k.dma("pool", wo_t[:, :, 0:1024], w_out[l].rearrange("(k p) n -> p k n", p=128), writes=[wo_d] + hmodT_d)
            G1b = [aux[:, 0:1024], aux[:, 1024:2048]]
            g1d = Dep()
            for r in range(2 if ctx_out else 1):
                for kk in range(8):
                    pa, pad = PA.next()
                    k.op("pe", lambda e, pa=pa, kk=kk, r=r: e.matmul(pa[:, 0:128], lhsT=modT[:, 16 + kk, r:r + 1].to_broadcast([128, 128]), rhs=IDF,
                                                                      start=True, stop=True), reads=[mod_dep, cdep], writes=[pad])
                    k.op("act", lambda e, pa=pa, kk=kk, r=r: e.copy(out=G1b[r][:, kk * 128:(kk + 1) * 128], in_=pa[:, 0:128]), reads=[pad], writes=[g1d, aux_d])
            ygf = ygs[:].rearrange("p a b -> p (a b)").bitcast(F32)
            hop = Rot([(ygf[:, 0:1024], Dep()), (ygf[:, 1024:2048], Dep())])
            for c in tiles_out:
                r = 1 if c < 2 else 0
                ht, hdp = hop.next()
                k.dma("sp", ht, hsrc(l, c), reads=[hd_dep[c]], writes=[hdp])
                for half in range(2):
                    pa, pad = PA.next()
                    for kk in range(8):
                        k.op("pe", lambda e, pa=pa, kk=kk, half=half, c=c: e.matmul(pa[:], lhsT=yT[:, kk, tcols(c)], rhs=wo_t[:, kk, half * 512:(half + 1) * 512],
                                                                                     start=(kk == 0), stop=(kk == 7)), reads=[yT_d[c], wo_d], writes=[pad])
                    ut, ud = usb.next()
                    k.op("dve", lambda e, ut=ut, pa=pa, r=r, half=half: e.tensor_tensor(out=ut[:], in0=pa[:], in1=G1b[r][:, half * 512:(half + 1) * 512], op=ALU.mult),
                         reads=[pad, g1d], writes=[ud])
                    k.op("pool", lambda e, ut=ut, ht=ht, half=half: e.tensor_tensor(out=ht[:, half * 512:(half + 1) * 512], in0=ht[:, half * 512:(half + 1) * 512], in1=ut[:], op=ALU.add),
                         reads=[ud], writes=[hdp])
                k.dma("sp", hd[c * 128:(c + 1) * 128, :], ht, reads=[hdp], writes=[hd_dep[c]])
            k.barrier()
            if l == 0:
                dump("d_hmid", hd, [NTOK, 1024], hd_dep, q="sp")
                chk("outproj")

        with ExitStack() as es:
            psum = [es.enter_context(nc.psum_tensor(k.name("pm"), [128, 512], F32)) for _ in range(8)]
            pdep = [Dep() for _ in range(8)]
            PG = Rot([(psum[0], pdep[0]), (psum[1], pdep[1])])
            PF = Rot([(psum[2], pdep[2]), (psum[3], pdep[3]), (psum[4], pdep[4]), (psum[5], pdep[5])])
            PC = Rot([(psum[6], pdep[6]), (psum[7], pdep[7])])
            H = sb("H", [128, NT, 1024], F32, es)
            H_d = [Dep() for _ in range(NT)]
            XN = sb("XN", [128, NT, 1024], BF16, es)
            XN_d = [Dep() for _ in range(NT)]
            G2b = sb("G2b", [128, 2, 1024], F32, es)
            g2d = Dep()
            AFF = sb("AFF", [128, NT, 16], F32, es)
            POS = sb("POS", [128, NT, 16], F32, es)
            MKT = sb("MKT", [128, NT, 16], BF16, es)
            aff_d = [Dep() for _ in range(NT)]
            pos_d = Dep()
            mkt_d = Dep()
            POST = sb("POST", [16, NTOK], BF16, es)
            post_d = Dep()
            esel_dep = Dep()
            RW = sb("RW", [128, 8, 16], BF16, es)
            rw_d = Dep()
            k.dma("pool", RW[:], router_w[l].rearrange("(k p) n -> p k n", p=128), writes=[rw_d])
            msm = rot("msm", 2, [128, 8], F32, es)
            XSM = sb("XSM", [128, 8, 288], BF16, es)
            XSM_d = Dep()
            jk2 = Rot([(XSM[:].rearrange("p a b -> p (a b)")[:, 0:1024], XSM_d)])
            tiles = list(range(NT)) if ctx_out else list(range(2, NT))

            for r in range(2 if ctx_out else 1):
                for kk in range(8):
                    pa, pad = PG.next()
                    k.op("pe", lambda e, pa=pa, kk=kk, r=r: e.matmul(pa[:, 0:128], lhsT=modT[:, 40 + kk, r:r + 1].to_broadcast([128, 128]), rhs=IDF,
                                                                      start=True, stop=True), reads=[mod_dep, cdep], writes=[pad])
                    k.op("act", lambda e, pa=pa, kk=kk, r=r: e.copy(out=G2b[:, r, kk * 128:(kk + 1) * 128], in_=pa[:, 0:128]), reads=[pad], writes=[g2d])

            with ExitStack() as rs:
                AFFT = sb("AFFT", [16, NTOK], F32, rs)
                afft_d = Dep()
                CMPB = sb("CMPB", [16, 2048], F32, rs)
                CMPC = sb("CMPC", [16, 256], F32, rs)
                bis = sb("bis", [16, 16], F32, rs)
                bis_d = Dep()
                hm2 = rot("hm2", 2, [128, 8, 128], BF16, rs)
                def r_tile(c, gid):
                    r = 1 if c < 2 else 0
                    k.dma("sp", H[:, c, :], hd[c * 128:(c + 1) * 128, :], reads=[hd_dep[c]], writes=[H_d[c]])
                    st, sd_ = msm.next(gid)
                    jt, jd = jk2.next()
                    yield
                    k.op("act", lambda e: e.activation(out=jt[:], in_=H[:, c, :], func=AF.Square, accum_out=st[:, 0:1]), reads=[H_d[c]], writes=[jd, sd_])
                    yield
                    k.op("act", lambda e: e.activation(out=st[:, 1:2], in_=st[:, 0:1], func=AF.Sqrt, scale=1.0 / 1024, bias=EPS), writes=[sd_])
                    yield
                    k.op("dve", lambda e: e.reciprocal(out=st[:, 2:3], in_=st[:, 1:2]), writes=[sd_])
                    yield
                    k.op("dve", lambda e: e.tensor_scalar(out=XN[:, c, :], in0=H[:, c, :], scalar1=st[:, 2:3], scalar2=None, op0=ALU.mult),
                         reads=[H_d[c], sd_], writes=[XN_d[c]])
                    yield
                    pt, ptd = PF.next()
                    ptb = pt[:].bitcast(BF16)
                    for kk in range(8):
                        k.op("pe", lambda e, kk=kk: e.transpose(ptb[:, kk * 128:(kk + 1) * 128], XN[:, c, kk * 128:(kk + 1) * 128], IDB),
                             reads=[XN_d[c], cdep], writes=[ptd])
                    hm, hmd = hm2.next(gid)
                    for kk in range(8):
                        k.op("act", lambda e, kk=kk: e.activation(out=hm[:, kk, :], in_=ptb[:, kk * 128:(kk + 1) * 128], func=AF.Identity,
                                                                   scale=A2[:, kk, r:r + 1], bias=modT[:, 24 + kk, r:r + 1]),
                             reads=[ptd, mod_dep], writes=[hmd])
                    yield
                    pl_, pld = PG.next()
                    for kk in range(8):
                        k.op("pe", lambda e, kk=kk: e.matmul(pl_[:, 0:16], lhsT=hm[:, kk, :], rhs=RW[:, kk, :], start=(kk == 0), stop=(kk == 7)),
                             reads=[hmd, rw_d], writes=[pld])
                    k.op("dve", lambda e: e.tensor_reduce(out=st[:, 3:4], in_=pl_[:, 0:16], axis=AX.X, op=ALU.max), reads=[pld], writes=[sd_])
                    yield
                    k.op("dve", lambda e: e.tensor_scalar(out=st[:, 3:4], in0=st[:, 3:4], scalar1=-1.0, scalar2=None, op0=ALU.mult), writes=[sd_])
                    yield
                    k.op("act", lambda e: e.activation(out=AFF[:, c, :], in_=pl_[:, 0:16], func=AF.Exp, bias=st[:, 3:4], accum_out=st[:, 4:5]),
                         reads=[pld], writes=[sd_, aff_d[c]])
                    yield
                    k.op("dve", lambda e: e.reciprocal(out=st[:, 5:6], in_=st[:, 4:5]), writes=[sd_])
                    yield
                    k.op("dve", lambda e: e.tensor_scalar(out=AFF[:, c, :], in0=AFF[:, c, :], scalar1=st[:, 5:6], scalar2=None, op0=ALU.mult),
                         reads=[sd_], writes=[aff_d[c]])
                    yield
                    pt2, pt2d = PG.next()
                    k.op("pe", lambda e: e.transpose(pt2[0:16, 0:128], AFF[:, c, :], IDF), reads=[aff_d[c], cdep], writes=[pt2d])
                    k.op("act", lambda e: e.copy(out=AFFT[:, tcols(c)], in_=pt2[0:16, 0:128]), reads=[pt2d], writes=[afft_d])
                    yield
                run_window([(lambda gid, c=c: r_tile(c, gid)) for c in tiles], 2)
                segs = [(256, 2048, 256.0, CMPB)] + ([(0, 256, 32.0, CMPC)] if ctx_out else [])
                bdeps = [Dep(), Dep()]

                def bis_gen(si, t0, n, kcap, CM):
                    lo = bis[:, 4 * si + 0:4 * si + 1]
                    cntc = bis[:, 4 * si + 1:4 * si + 2]
                    gew = bis[:, 4 * si + 2:4 * si + 3]
                    bd = bdeps[si]
                    k.op("dve", lambda e: e.memset(lo, 0.0), writes=[bd])
                    w = 0.5
                    for it in range(24):
                        k.op("dve", lambda e, w=w: e.tensor_scalar(out=CM[:, 0:n], in0=AFFT[:, t0:t0 + n], scalar1=lo, scalar2=w, op0=ALU.subtract, op1=ALU.is_gt),
                             reads=[afft_d], writes=[bd])
                        yield
                        k.op("dve", lambda e: e.tensor_reduce(out=cntc, in_=CM[:, 0:n], axis=AX.X, op=ALU.add), writes=[bd])
                        yield
                        k.op("dve", lambda e, w=w: e.tensor_scalar(out=gew, in0=cntc, scalar1=kcap, scalar2=w, op0=ALU.is_ge, op1=ALU.mult), writes=[bd])
                        yield
                        k.op("dve", lambda e: e.tensor_tensor(out=lo, in0=lo, in1=gew, op=ALU.add), writes=[bd])
                        yield
                        w *= 0.5
                run_window([(lambda gid, si=si, sg=sg: bis_gen(si, *sg)) for si, sg in enumerate(segs)], 2)
                for si, (t0, n, kcap, CM) in enumerate(segs):
                    lo = bis[:, 4 * si + 0:4 * si + 1]
                    bis_d = bdeps[si]
                    CMPB_ = CM
                    k.op("dve", lambda e, lo=lo, t0=t0, n=n: e.tensor_scalar(out=CMPB_[:, 0:n], in0=AFFT[:, t0:t0 + n], scalar1=lo, scalar2=None, op0=ALU.is_gt),
                         reads=[afft_d], writes=[bis_d])
                    for c in range(t0 // 128, (t0 + n) // 128):
                        pt2, pt2d = PG.next()
                        cc = c * 128 - t0
                        k.op("pe", lambda e, pt2=pt2, cc=cc: e.transpose(pt2[:, 0:16], CMPB_[:, cc:cc + 128], IDF[0:16, 0:16]), reads=[bis_d, cdep], writes=[pt2d])
                        k.op("act", lambda e, pt2=pt2, c=c: e.copy(out=MKT[:, c, :], in_=pt2[:, 0:16]), reads=[pt2d], writes=[mkt_d])
                        k.op("dve", lambda e, pt2=pt2, c=c: e.tensor_copy(out=POS[:, c, :], in_=pt2[:, 0:16]), reads=[pt2d], writes=[pos_d])
                k.barrier()
            SEL = sb("SEL", [128, NT, 256], BF16, es)
            SEL_d = Dep()
            HID = sb("HID", [128, 12, 288], BF16, es)
            HID_d = Dep()
            YE = sb("YE", [128, 3, 1024], BF16, es)
            YE_d = Dep()
            YE2 = sb("YE2", [128, 3, 1024], BF16, es)
            YE2_d = Dep()
            wp = rot("wp", 4, [128, 8, 512], BF16, es)
            selT = rot("selT", 4, [128, 2, 512], BF16, es)
            sgp = rot("sg", 2, [128, 288], BF16, es)
            affc = sb("affc", [128, 3, 2], F32, es)
            affc_d = Dep()
            for (c_lo, c_hi) in ([(2, NT)] + ([(0, 2)] if ctx_out else [])):
                for c in range(c_lo, c_hi):
                    pp, ppd = PG.next()
                    prev = list(range(c_lo, c))
                    for i, cp in enumerate(prev):
                        k.op("pe", lambda e, pp=pp, cp=cp, i=i: e.matmul(pp[:, 0:16], lhsT=ONEB, rhs=MKT[:, cp, :], start=(i == 0), stop=False),
                             reads=[mkt_d, cdep], writes=[ppd])
                    k.op("pe", lambda e, pp=pp, c=c, prev=prev: e.matmul(pp[:, 0:16], lhsT=TRIB, rhs=MKT[:, c, :], start=(len(prev) == 0), stop=True),
                         reads=[mkt_d, cdep], writes=[ppd])
                    k.op("dve", lambda e, pp=pp, c=c: e.tensor_tensor(out=POS[:, c, :], in0=pp[:, 0:16], in1=POS[:, c, :], op=ALU.mult), reads=[ppd], writes=[pos_d])
                    pt2, pt2d = PG.next()
                    k.op("pe", lambda e, pt2=pt2, c=c: e.transpose(pt2[0:16, 0:128], POS[:, c, :], IDF), reads=[pos_d, cdep], writes=[pt2d])
                    k.op("act", lambda e, pt2=pt2, c=c: e.copy(out=POST[:, tcols(c)], in_=pt2[0:16, 0:128]), reads=[pt2d], writes=[post_d])
            AFFH = sb("AFFH", [128, NT, 16], BF16, es)
            AFFL = sb("AFFL", [128, NT, 16], BF16, es)
            afh_d = Dep()
            for c in tiles:
                st, sd_ = msm.next()
                k.op("act", lambda e, c=c: e.copy(out=AFFH[:, c, :], in_=AFF[:, c, :]), reads=[aff_d[c]], writes=[afh_d])
                sg, sgd = sgp.next()
                k.op("dve", lambda e, c=c, sg=sg: e.tensor_tensor(out=sg[:, 0:16], in0=AFF[:, c, :], in1=AFFH[:, c, :], op=ALU.subtract), reads=[aff_d[c], afh_d], writes=[sgd])
                k.op("act", lambda e, c=c, sg=sg: e.copy(out=AFFL[:, c, :], in_=sg[:, 0:16]), reads=[sgd], writes=[afh_d])

            print("SBUF remaining (moe phase)", nc.sbuf_bytes_remaining)
            if l == 0:
                k.barrier()
                chk("moe_r")
            IOC = CF[:, C_IOC:C_IOC + 256]
            wg_l = [ewg[l, e_].rearrange("(k p) n -> p k n", p=128) for e_ in range(16)]
            wu_l = [ewu[l, e_].rearrange("(k p) n -> p k n", p=128) for e_ in range(16)]
            wd_l = [ewd[l, e_].rearrange("(j p) n -> p j n", p=128) for e_ in range(16)]
            ncc = 3 if ctx_out else 2
            NX = 288 if ctx_out else 256
            YEs = [(YE, YE_d), (YE2, YE2_d)]

            def st_A(ex):
                for c in tiles:
                    n = 32 if c < 2 else 256
                    k.op("dve", lambda e, c=c, n=n: e.tensor_scalar(out=SEL[:, c, 0:n], in0=IOC[:, 0:n], scalar1=POS[:, c, ex:ex + 1], scalar2=None, op0=ALU.is_equal),
                         reads=[pos_d, cdep], writes=[SEL_d])

            def st_B(ex):
                pa_, pad_ = PG.next()
                for cc in range(ncc):
                    tl = [2 + i for i in range(16)] if cc < 2 else [0, 1]
                    M = 128 if cc < 2 else 32
                    co = (cc * 128) if cc < 2 else 0
                    seq = [(c, AFFH) for c in tl] + [(c, AFFL) for c in tl]
                    for i, (c, src) in enumerate(seq):
                        k.op("pe", lambda e, cc=cc, c=c, i=i, n_=len(seq), M=M, co=co, src=src: e.matmul(pa_[0:M, 4 * cc:4 * cc + 1], lhsT=SEL[:, c, co:co + M], rhs=src[:, c, ex:ex + 1],
                                                                                                   start=(i == 0), stop=(i == n_ - 1)), reads=[SEL_d, afh_d], writes=[pad_])
                for cc in range(ncc):
                    M = 128 if cc < 2 else 32
                    k.op("dve", lambda e, cc=cc, M=M: e.tensor_copy(out=affc[0:M, cc, 0:1], in_=pa_[0:M, 4 * cc:4 * cc + 1]),
                         reads=[pad_], writes=[affc_d])
                for kk in range(8):
                    pg_, pgd = PG.next()
                    lt = list(range(2, NT))
                    for i, c in enumerate(lt):
                        k.op("pe", lambda e, kk=kk, c=c, i=i, pg_=pg_: e.matmul(pg_[:, 0:256], lhsT=XN[:, c, kk * 128:(kk + 1) * 128], rhs=SEL[:, c, 0:256],
                                                                                 start=(i == 0), stop=(i == 15)), reads=[XN_d[c], SEL_d], writes=[pgd])
                    k.op("act", lambda e, kk=kk, pg_=pg_: e.activation(out=XSM[:, kk, 0:256], in_=pg_[:, 0:256], func=AF.Identity, scale=A2[:, kk, 0:1], bias=modT[:, 24 + kk, 0:1]),
                         reads=[pgd, mod_dep], writes=[XSM_d])
                    if ctx_out:
                        for i, c in enumerate([0, 1]):
                            k.op("pe", lambda e, kk=kk, c=c, i=i, pg_=pg_: e.matmul(pg_[:, 256:288], lhsT=XN[:, c, kk * 128:(kk + 1) * 128], rhs=SEL[:, c, 0:32],
                                                                                     start=(i == 0), stop=(i == 1)), reads=[XN_d[c], SEL_d], writes=[pgd])
                        k.op("act", lambda e, kk=kk, pg_=pg_: e.activation(out=XSM[:, kk, 256:288], in_=pg_[:, 256:288], func=AF.Identity, scale=A2[:, kk, 1:2], bias=modT[:, 24 + kk, 1:2]),
                             reads=[pgd, mod_dep], writes=[XSM_d])

            def st_C(ex):
                for jg in range(3):
                    wgt, wgd = wp.next()
                    k.dma("pool", wgt[:], wg_l[ex][:, :, jg * 512:(jg + 1) * 512], writes=[wgd])
                    wut, wud = wp.next()
                    k.dma("pool", wut[:], wu_l[ex][:, :, jg * 512:(jg + 1) * 512], writes=[wud])
                    for jj in range(4):
                        j = jg * 4 + jj
                        pgt_, pgtd = PF.next()
                        put_, putd = PF.next()
                        for kk in range(8):
                            k.op("pe", lambda e, kk=kk, jj=jj, pgt_=pgt_, wgt=wgt: e.matmul(pgt_[:, 0:NX], lhsT=wgt[:, kk, jj * 128:(jj + 1) * 128], rhs=XSM[:, kk, 0:NX],
                                                                                         start=(kk == 0), stop=(kk == 7)), reads=[wgd, XSM_d], writes=[pgtd])
                        for kk in range(8):
                            k.op("pe", lambda e, kk=kk, jj=jj, put_=put_, wut=wut: e.matmul(put_[:, 0:NX], lhsT=wut[:, kk, jj * 128:(jj + 1) * 128], rhs=XSM[:, kk, 0:NX],
                                                                                         start=(kk == 0), stop=(kk == 7)), reads=[wud, XSM_d], writes=[putd])
                        sg, sgd = sgp.next()
                        k.op("act", lambda e, sg=sg, pgt_=pgt_: e.activation(out=sg[:, 0:NX], in_=pgt_[:, 0:NX], func=AF.Silu), reads=[pgtd], writes=[sgd])
                        k.op("dve", lambda e, sg=sg, put_=put_, j=j: e.tensor_tensor(out=HID[:, j, 0:NX], in0=put_[:, 0:NX], in1=sg[:, 0:NX], op=ALU.mult),
                             reads=[putd, sgd], writes=[HID_d])

            def st_D(ex, yb):
                YEt, YEd = YEs[yb]
                accs = [(PF.next()) for _ in range(4)] + [PG.next(), PG.next()]
                for jg in range(3):
                    wdt, wdd = wp.next()
                    wv = wdt[:].rearrange("p a b -> p (a b)").rearrange("p (j n) -> p j n", j=4)
                    k.dma("pool", wv, wd_l[ex][:, jg * 4:(jg + 1) * 4, :], writes=[wdd])
                    for cc in range(ncc):
                        M = 128 if cc < 2 else 32
                        co = cc * 128
                        for half in range(2):
                            pa2, pa2d = accs[cc * 2 + half]
                            for jj in range(4):
                                j = jg * 4 + jj
                                k.op("pe", lambda e, pa2=pa2, M=M, co=co, half=half, jj=jj, j=j, wv=wv: e.matmul(pa2[0:M, :], lhsT=HID[:, j, co:co + M], rhs=wv[:, jj, half * 512:(half + 1) * 512],
                                                                                                              start=(j == 0), stop=(j == 11)), reads=[HID_d, wdd], writes=[pa2d])
                for cc in range(ncc):
                    M = 128 if cc < 2 else 32
                    r = 1 if cc == 2 else 0
                    for half in range(2):
                        pa2, pa2d = accs[cc * 2 + half]
                        k.op("dve", lambda e, pa2=pa2, M=M, cc=cc, half=half, r=r: e.scalar_tensor_tensor(out=YEt[0:M, cc, half * 512:(half + 1) * 512], in0=pa2[0:M, :], scalar=affc[0:M, cc, 0:1],
                                                                                                         in1=G2b[0:M, r, half * 512:(half + 1) * 512], op0=ALU.mult, op1=ALU.mult),
                             reads=[pa2d, affc_d, g2d], writes=[YEd])

            def st_E(exs):
                blocks = [(256 + 512 * i, 512, 0) for i in range(4)] + ([(0, 256, 1)] if ctx_out else [])
                for (t0, n, isctx) in blocks:
                    sts = []
                    for bi, ex in enumerate(exs):
                        pb, pbd = PC.next()
                        k.op("pe", lambda e, pb=pb, ex=ex: e.matmul(pb[:, 0:n], lhsT=IDB[0:16, ex:ex + 1].to_broadcast([16, 128]), rhs=POST[:, t0:t0 + n], start=True, stop=True),
                             reads=[cdep, post_d], writes=[pbd])
                        st_, std_ = selT.next()
                        nch = 1 if isctx else 2
                        for j in range(nch):
                            k.op("dve", lambda e, pb=pb, st_=st_, j=j: e.tensor_scalar(out=st_[:, j, 0:n], in0=pb[:, 0:n], scalar1=CF[:, C_IOP + j:C_IOP + j + 1], scalar2=None, op0=ALU.is_equal),
                                 reads=[pbd, cdep], writes=[std_])
                        sts.append((st_, std_))
                    for ti in range(n // 128):
                        c = t0 // 128 + ti
                        for half in range(2):
                            pc_, pcd = PF.next()
                            mms = []
                            for bi in range(len(exs)):
                                st_, std_ = sts[bi]
                                YEt, YEd = YEs[bi]
                                if isctx:
                                    mms.append((st_[0:32, 0, ti * 128:(ti + 1) * 128], YEt[0:32, 2, half * 512:(half + 1) * 512], std_, YEd))
                                else:
                                    for j in range(2):
                                        mms.append((st_[:, j, ti * 128:(ti + 1) * 128], YEt[:, j, half * 512:(half + 1) * 512], std_, YEd))
                            for i, (lh, rh, d1, d2) in enumerate(mms):
                                k.op("pe", lambda e, pc_=pc_, lh=lh, rh=rh, i=i, nm=len(mms): e.matmul(pc_[:], lhsT=lh, rhs=rh, start=(i == 0), stop=(i == nm - 1)),
                                     reads=[d1, d2], writes=[pcd])
                            k.op("dve", lambda e, pc_=pc_, c=c, half=half: e.tensor_tensor(out=H[:, c, half * 512:(half + 1) * 512], in0=pc_[:], in1=H[:, c, half * 512:(half + 1) * 512], op=ALU.add),
                                 reads=[pcd], writes=[H_d[c]])

            st_A(0)
            for p_ in range(8):
                e0, e1 = 2 * p_, 2 * p_ + 1
                st_B(e0)
                st_A(e1)
                st_C(e0)
                st_D(e0, 0)
                st_B(e1)
                if e1 + 1 < 16:
                    st_A(e1 + 1)
                st_C(e1)
                st_D(e1, 1)
                st_E([e0, e1])
            if l < nlayers - 1:
                for c in tiles:
                    k.dma("sp", hd[c * 128:(c + 1) * 128, :], H[:, c, :], reads=[H_d[c]], writes=[hd_dep[c]])
                k.barrier()
                dump("d_hend", hd, [NTOK, 1024], hd_dep, q="sp")
                chk("moe0")
            else:
                FN = G2b[:, 0, :]
                cmb = Rot([(XN[:, 2 * i:2 * i + 2, :].bitcast(F32).rearrange("p a b -> p (a b)"), Dep()) for i in range(4)])
                k.dma("sp", FN, fnw.partition_broadcast(128), reads=[], writes=[g2d])
                for c in range(2, NT):
                    st, sd_ = msm.next()
                    jt, jd = jk2.next()
                    k.op("act", lambda e, c=c, st=st, jt=jt: e.activation(out=jt[:], in_=H[:, c, :], func=AF.Square, accum_out=st[:, 0:1]), reads=[H_d[c]], writes=[jd, sd_])
                    k.op("act", lambda e, st=st: e.activation(out=st[:, 1:2], in_=st[:, 0:1], func=AF.Sqrt, scale=1.0 / 1024, bias=EPS), writes=[sd_])
                    k.op("dve", lambda e, st=st: e.reciprocal(out=st[:, 2:3], in_=st[:, 1:2]), writes=[sd_])
                    cm, cmd = cmb.next()
                    k.op("dve", lambda e, st=st, c=c, cm=cm: e.scalar_tensor_tensor(out=cm, in0=H[:, c, :], scalar=st[:, 2:3], in1=FN, op0=ALU.mult, op1=ALU.mult),
                         reads=[H_d[c], sd_, g2d] + XN_d, writes=[cmd])
                    k.dma("sp", y_d[(c - 2) * 128:(c - 1) * 128, :], cm, reads=[cmd], writes=[y_dep])
            k.barrier()
    k.barrier()


def _consts():
    j = np.arange(128)[:, None]
    i = np.arange(128)[None, :]
    cst = np.zeros((128, NCST), np.float32)
    tri_f = (j <= i).astype(np.float32)
    tri_r = (j >= i).astype(np.float32)
    cst[:, 0:128] = tri_f
    cst[:, 128:256] = tri_r
    cst[:, 256:384] = (j > i).astype(np.float32)
    cst[:, 384:512] = (j < i).astype(np.float32)
    cst[:, 512:640] = np.eye(128, dtype=np.float32)
    cst[:, 640:768] = 1.0
    cst[:, 768:1024] = np.tile(np.where(j <= i, 0.0, NEGV), (1, 2))
    cst[:, 1024:1280] = np.tile(np.where(j >= i, 0.0, NEGV), (1, 2))
    cst[:, 1280:1536] = np.arange(1, 257, dtype=np.float32)[None, :]
    cst[:, 1536] = np.arange(1, 129, dtype=np.float32)
    cst[:, 1537] = np.arange(129, 257, dtype=np.float32)
    msk = np.concatenate([tri_f, tri_f, tri_r, tri_r], axis=1).astype(np.float32)
    esel = np.zeros((16, 16, 128), np.float32)
    for e in range(16):
        esel[e, e, :] = 1.0
    esel = esel.reshape(16, 2048)
    rope = np.zeros((2, 18, 128, 2, 256), np.float32)
    inv = (10000.0 ** (-np.arange(16, dtype=np.float32) / 16)).astype(np.float32)
    for c in range(18):
        for p in range(128):
            if c < 2:
                cosf = np.ones(128, np.float32)
                sinf = np.zeros(128, np.float32)
            else:
                t = (c - 2) * 128 + p
                pos = (np.float32(t // 64), np.float32(t % 64))
                cosf = np.zeros(128, np.float32)
                sinf = np.zeros(128, np.float32)
                for h in range(2):
                    for a in range(2):
                        ang = (pos[a] * inv).astype(np.float32)
                        cs, sn = np.cos(ang), np.sin(ang)
                        b0 = h * 64 + a * 32
                        cosf[b0:b0 + 16] = cs
                        cosf[b0 + 16:b0 + 32] = cs
                        sinf[b0:b0 + 16] = -sn
                        sinf[b0 + 16:b0 + 32] = sn
            rope[0, c, p, 0, 0:128] = cosf * 0.125
            rope[0, c, p, 0, 128:256] = cosf
            rope[0, c, p, 1, 0:128] = sinf * 0.125
            rope[0, c, p, 1, 128:256] = sinf
            rope[1, c, p, 0, 0:128] = 1.0
            rope[1, c, p, 0, 128:256] = 0.125
    return cst, msk, esel, rope


_CACHE = {}


def kernel(**inputs):
    f = lambda a: np.ascontiguousarray(np.asarray(a, dtype=np.float32))
    inp = {kk: f(v) for kk, v in inputs.items()}
    if "nc" not in _CACHE:
        _CACHE["nc"] = build(2)
        _CACHE["consts"] = _consts()
    nc = _CACHE["nc"]
    cst, msk, esel, rope = _CACHE["consts"]
    shared = {
        "w_mod": inp["w_mod"],
        "bmT": np.ascontiguousarray(inp["b_mod"].reshape(2, 48, 128).transpose(0, 2, 1)),
        "nw1T": np.ascontiguousarray(inp["norm1_w"].reshape(2, 8, 128).transpose(0, 2, 1)),
        "nw2T": np.ascontiguousarray(inp["norm2_w"].reshape(2, 8, 128).transpose(0, 2, 1)),
        "w_in": inp["w_in"], "w_out": inp["w_out"],
        "ret_decay_logit": inp["ret_decay_logit"], "ret_norm_w": inp["ret_norm_w"],
        "gla_gate_w": inp["gla_gate_w"], "gla_gate_b": inp["gla_gate_b"], "gla_norm_w": inp["gla_norm_w"],
        "ssd_conv_w": inp["ssd_conv_w"], "ssd_conv_b": inp["ssd_conv_b"], "ssd_dt_bias": inp["ssd_dt_bias"],
        "ssd_a_log": inp["ssd_a_log"], "ssd_d": inp["ssd_d"], "ssd_norm_w": inp["ssd_norm_w"],
        "mlstm_gate_b": inp["mlstm_gate_b"], "mlstm_norm_w": inp["mlstm_norm_w"],
        "router_w": inp["router_w"], "ewg": inp["expert_w_gate"], "ewu": inp["expert_w_up"], "ewd": inp["expert_w_down"],
        "fnw": inp["final_norm_w"].reshape(1, 1024),
        "rope": rope, "cst": cst, "msk": msk, "esel": esel,
    }
    in_maps = []
    for b in range(8):
        m = dict(shared)
        m["x"] = inp["x"][b]
        m["ctx"] = inp["ctx"][b]
        cin = np.stack([inp["c"][b].reshape(8, 128).T, inp["c_ctx"].reshape(8, 128).T], axis=-1)
        m["cin"] = np.ascontiguousarray(cin.astype(np.float32))
        in_maps.append(m)
    res = run_bass_kernel_spmd(nc, in_maps, core_ids=list(range(8)))
    return np.stack([np.asarray(r["y"], dtype=np.float32) for r in res.results], axis=0)
```
